# Optimizing a Trainium2 kernel written in Bass

```python
import jax, jax.numpy as jnp
from jax import lax
import numpy as np

D_MODEL = 1024
BATCH = 4
SEQ = 8192
DEPTH = 4

N_A = DEPTH // 2
N_B = DEPTH - N_A
RET_HEADS = 4
RET_QK_DIM = D_MODEL // RET_HEADS
RET_V_DIM = 2 * RET_QK_DIM
RET_PROJ = 2 * RET_HEADS * RET_QK_DIM + 2 * RET_HEADS * RET_V_DIM
RET_CHUNK = 128
GN_EPS = 1e-6
MLA_HEADS = 8
QK_NOPE = 128
QK_ROPE = 64
V_HEAD = 128
Q_LORA = 256
KV_LORA = 256
ATTN_BLOCK = 128
RMS_EPS = 1e-6
ROPE_THETA = 10000.0
MAX_POS_OFFSET = 4096
N_EXPERTS = 32
TOP_K = 4
D_FF = D_MODEL
SWIGLU_LIMIT = 7.0
SWIGLU_ALPHA = 1.702
MOE_BLOCK = 128
PLE_DIM = 256
DN_ALPHA = (2 * DEPTH) ** 0.25
DN_BETA = (8 * DEPTH) ** -0.25
LN_EPS = 1e-5

kernel_name = 'yoco_retnet_mla_moe_deepnorm_ple'


def layer_norm(x, g, b):
    xf = x.astype(jnp.float32)
    mu = xf.mean(-1, keepdims=True)
    var = jnp.square(xf - mu).mean(-1, keepdims=True)
    return ((xf - mu) * lax.rsqrt(var + LN_EPS) * g + b).astype(x.dtype)


def rms_norm(x, g):
    xf = x.astype(jnp.float32)
    return (xf * lax.rsqrt(jnp.square(xf).mean(-1, keepdims=True) + RMS_EPS) * g).astype(x.dtype)


def rope_tables(positions, dim):
    inv = ROPE_THETA ** (-jnp.arange(0, dim, 2, dtype=jnp.float32) / dim)
    ang = positions.astype(jnp.float32)[..., None] * inv
    return jnp.cos(ang), jnp.sin(ang)


def apply_rope(x, cos, sin):
    x1, x2 = jnp.split(x, 2, axis=-1)
    c = cos[:, :, None, :].astype(x.dtype)
    s = sin[:, :, None, :].astype(x.dtype)
    return jnp.concatenate([x1 * c - x2 * s, x1 * s + x2 * c], axis=-1)


def chunkwise_retention(q, k, v):
    B, S, H, dk = q.shape
    dv = v.shape[-1]
    C = RET_CHUNK
    N = S // C
    log_g = jnp.log(1.0 - 2.0 ** (-5.0 - jnp.arange(H, dtype=jnp.float32)))
    idx = jnp.arange(C, dtype=jnp.float32)
    diff = idx[:, None] - idx[None, :]
    decay_intra = jnp.where(diff >= 0, jnp.exp(log_g[:, None, None] * jnp.maximum(diff, 0.0)), 0.0)
    decay_q = jnp.exp(log_g[:, None] * (idx + 1.0))[None, :, :, None]
    decay_k = jnp.exp(log_g[:, None] * (C - 1.0 - idx))[None, :, :, None]
    decay_chunk = jnp.exp(log_g * C)[None, :, None, None]

    def to_chunks(t):
        return t.astype(jnp.float32).reshape(B, N, C, H, t.shape[-1]).transpose(1, 0, 3, 2, 4)

    qc, kc, vc = to_chunks(q), to_chunks(k), to_chunks(v)

    def step(state, inp):
        qi, ki, vi = inp
        inner = jnp.einsum('bhqd,bhkd->bhqk', qi, ki) * decay_intra
        y = jnp.einsum('bhqk,bhkv->bhqv', inner, vi)
        y = y + jnp.einsum('bhqd,bhdv->bhqv', qi, state) * decay_q
        state = state * decay_chunk + jnp.einsum('bhkd,bhkv->bhdv', ki * decay_k, vi)
        return state, y

    state0 = jnp.zeros((B, H, dk, dv), jnp.float32)
    _, ys = lax.scan(step, state0, (qc, kc, vc))
    return ys.transpose(1, 0, 3, 2, 4).reshape(B, S, H, dv)


def retention_mixer(x, cos, sin, w_in, gn_g, gn_b, w_out):
    B, S, _ = x.shape
    H, dk, dv = RET_HEADS, RET_QK_DIM, RET_V_DIM
    q, k, v, g = jnp.split(x @ w_in, [H * dk, 2 * H * dk, 2 * H * dk + H * dv], axis=-1)
    q = apply_rope(q.reshape(B, S, H, dk), cos, sin)
    k = apply_rope(k.reshape(B, S, H, dk), cos, sin) * (dk ** -0.5)
    y = chunkwise_retention(q, k, v.reshape(B, S, H, dv))
    mu = y.mean(-1, keepdims=True)
    var = jnp.square(y - mu).mean(-1, keepdims=True)
    y = ((y - mu) * lax.rsqrt(var + GN_EPS)).reshape(B, S, H * dv) * gn_g + gn_b
    y = y.astype(x.dtype)
    return (jax.nn.silu(g) * y) @ w_out


def mla_shared_kv(h, cos, sin, w_kv_a, kv_norm_g, w_kv_b):
    B, S, _ = h.shape
    c_kv, k_rope = jnp.split(h @ w_kv_a, [KV_LORA], axis=-1)
    c_kv = rms_norm(c_kv, kv_norm_g)
    kv = (c_kv @ w_kv_b).reshape(B, S, MLA_HEADS, QK_NOPE + V_HEAD)
    k_nope, v = jnp.split(kv, [QK_NOPE], axis=-1)
    k_rope = apply_rope(k_rope[:, :, None, :], cos, sin)[:, :, 0]
    return k_nope, k_rope, v


def mla_mixer(x, cos, sin, k_nope, k_rope, v, w_q_a, q_norm_g, w_q_b, w_o):
    B, S, _ = x.shape
    H = MLA_HEADS
    q = (rms_norm(x @ w_q_a, q_norm_g) @ w_q_b).reshape(B, S, H, QK_NOPE + QK_ROPE)
    q_nope, q_rope = jnp.split(q, [QK_NOPE], axis=-1)
    q_rope = apply_rope(q_rope, cos, sin)
    scale = (QK_NOPE + QK_ROPE) ** -0.5
    nblk = S // ATTN_BLOCK
    qn_b = q_nope.reshape(B, nblk, ATTN_BLOCK, H, QK_NOPE).transpose(1, 0, 2, 3, 4)
    qr_b = q_rope.reshape(B, nblk, ATTN_BLOCK, H, QK_ROPE).transpose(1, 0, 2, 3, 4)
    key_pos = jnp.arange(S)

    def block(args):
        i, qn, qr = args
        s = jnp.einsum('bqhd,bkhd->bhqk', qn, k_nope) + jnp.einsum('bqhr,bkr->bhqk', qr, k_rope)
        s = s.astype(jnp.float32) * scale
        q_pos = i * ATTN_BLOCK + jnp.arange(ATTN_BLOCK)
        s = jnp.where(key_pos[None, :] <= q_pos[:, None], s, -jnp.inf)
        pr = jax.nn.softmax(s, axis=-1).astype(v.dtype)
        return jnp.einsum('bhqk,bkhv->bqhv', pr, v)

    o = lax.map(block, (jnp.arange(nblk), qn_b, qr_b))
    o = o.transpose(1, 0, 2, 3, 4).reshape(B, S, H * V_HEAD)
    return o @ w_o


def moe_ffn(x, w_router, b_router, w_gate_up, b_gate_up, w_down, b_down):
    B, S, D = x.shape
    T = B * S
    xf = x.reshape(T, D)
    logits = (xf @ w_router + b_router).astype(jnp.float32)
    top_vals, top_idx = lax.top_k(logits, TOP_K)
    gates = jax.nn.softmax(top_vals, axis=-1).astype(x.dtype)
    n_assign = T * TOP_K
    e_flat = top_idx.reshape(-1).astype(jnp.int32)
    tok_flat = jnp.repeat(jnp.arange(T, dtype=jnp.int32), TOP_K)
    g_flat = gates.reshape(-1)
    order = jnp.argsort(e_flat)
    e_sorted, tok_sorted, g_sorted = e_flat[order], tok_flat[order], g_flat[order]
    counts = jnp.zeros((N_EXPERTS,), jnp.int32).at[e_flat].add(1)
    padded = (counts + MOE_BLOCK - 1) // MOE_BLOCK * MOE_BLOCK
    start = jnp.cumsum(counts) - counts
    pend = jnp.cumsum(padded)
    pstart = pend - padded
    dest = pstart[e_sorted] + jnp.arange(n_assign, dtype=jnp.int32) - start[e_sorted]
    n_slots = n_assign + N_EXPERTS * MOE_BLOCK
    n_blocks = n_slots // MOE_BLOCK
    slot_tok = jnp.zeros((n_slots,), jnp.int32).at[dest].set(tok_sorted)
    slot_gate = jnp.zeros((n_slots,), x.dtype).at[dest].set(g_sorted)
    block_start = jnp.arange(n_blocks, dtype=jnp.int32) * MOE_BLOCK
    block_expert = jnp.minimum(jnp.searchsorted(pend, block_start, side='right'), N_EXPERTS - 1)

    def expert_block(args):
        e, toks, gts = args
        hgu = xf[toks] @ w_gate_up[e] + b_gate_up[e]
        gate, up = jnp.split(hgu, 2, axis=-1)
        gate = jnp.minimum(gate, SWIGLU_LIMIT)
        up = jnp.clip(up, -SWIGLU_LIMIT, SWIGLU_LIMIT)
        act = gate * jax.nn.sigmoid(SWIGLU_ALPHA * gate) * (up + 1.0)
        return (act @ w_down[e] + b_down[e]) * gts[:, None]

    y_slots = lax.map(expert_block, (block_expert, slot_tok.reshape(n_blocks, MOE_BLOCK),
                                     slot_gate.reshape(n_blocks, MOE_BLOCK)))
    y = jnp.zeros((T, D), x.dtype).at[slot_tok].add(y_slots.reshape(n_slots, D))
    return y.reshape(B, S, D)


def setup_inputs(seed: int = 0) -> dict:
    key = jax.random.key(seed)
    ks = jax.random.split(key, 32)
    f32 = jnp.float32

    def nrm(k, shape, scale):
        return jax.random.normal(k, shape, f32) * scale

    def gain(k, shape):
        return 1.0 + 0.02 * jax.random.normal(k, shape, f32)

    H, dk, dv = RET_HEADS, RET_QK_DIM, RET_V_DIM
    x = nrm(ks[0], (BATCH, SEQ, D_MODEL), 1.0)
    p = nrm(ks[1], (DEPTH, BATCH, SEQ, PLE_DIM), 1.0)
    positions = (jax.random.randint(ks[2], (BATCH, 1), 0, MAX_POS_OFFSET, jnp.int32)
                 + jnp.arange(SEQ, dtype=jnp.int32)[None, :])
    ret_col = jnp.concatenate([jnp.ones((2 * H * dk,), f32), jnp.full((H * dv,), DN_BETA, f32),
                               jnp.ones((H * dv,), f32)])
    ret_w_in = nrm(ks[3], (N_A, D_MODEL, RET_PROJ), D_MODEL ** -0.5) * ret_col
    ret_gn_g = gain(ks[4], (N_A, H * dv))
    ret_gn_b = nrm(ks[5], (N_A, H * dv), 0.02)
    ret_w_out = nrm(ks[6], (N_A, H * dv, D_MODEL), (H * dv) ** -0.5 * DN_BETA)
    mla_w_kv_a = nrm(ks[7], (D_MODEL, KV_LORA + QK_ROPE), D_MODEL ** -0.5)
    mla_kv_norm_g = gain(ks[8], (KV_LORA,))
    kv_col = jnp.concatenate([jnp.ones((QK_NOPE,), f32), jnp.full((V_HEAD,), DN_BETA, f32)])
    mla_w_kv_b = (nrm(ks[9], (KV_LORA, MLA_HEADS, QK_NOPE + V_HEAD), KV_LORA ** -0.5) * kv_col
                  ).reshape(KV_LORA, MLA_HEADS * (QK_NOPE + V_HEAD))
    mla_w_q_a = nrm(ks[10], (N_B, D_MODEL, Q_LORA), D_MODEL ** -0.5)
    mla_q_norm_g = gain(ks[11], (N_B, Q_LORA))
    mla_w_q_b = nrm(ks[12], (N_B, Q_LORA, MLA_HEADS * (QK_NOPE + QK_ROPE)), Q_LORA ** -0.5)
    mla_w_o = nrm(ks[13], (N_B, MLA_HEADS * V_HEAD, D_MODEL), (MLA_HEADS * V_HEAD) ** -0.5 * DN_BETA)
    ln_mix_g = gain(ks[14], (DEPTH, D_MODEL))
    ln_mix_b = nrm(ks[15], (DEPTH, D_MODEL), 0.02)
    ln_ffn_g = gain(ks[16], (DEPTH, D_MODEL))
    ln_ffn_b = nrm(ks[17], (DEPTH, D_MODEL), 0.02)
    moe_w_router = nrm(ks[18], (DEPTH, D_MODEL, N_EXPERTS), D_MODEL ** -0.5)
    moe_b_router = nrm(ks[19], (DEPTH, N_EXPERTS), 0.01)
    moe_w_gate_up = nrm(ks[20], (DEPTH, N_EXPERTS, D_MODEL, 2 * D_FF), D_MODEL ** -0.5)
    moe_b_gate_up = nrm(ks[21], (DEPTH, N_EXPERTS, 2 * D_FF), 0.02)
    moe_w_down = nrm(ks[22], (DEPTH, N_EXPERTS, D_FF, D_MODEL), D_FF ** -0.5 * DN_BETA)
    moe_b_down = nrm(ks[23], (DEPTH, N_EXPERTS, D_MODEL), 0.02)
    ple_w_gate = nrm(ks[24], (DEPTH, D_MODEL, D_MODEL), D_MODEL ** -0.5)
    ple_w_proj = nrm(ks[25], (DEPTH, PLE_DIM, D_MODEL), PLE_DIM ** -0.5)
    return {'x': x, 'p': p, 'positions': positions,
            'ret_w_in': ret_w_in, 'ret_gn_g': ret_gn_g, 'ret_gn_b': ret_gn_b, 'ret_w_out': ret_w_out,
            'mla_w_kv_a': mla_w_kv_a, 'mla_kv_norm_g': mla_kv_norm_g, 'mla_w_kv_b': mla_w_kv_b,
            'mla_w_q_a': mla_w_q_a, 'mla_q_norm_g': mla_q_norm_g, 'mla_w_q_b': mla_w_q_b, 'mla_w_o': mla_w_o,
            'ln_mix_g': ln_mix_g, 'ln_mix_b': ln_mix_b, 'ln_ffn_g': ln_ffn_g, 'ln_ffn_b': ln_ffn_b,
            'moe_w_router': moe_w_router, 'moe_b_router': moe_b_router,
            'moe_w_gate_up': moe_w_gate_up, 'moe_b_gate_up': moe_b_gate_up,
            'moe_w_down': moe_w_down, 'moe_b_down': moe_b_down,
            'ple_w_gate': ple_w_gate, 'ple_w_proj': ple_w_proj}


def reference(x, p, positions, ret_w_in, ret_gn_g, ret_gn_b, ret_w_out,
              mla_w_kv_a, mla_kv_norm_g, mla_w_kv_b, mla_w_q_a, mla_q_norm_g, mla_w_q_b, mla_w_o,
              ln_mix_g, ln_mix_b, ln_ffn_g, ln_ffn_b, moe_w_router, moe_b_router,
              moe_w_gate_up, moe_b_gate_up, moe_w_down, moe_b_down, ple_w_gate, ple_w_proj):
    cos_r, sin_r = rope_tables(positions, RET_QK_DIM)
    cos_m, sin_m = rope_tables(positions, QK_ROPE)
    h = x
    shared_kv = None
    for i in range(DEPTH):
        if i < N_A:
            mix = retention_mixer(h, cos_r, sin_r, ret_w_in[i], ret_gn_g[i], ret_gn_b[i], ret_w_out[i])
        else:
            if i == N_A:
                shared_kv = mla_shared_kv(h, cos_m, sin_m, mla_w_kv_a, mla_kv_norm_g, mla_w_kv_b)
            j = i - N_A
            mix = mla_mixer(h, cos_m, sin_m, shared_kv[0], shared_kv[1], shared_kv[2],
                            mla_w_q_a[j], mla_q_norm_g[j], mla_w_q_b[j], mla_w_o[j])
        h = layer_norm(DN_ALPHA * h + mix, ln_mix_g[i], ln_mix_b[i])
        ffn = moe_ffn(h, moe_w_router[i], moe_b_router[i], moe_w_gate_up[i], moe_b_gate_up[i],
                      moe_w_down[i], moe_b_down[i])
        h = layer_norm(DN_ALPHA * h + ffn, ln_ffn_g[i], ln_ffn_b[i])
        h = h + jax.nn.sigmoid(h @ ple_w_gate[i]) * (p[i] @ ple_w_proj[i])
    return h
```

```python
import contextlib
import numpy as np
import concourse.bass as bass
import concourse.mybir as mybir
from concourse.bass_utils import run_bass_kernel_spmd

F32 = mybir.dt.float32
BF16 = mybir.dt.bfloat16
I32 = mybir.dt.int32
U32 = mybir.dt.uint32
AF = mybir.ActivationFunctionType
ALU = mybir.AluOpType

D = 1024
SEM_ROT = 30000
DN_EPS = 1e-5


import types


def _snap(fn):
    if fn.__closure__ is None:
        return fn
    cells = []
    for c in fn.__closure__:
        try:
            cells.append(types.CellType(c.cell_contents))
        except ValueError:
            cells.append(c)
    g = types.FunctionType(fn.__code__, fn.__globals__, fn.__name__, fn.__defaults__, tuple(cells))
    g.__kwdefaults__ = fn.__kwdefaults__
    return g


class Op:
    __slots__ = ("eng", "fn", "deps", "signal", "event", "dma_key")

    def __init__(self, eng, fn, deps, dma_key):
        self.eng = eng
        self.fn = fn
        self.deps = deps
        self.signal = dma_key is not None
        self.event = None
        self.dma_key = dma_key


class Prog:
    ENGS = ("pe", "act", "dve", "pool", "sp")

    def __init__(self, nc):
        self.nc = nc
        self.ops = {e: [] for e in self.ENGS}
        self.last_w = {}
        self.readers = {}
        self.all_ops = []
        self.pending = {}
        self.last_dma = {}

    def add(self, eng, fn, reads=(), writes=(), dma_key=None):
        pr = [r for r in reads if isinstance(r, str) and r.startswith(("psf", "psb"))]
        if pr:
            reads = [r for r in reads if r not in pr]
            writes = list(writes) + [r for r in pr if r not in writes]
        deps = []
        for r in reads:
            w = self.last_w.get(r)
            if w is not None:
                deps.append(w)
        for w_ in writes:
            w = self.last_w.get(w_)
            if w is not None:
                deps.append(w)
            deps.extend(self.readers.get(w_, ()))
        if eng in self.pending:
            deps.extend(self.pending.pop(eng))
        op = Op(eng, _snap(fn), deps, dma_key)
        for r in reads:
            self.readers.setdefault(r, []).append(op)
        for w_ in writes:
            self.last_w[w_] = op
            self.readers[w_] = []
        self.ops[eng].append(op)
        self.all_ops.append(op)
        if dma_key is not None:
            self.last_dma[dma_key] = op
        return op

    def barrier(self):
        deps = [self.ops[e][-1] for e in self.ENGS if self.ops[e]]
        deps += list(self.last_dma.values())
        for e in self.ENGS:
            self.pending[e] = list(deps) + self.pending.get(e, [])
        self.last_w = {}
        self.readers = {}

    def emit(self, final_ops=()):
        nc = self.nc
        for op in self.all_ops:
            for d in op.deps:
                if d is op:
                    continue
                if d.eng == "pe" and op.eng == "pe" and d.dma_key is None and op.dma_key is None:
                    continue
                d.signal = True
        for op in final_ops:
            op.signal = True
        eng_cnt = {e: 0 for e in self.ENGS}
        key_cnt = {}
        names = []
        for e in self.ENGS:
            for op in self.ops[e]:
                if op.dma_key is not None:
                    k = ("dma", op.dma_key)
                    key_cnt[k] = key_cnt.get(k, 0) + 16
                    op.event = (k, key_cnt[k])
                elif op.signal:
                    eng_cnt[e] += 1
                    c = eng_cnt[e]
                    k = ("eng", e, (c - 1) // SEM_ROT)
                    op.event = (k, (c - 1) % SEM_ROT + 1)
                else:
                    continue
                if op.event[0] not in names:
                    names.append(op.event[0])
        self.n_sems = len(names)
        with contextlib.ExitStack() as st:
            sems = {}
            for i, k in enumerate(names):
                sems[k] = st.enter_context(nc.semaphore("s%d" % i))
            block = st.enter_context(nc.Block())
            engmap = {"pe": block.tensor, "act": block.scalar, "dve": block.vector,
                      "pool": block.gpsimd, "sp": block.sync}
            for e in self.ENGS:
                ops = self.ops[e]
                if not ops and e != "sp":
                    continue

                def body(eng, ops=ops, e=e):
                    known = {}
                    for op in ops:
                        need = {}
                        for d in op.deps:
                            if d is op or d.event is None:
                                continue
                            if d.eng == "pe" and e == "pe" and d.dma_key is None and op.dma_key is None:
                                continue
                            k, v = d.event
                            if need.get(k, 0) < v:
                                need[k] = v
                        for k, v in need.items():
                            if known.get(k, 0) < v:
                                eng.wait_ge(sems[k], v)
                                known[k] = v
                        ins = op.fn(eng)
                        if op.event is not None:
                            ins.then_inc(sems[op.event[0]], 16 if op.dma_key is not None else 1)
                    if e == "sp":
                        for op in final_ops:
                            k, v = op.event
                            if known.get(k, 0) < v:
                                eng.wait_ge(sems[k], v)
                                known[k] = v
                engmap[e](body)


def model_consts(E):
    H, dk, C = 4, 256, 128
    log_g = np.log(1.0 - 2.0 ** (-5.0 - np.arange(H, dtype=np.float64)))
    idx = np.arange(C, dtype=np.float64)
    diff = idx[:, None] - idx[None, :]
    intra = np.where(diff >= 0, np.exp(log_g[:, None, None] * np.maximum(diff, 0.0)), 0.0)
    maskT = np.transpose(intra, (0, 2, 1)) * dk ** -0.5
    decay_q = np.exp(log_g[None, :] * (idx[:, None] + 1.0))
    decay_k = np.exp(log_g[None, :] * (C - 1.0 - idx[:, None])) * dk ** -0.5
    decay_chunk = np.exp(log_g * C)
    inv_r = (10000.0 ** (-np.arange(0, 256, 2, dtype=np.float32) / np.float32(256))).astype(np.float32)
    inv_m = (10000.0 ** (-np.arange(0, 64, 2, dtype=np.float32) / np.float32(64))).astype(np.float32)
    c = {}
    c["ident"] = np.eye(128, dtype=np.float32)
    c["maskT"] = np.ascontiguousarray(np.transpose(maskT, (1, 0, 2))).astype(np.float32)
    c["dq"] = decay_q.astype(np.float32)
    c["dk"] = decay_k.astype(np.float32)
    c["inv_r"] = np.tile(inv_r[None, :], (128, 1)).astype(np.float32)
    c["inv_m"] = np.tile(inv_m[None, :], (128, 1)).astype(np.float32)
    c["causal"] = (np.arange(128)[:, None] <= np.arange(128)[None, :]).astype(np.float32)
    c["tri"] = (np.arange(128)[:, None] < np.arange(128)[None, :]).astype(np.float32)
    c["iota_e"] = np.tile(np.arange(E, dtype=np.float32)[None, :], (128, 1))
    return c, [float(v) for v in decay_chunk]


CONST_SHAPES = lambda E: {"ident": [128, 128], "maskT": [128, 4, 128], "dq": [128, 4], "dk": [128, 4],
                          "inv_r": [128, 128], "inv_m": [128, 32], "causal": [128, 128], "tri": [128, 128],
                          "iota_e": [128, E]}


def build(S, E, DEPTH, NA, CAP, dbg=False):
    import os
    LIMIT = int(os.environ.get('KLIMIT', '999'))
    PH = [0]

    def skip():
        PH[0] += 1
        return PH[0] > LIMIT
    NT = S // 128
    NB = DEPTH - NA
    TOPK = 4
    ALPHA = float((2 * DEPTH) ** 0.25)
    SCALE = float(192 ** -0.5)
    NSLOT = E * CAP
    CT = CAP // 128
    consts_np, dchunk = model_consts(E)

    nc = bass.Bass("TRN2", target_bir_lowering=False)

    def din(name, shape, dt=F32):
        return nc.dram_tensor(name, list(shape), dt, kind="ExternalInput")

    x_d = din("x", [S, D])
    p_d = din("p", [DEPTH, S, 256])
    pos_d = din("pos_tm", [128, NT], I32)
    W = {}
    W["ret_w_in"] = din("ret_w_in", [NA, D, 6144])
    W["ret_gn_g"] = din("ret_gn_g", [NA, 2048])
    W["ret_gn_b"] = din("ret_gn_b", [NA, 2048])
    W["ret_w_out"] = din("ret_w_out", [NA, 2048, D])
    W["mla_w_kv_a"] = din("mla_w_kv_a", [D, 320])
    W["mla_kv_norm_g"] = din("mla_kv_norm_g", [1, 256])
    W["mla_w_kv_b"] = din("mla_w_kv_b", [256, 2048])
    W["mla_w_q_a"] = din("mla_w_q_a", [NB, D, 256])
    W["mla_q_norm_g"] = din("mla_q_norm_g", [NB, 256])
    W["mla_w_q_b"] = din("mla_w_q_b", [NB, 256, 1536])
    W["mla_w_o"] = din("mla_w_o", [NB, D, D])
    for n in ("ln_mix_g", "ln_mix_b", "ln_ffn_g", "ln_ffn_b"):
        W[n] = din(n, [DEPTH, D])
    W["moe_w_router"] = din("moe_w_router", [DEPTH, D, E])
    W["moe_b_router"] = din("moe_b_router", [DEPTH, E])
    W["moe_w_gate_up"] = din("moe_w_gate_up", [DEPTH, E, D, 2048])
    W["moe_b_gate_up"] = din("moe_b_gate_up", [DEPTH, E, 128, 16])
    W["moe_w_down"] = din("moe_w_down", [DEPTH, E, D, D])
    W["moe_b_down"] = din("moe_b_down", [DEPTH, E, D])
    W["ple_w_gate"] = din("ple_w_gate", [DEPTH, D, D])
    W["ple_w_proj"] = din("ple_w_proj", [DEPTH, 256, D])
    CD = {k: din("c_" + k, shp) for k, shp in CONST_SHAPES(E).items()}
    y_d = nc.dram_tensor("y", [S, D], F32, kind="ExternalOutput")
    dbg_d = nc.dram_tensor("dbg", [DEPTH, S, D], F32, kind="ExternalOutput") if dbg else None
    dq_d = nc.dram_tensor("dq", [S, 6144], BF16, kind="ExternalOutput") if dbg else None
    dq2_d = nc.dram_tensor("dq2", [S, 6144], BF16, kind="ExternalOutput") if dbg else None

    hbuf = nc.dram_tensor("hbuf", [S, D], F32)
    qkvg_d = nc.dram_tensor("qkvg", [S, 6144], BF16)
    xs_h = [nc.dram_tensor("xs%d" % i, [NSLOT, 512], BF16) for i in range(2)]
    ys_q = [nc.dram_tensor("ys%d" % i, [NSLOT, 256], F32) for i in range(4)]
    KT_d = nc.dram_tensor("KT", [8, 128, S], BF16)
    KR_d = nc.dram_tensor("KR", [65, S], BF16)
    V_d = nc.dram_tensor("Vx", [S, 8, 130], BF16)
    QT_d = nc.dram_tensor("QT", [8, 128, S], BF16)
    QR_d = nc.dram_tensor("QR", [8, 65, S], BF16)
    O_d = nc.dram_tensor("Oa", [S, D], BF16)

    P = Prog(nc)
    uid = [0]

    def bc_row(handle, row_off, n):
        return bass.AP(handle, row_off, [[0, 128], [1, n]])

    with contextlib.ExitStack() as root:
        sbstate = {"cur": 0, "persist": 0, "st": None}
        DTB = {F32: 4, BF16: 2, I32: 4, U32: 4}

        def sb(st, name, shape, dt):
            uid[0] += 1
            nb = DTB[dt]
            for d_ in shape[1:]:
                nb *= d_
            nb = (nb + 63) // 64 * 64
            if True:
                return nc.alloc_sbuf_tensor("%s_%d" % (name, uid[0]), list(shape), dt) if st is root else st.enter_context(nc.sbuf_tensor("%s_%d" % (name, uid[0]), list(shape), dt))
            if st is root:
                assert sbstate["st"] is None
                off = sbstate["persist"]
                sbstate["persist"] += nb
            else:
                if sbstate["st"] is not st:
                    sbstate["st"] = st
                    sbstate["cur"] = sbstate["persist"]
                off = sbstate["cur"]
                sbstate["cur"] += nb
                assert sbstate["cur"] <= 190 * 1024, ("SBUF overflow", name, sbstate["cur"])
            return nc.alloc_sbuf_tensor_at("%s_%d" % (name, uid[0]), list(shape), dt, offset=off)

        psf = [root.enter_context(nc.psum_tensor("psf%d" % i, [128, 512], F32)) for i in range(6)]
        psb = [root.enter_context(nc.psum_tensor("psb%d" % i, [128, 1024], BF16)) for i in range(2)]
        PSF = ["psf%d" % i for i in range(6)]
        PSB = ["psb%d" % i for i in range(2)]

        ident_f = sb(root, "ident_f", [128, 128], F32)
        ident_b = sb(root, "ident_b", [128, 128], BF16)
        ones_b = sb(root, "ones_b", [128, 128], BF16)
        ones_f = sb(root, "ones_f", [128, 128], F32)
        eps_ln = sb(root, "eps_ln", [128, 1], F32)
        eps_6 = sb(root, "eps_6", [128, 1], F32)
        posf = sb(root, "posf", [128, NT], F32)
        posi = sb(root, "posi", [128, NT], I32)
        inv_r = sb(root, "inv_r", [128, 128], F32)
        inv_m = sb(root, "inv_m", [128, 32], F32)
        slots_i = sb(root, "slots_i", [128, NT, TOPK], I32)
        gates_s = sb(root, "gates_s", [128, NT, TOPK], F32)

        def dma(eng, out, in_, reads, writes, key):
            return P.add(eng, lambda e: e.dma_start(out=out, in_=in_), reads, writes, dma_key=key)

        dma("sp", ident_f[:], CD["ident"].ap(), [], ["ident_f"], "ident_f")
        dma("sp", posi[:], pos_d.ap(), [], ["posi"], "posi")
        dma("sp", inv_r[:], CD["inv_r"].ap(), [], ["inv_r"], "inv_r")
        dma("sp", inv_m[:], CD["inv_m"].ap(), [], ["inv_m"], "inv_m")
        P.add("dve", lambda e: e.tensor_copy(out=ident_b[:], in_=ident_f[:]), ["ident_f"], ["ident_b"])
        P.add("dve", lambda e: e.tensor_copy(out=posf[:], in_=posi[:]), ["posi"], ["posf"])
        zeros_f = sb(root, "zeros_f", [128, 1024], F32)
        P.add("dve", lambda e: e.memset(zeros_f[:], 0.0), [], ["zeros_f"])
        P.add("pool", lambda e: e.memset(ones_f[:], 1.0), [], ["ones_f"])
        P.add("pool", lambda e: e.tensor_copy(out=ones_b[:], in_=ones_f[:]), ["ones_f"], ["ones_b"])
        P.add("pool", lambda e: e.memset(eps_ln[:], DN_EPS), [], ["eps_ln"])
        P.add("pool", lambda e: e.memset(eps_6[:], 1e-6), [], ["eps_6"])
        dma("sp", hbuf.ap(), x_d.ap(), [], ["hbuf"], "x2h")
        P.barrier()

        rr = {"cast": 0, "ps": 0}

        def load_w_bf16(st, dst, dst_name, src2d, KC, N, stage, nstage):
            CH = stage[0].shape[1]
            i = 0
            for kc in range(KC):
                for n0 in range(0, N, CH):
                    n1 = min(N, n0 + CH)
                    b = rr["cast"] % nstage
                    rr["cast"] += 1
                    sname = "wst%d" % b
                    dma("sp", stage[b][:, 0:n1 - n0], src2d[kc * 128:(kc + 1) * 128, n0:n1], [], [sname], sname)
                    eng = "pool" if (rr["cast"] % 2) else "act"
                    if eng == "pool":
                        P.add("pool", lambda e, b=b, kc=kc, n0=n0, n1=n1: e.tensor_copy(out=dst[:, kc, n0:n1], in_=stage[b][:, 0:n1 - n0]),
                              [sname], [dst_name])
                    else:
                        P.add("act", lambda e, b=b, kc=kc, n0=n0, n1=n1: e.copy(out=dst[:, kc, n0:n1], in_=stage[b][:, 0:n1 - n0]),
                              [sname], [dst_name])
                    i += 1

        def transpose_blocks(src, src_name, nblk, dst, dst_name, rows=128, blkw=128, src_off=0):
            for g0 in range(0, nblk, 8):
                g1 = min(nblk, g0 + 8)
                pb = rr["ps"] % 2
                rr["ps"] += 1
                for j in range(g0, g1):
                    P.add("pe", lambda e, j=j, pb=pb, g0=g0: e.transpose(
                        out=psb[pb][0:blkw, (j - g0) * 128:(j - g0 + 1) * 128],
                        in_=src[:, src_off + j * blkw: src_off + (j + 1) * blkw], identity=ident_b[:]),
                        [src_name, "ident_b"], [PSB[pb]])
                P.add("act", lambda e, pb=pb, g0=g0, g1=g1: e.copy(
                    out=dst[0:blkw, g0:g1, :], in_=psb[pb][0:blkw, 0:(g1 - g0) * 128].rearrange("p (j t) -> p j t", t=128)),
                    [PSB[pb]], [dst_name])

        def rstd_from(var_ap, eps_tile, out_ap, tmp_ap, names_r, name_w, scale=1.0):
            epsv = DN_EPS if eps_tile is eps_ln else 1e-6
            P.add("dve", lambda e: e.tensor_scalar(out=tmp_ap, in0=var_ap, scalar1=float(scale), scalar2=float(epsv), op0=ALU.mult, op1=ALU.add),
                  list(names_r), [name_w + "_t"])
            P.add("act", lambda e: e.activation(out=tmp_ap, in_=tmp_ap, func=AF.Ln), [name_w + "_t"], [name_w + "_t"])
            P.add("act", lambda e: e.activation(out=out_ap, in_=tmp_ap, func=AF.Exp, scale=-0.5),
                  [name_w + "_t"], [name_w])

        def layer_norm(z, zname, out, oname, g_t, b_t, gb_names, small, sname):
            for c in range(2):
                P.add("dve", lambda e, c=c: e.bn_stats(out=small[:, c * 6:(c + 1) * 6], in_=z[:, c * 512:(c + 1) * 512]),
                      [zname], [sname])
            P.add("dve", lambda e: e.bn_aggr(out=small[:, 12:14], in_=small[:, 0:12].rearrange("p (c s) -> p c s", s=6)),
                  [sname], [sname])
            rstd_from(small[:, 13:14], eps_ln, small[:, 15:16], small[:, 14:15], [sname], sname + "r")
            P.add("dve", lambda e: e.tensor_scalar(out=out[:], in0=z[:], scalar1=small[:, 12:13], scalar2=small[:, 15:16],
                                                   op0=ALU.subtract, op1=ALU.mult), [zname, sname, sname + "r"], [oname])
            P.add("pool", lambda e: e.tensor_tensor(out=out[:], in0=out[:], in1=g_t[:], op=ALU.mult), [oname, gb_names[0]], [oname])
            P.add("pool", lambda e: e.tensor_tensor(out=out[:], in0=out[:], in1=b_t[:], op=ALU.add), [oname, gb_names[1]], [oname])

        def rope_tables(t, inv_t, nf, cos_t, sin_t, tmpf, tmpi, tag):
            for which, dst, shift in (("s", sin_t, 0.0), ("c", cos_t, float(np.pi / 2))):
                nm = tag + which
                P.add("dve", lambda e, dst=dst, shift=shift: e.tensor_scalar(
                    out=dst[:, 0:nf], in0=inv_t[:, 0:nf], scalar1=posf[:, t:t + 1], scalar2=shift, op0=ALU.mult, op1=ALU.add),
                    ["posf", "inv"], [nm])
                P.add("dve", lambda e, dst=dst: e.tensor_scalar(out=tmpf[:, 0:nf], in0=dst[:, 0:nf], scalar1=float(1 / (2 * np.pi)),
                                                                scalar2=None, op0=ALU.mult), [nm], [tag + "tf"])
                P.add("dve", lambda e: e.tensor_copy(out=tmpi[:, 0:nf], in_=tmpf[:, 0:nf]), [tag + "tf"], [tag + "ti"])
                P.add("dve", lambda e: e.tensor_copy(out=tmpf[:, 0:nf], in_=tmpi[:, 0:nf]), [tag + "ti"], [tag + "tf"])
                P.add("dve", lambda e, dst=dst: e.scalar_tensor_tensor(out=dst[:, 0:nf], in0=tmpf[:, 0:nf], scalar=float(-2 * np.pi),
                                                                       in1=dst[:, 0:nf], op0=ALU.mult, op1=ALU.add), [tag + "tf", nm], [nm])
                P.add("dve", lambda e, dst=dst: e.tensor_scalar(out=tmpf[:, 0:nf], in0=dst[:, 0:nf], scalar1=float(np.pi), scalar2=float(-2 * np.pi),
                                                                op0=ALU.is_gt, op1=ALU.mult), [nm], [tag + "tf"])
                P.add("dve", lambda e, dst=dst: e.tensor_tensor(out=dst[:, 0:nf], in0=dst[:, 0:nf], in1=tmpf[:, 0:nf], op=ALU.add), [nm, tag + "tf"], [nm])
                P.add("dve", lambda e, dst=dst: e.tensor_scalar(out=tmpf[:, 0:nf], in0=dst[:, 0:nf], scalar1=float(-np.pi), scalar2=float(2 * np.pi),
                                                                op0=ALU.is_lt, op1=ALU.mult), [nm], [tag + "tf"])
                P.add("dve", lambda e, dst=dst: e.tensor_tensor(out=dst[:, 0:nf], in0=dst[:, 0:nf], in1=tmpf[:, 0:nf], op=ALU.add), [nm, tag + "tf"], [nm])
                P.add("act", lambda e, dst=dst: e.activation(out=dst[:, 0:nf], in_=dst[:, 0:nf], func=AF.Sin), [nm], [nm])

        def rope_apply(x1, x2, o1, o2, cos_t, sin_t, nf, t1, t2, rnames, wname, tag):
            P.add("dve", lambda e: e.tensor_tensor(out=t1[:, 0:nf], in0=x1, in1=cos_t[:, 0:nf], op=ALU.mult), rnames + [tag + "c"], [tag + "t1"])
            P.add("dve", lambda e: e.tensor_tensor(out=t2[:, 0:nf], in0=x2, in1=sin_t[:, 0:nf], op=ALU.mult), rnames + [tag + "s"], [tag + "t2"])
            P.add("pool", lambda e: e.tensor_tensor(out=o1, in0=t1[:, 0:nf], in1=t2[:, 0:nf], op=ALU.subtract), [tag + "t1", tag + "t2"], [wname])
            P.add("dve", lambda e: e.tensor_tensor(out=t1[:, 0:nf], in0=x1, in1=sin_t[:, 0:nf], op=ALU.mult), rnames + [tag + "s"], [tag + "t1"])
            P.add("dve", lambda e: e.tensor_tensor(out=t2[:, 0:nf], in0=x2, in1=cos_t[:, 0:nf], op=ALU.mult), rnames + [tag + "c"], [tag + "t2"])
            P.add("pool", lambda e: e.tensor_tensor(out=o2, in0=t1[:, 0:nf], in1=t2[:, 0:nf], op=ALU.add), [tag + "t1", tag + "t2"], [wname])

        def linear(xT, xT_name, KC, w, w_name, n0, n1, bank, extra=None):
            for kc in range(KC):
                P.add("pe", lambda e, kc=kc: e.matmul(psf[bank][:, 0:n1 - n0], lhsT=xT[:, kc, :], rhs=w[:, kc, n0:n1],
                                                     start=(kc == 0), stop=(kc == KC - 1 and extra is None)),
                      [xT_name, w_name], [PSF[bank]])

        def retention_layer(l):
            if skip():
                return
            with contextlib.ExitStack() as st:
                win = sb(st, "win", [128, 8, 6144], BF16)
                stage = [sb(st, "wst", [128, 2048], F32) for _ in range(2)]
                load_w_bf16(st, win, "win", W["ret_w_in"].ap()[l], 8, 6144, stage, 2)
                hs = [sb(st, "hs", [128, D], F32) for _ in range(2)]
                hb = sb(st, "hb", [128, D], BF16)
                hT = sb(st, "hT", [128, 8, 128], BF16)
                row = [sb(st, "row", [128, 6144], BF16) for _ in range(2)]
                cos_t = sb(st, "cos", [128, 128], F32)
                sin_t = sb(st, "sin", [128, 128], F32)
                tf = sb(st, "tf", [128, 128], F32)
                ti = sb(st, "ti", [128, 128], I32)
                t1 = sb(st, "t1", [128, 128], F32)
                t2 = sb(st, "t2", [128, 128], F32)
                P.last_w["inv"] = P.last_w.get("inv_r")
                for t in range(NT):
                    b = t % 2
                    dma("sp", hs[b][:], hbuf.ap()[t * 128:(t + 1) * 128, :], [("hbuf", t)], ["hs%d" % b], "hs%d" % b)
                    P.add("pool", lambda e, b=b: e.tensor_copy(out=hb[:], in_=hs[b][:]), ["hs%d" % b], ["hb"])
                    transpose_blocks(hb, "hb", 8, hT, "hT")
                    rope_tables(t, inv_r, 128, cos_t, sin_t, tf, ti, "rp")
                    rw = "row%d" % b
                    for n in range(12):
                        bank = n % 4
                        linear(hT, "hT", 8, win, "win", n * 512, (n + 1) * 512, bank)
                        if n < 4:
                            for hh in range(2):
                                c0 = hh * 256
                                rope_apply(psf[bank][:, c0:c0 + 128], psf[bank][:, c0 + 128:c0 + 256],
                                           row[b][:, n * 512 + c0: n * 512 + c0 + 128], row[b][:, n * 512 + c0 + 128: n * 512 + c0 + 256],
                                           cos_t, sin_t, 128, t1, t2, [PSF[bank]], rw, "rp")
                        elif n < 8:
                            P.add("act", lambda e, n=n, b=b, bank=bank: e.copy(out=row[b][:, n * 512:(n + 1) * 512], in_=psf[bank][:, :]),
                                  [PSF[bank]], [rw])
                        else:
                            P.add("act", lambda e, n=n, b=b, bank=bank: e.activation(out=row[b][:, n * 512:(n + 1) * 512], in_=psf[bank][:, :], func=AF.Silu),
                                  [PSF[bank]], [rw])
                    dma("sp", qkvg_d.ap()[t * 128:(t + 1) * 128, :], row[b][:], [rw], [("qkvg", t)], rw)
            P.barrier()
            if dbg and l == 0:
                dma("sp", dq2_d.ap(), qkvg_d.ap(), [], [], "dq2dbg")
                P.barrier()
            if skip():
                return
            with contextlib.ExitStack() as st:
                wout = sb(st, "wout", [128, 16, D], BF16)
                stage = [sb(st, "wst", [128, 1024], F32) for _ in range(2)]
                load_w_bf16(st, wout, "wout", W["ret_w_out"].ap()[l], 16, D, stage, 2)
                gn_g = sb(st, "gn_g", [128, 2048], F32)
                gn_b = sb(st, "gn_b", [128, 2048], F32)
                ln_g = sb(st, "ln_g", [128, D], F32)
                ln_b = sb(st, "ln_b", [128, D], F32)
                dma("sp", gn_g[:], bc_row(W["ret_gn_g"], l * 2048, 2048), [], ["gn_g"], "gn_g")
                dma("sp", gn_b[:], bc_row(W["ret_gn_b"], l * 2048, 2048), [], ["gn_b"], "gn_b")
                dma("sp", ln_g[:], bc_row(W["ln_mix_g"], l * D, D), [], ["ln_g"], "ln_g")
                dma("sp", ln_b[:], bc_row(W["ln_mix_b"], l * D, D), [], ["ln_b"], "ln_b")
                maskT = sb(st, "maskT", [128, 4, 128], F32)
                dq = sb(st, "dq", [128, 4], F32)
                dkc = sb(st, "dkc", [128, 4], F32)
                dma("sp", maskT[:], CD["maskT"].ap(), [], ["maskT"], "maskT")
                dma("sp", dq[:], CD["dq"].ap(), [], ["dq"], "dq")
                dma("sp", dkc[:], CD["dk"].ap(), [], ["dkc"], "dkc")
                Sf = sb(st, "Sf", [128, 8, 512], F32)
                Sb = sb(st, "Sb", [128, 8, 512], BF16)
                P.add("dve", lambda e: e.memset(Sf[:], 0.0), [], ["Sf"])
                for i_ in range(8):
                    P.add("dve", lambda e, i_=i_: e.tensor_copy(out=Sb[:, i_, :], in_=zeros_f[:, 0:512]), ["zeros_f"], ["Sb"])
                row = [sb(st, "row", [128, 6144], BF16) for _ in range(2)]
                hs = [sb(st, "hs", [128, D], F32) for _ in range(2)]
                qd = sb(st, "qd", [128, D], BF16)
                kd = sb(st, "kd", [128, D], BF16)
                qT = sb(st, "qT", [128, 8, 128], BF16)
                qdT = sb(st, "qdT", [128, 8, 128], BF16)
                kT = sb(st, "kT", [128, 8, 128], BF16)
                innerT = sb(st, "innerT", [128, 128], BF16)
                yn = sb(st, "yn", [128, 512], F32)
                yg = sb(st, "yg", [128, 2048], BF16)
                ygT = sb(st, "ygT", [128, 16, 128], BF16)
                z = sb(st, "z", [128, D], F32)
                h1 = [sb(st, "h1", [128, D], F32) for _ in range(2)]
                small = sb(st, "small", [128, 32], F32)
                gsm = sb(st, "gsm", [128, 32], F32)
                ydb = sb(st, "ydb", [128, 512], F32)
                for t in range(NT):
                    b = t % 2
                    rw = "row%d" % b
                    R = row[b]
                    dma("sp", R[:], qkvg_d.ap()[t * 128:(t + 1) * 128, :], [("qkvg", t)], [rw], rw)
                    dma("sp", hs[b][:], hbuf.ap()[t * 128:(t + 1) * 128, :], [("hbuf", t)], ["hs%d" % b], "hs%d" % b)
                    for h in range(4):
                        P.add("dve", lambda e, h=h, R=R: e.tensor_scalar(out=qd[:, h * 256:(h + 1) * 256], in0=R[:, h * 256:(h + 1) * 256],
                                                                          scalar1=dq[:, h:h + 1], scalar2=None, op0=ALU.mult), [rw, "dq"], ["qd"])
                        P.add("pool", lambda e, h=h, R=R: e.tensor_scalar(out=kd[:, h * 256:(h + 1) * 256], in0=R[:, 1024 + h * 256:1024 + (h + 1) * 256],
                                                                           scalar1=dkc[:, h:h + 1], scalar2=None, op0=ALU.mult), [rw, "dkc"], ["kd"])
                    transpose_blocks(R, rw, 8, qT, "qT", src_off=0)
                    transpose_blocks(R, rw, 8, kT, "kT", src_off=1024)
                    transpose_blocks(qd, "qd", 8, qdT, "qdT")
                    for h in range(4):
                        vs = R[:, 2048 + h * 512: 2048 + (h + 1) * 512]
                        for j in range(2):
                            P.add("pe", lambda e, h=h, j=j: e.matmul(psf[4][:, 0:128], lhsT=kT[:, 2 * h + j, :], rhs=qT[:, 2 * h + j, :],
                                                                    start=(j == 0), stop=(j == 1)), ["kT", "qT"], [PSF[4]])
                        P.add("dve", lambda e, h=h: e.tensor_tensor(out=innerT[:], in0=psf[4][:, 0:128], in1=maskT[:, h, :], op=ALU.mult),
                              [PSF[4], "maskT"], ["innerT"])
                        yb = h % 2
                        NOACC = os.environ.get("KNOACC", "") == "1"
                        P.add("pe", lambda e, vs=vs, yb=yb: e.matmul(psf[yb][:, :], lhsT=innerT[:], rhs=vs, start=True, stop=NOACC),
                              ["innerT", rw], [PSF[yb]])
                        for j in range(0 if NOACC else 2):
                            P.add("pe", lambda e, h=h, j=j, yb=yb: e.matmul(psf[yb][:, :], lhsT=qdT[:, 2 * h + j, :], rhs=Sb[:, 2 * h + j, :],
                                                                           start=False, stop=(j == 1)), ["qdT", "Sb"], [PSF[yb]])
                        if os.environ.get("KDBG", "") in ("y", "rv") and h == 3:
                            P.add("dve", lambda e, yb=yb, b=b: e.tensor_copy(out=ydb[:], in_=psf[yb][:, :]), [PSF[yb]], ["ydb"])
                        P.add("dve", lambda e, yb=yb: e.bn_stats(out=gsm[:, 0:6], in_=psf[yb][:, :]), [PSF[yb]], ["gsm"])
                        P.add("dve", lambda e: e.bn_aggr(out=gsm[:, 6:8], in_=gsm[:, 0:6]), ["gsm"], ["gsm"])
                        rstd_from(gsm[:, 7:8], eps_6, gsm[:, 9:10], gsm[:, 8:9], ["gsm"], "gsmr")
                        P.add("dve", lambda e, yb=yb: e.tensor_scalar(out=yn[:], in0=psf[yb][:, :], scalar1=gsm[:, 6:7], scalar2=gsm[:, 9:10],
                                                                      op0=ALU.subtract, op1=ALU.mult), [PSF[yb], "gsm", "gsmr"], ["yn"])
                        P.add("pool", lambda e, h=h: e.tensor_tensor(out=yn[:], in0=yn[:], in1=gn_g[:, h * 512:(h + 1) * 512], op=ALU.mult), ["yn", "gn_g"], ["yn"])
                        P.add("pool", lambda e, h=h: e.tensor_tensor(out=yn[:], in0=yn[:], in1=gn_b[:, h * 512:(h + 1) * 512], op=ALU.add), ["yn", "gn_b"], ["yn"])
                        P.add("pool", lambda e, h=h, R=R: e.tensor_tensor(out=yg[:, h * 512:(h + 1) * 512], in0=yn[:], in1=R[:, 4096 + h * 512:4096 + (h + 1) * 512],
                                                                           op=ALU.mult), ["yn", rw], ["yg"])
                        for j in range(2):
                            sbk = 2 + j
                            P.add("pe", lambda e, h=h, j=j, vs=vs, sbk=sbk: e.matmul(psf[sbk][:, :], lhsT=kd[:, h * 256 + j * 128: h * 256 + (j + 1) * 128],
                                                                                    rhs=vs, start=True, stop=True), ["kd", rw], [PSF[sbk]])
                            P.add("dve", lambda e, h=h, j=j, sbk=sbk: e.scalar_tensor_tensor(out=Sf[:, 2 * h + j, :], in0=Sf[:, 2 * h + j, :], scalar=dchunk[h],
                                                                                             in1=psf[sbk][:, :], op0=ALU.mult, op1=ALU.add),
                                  ["Sf", PSF[sbk]], ["Sf"])
                            P.add("dve", lambda e, h=h, j=j: e.tensor_copy(out=Sb[:, 2 * h + j, :], in_=Sf[:, 2 * h + j, :]), ["Sf"], ["Sb"])
                    transpose_blocks(yg, "yg", 16, ygT, "ygT")
                    for nh in range(2):
                        for fc in range(16):
                            P.add("pe", lambda e, nh=nh, fc=fc: e.matmul(psf[nh][:, :], lhsT=ygT[:, fc, :], rhs=wout[:, fc, nh * 512:(nh + 1) * 512],
                                                                        start=(fc == 0), stop=(fc == 15)), ["ygT", "wout"], [PSF[nh]])
                        P.add("dve", lambda e, nh=nh, b=b: e.scalar_tensor_tensor(out=z[:, nh * 512:(nh + 1) * 512], in0=hs[b][:, nh * 512:(nh + 1) * 512],
                                                                                  scalar=ALPHA, in1=psf[nh][:, :], op0=ALU.mult, op1=ALU.add),
                              ["hs%d" % b, PSF[nh]], ["z"])
                    layer_norm(z, "z", h1[b], "h1%d" % b, ln_g, ln_b, ["ln_g", "ln_b"], small, "small")
                    kd_ = os.environ.get("KDBG", "")
                    if kd_ == "z":
                        P.add("dve", lambda e, b=b: e.tensor_copy(out=h1[b][:], in_=z[:]), ["z", "h1%d" % b], ["h1%d" % b])
                    elif kd_ == "yg":
                        P.add("dve", lambda e, b=b: e.tensor_copy(out=h1[b][:], in_=yg[:, 0:1024]), ["yg", "h1%d" % b], ["h1%d" % b])
                    elif kd_ == "y":
                        P.add("dve", lambda e, b=b: e.tensor_copy(out=h1[b][:, 0:512], in_=yg[:, 1536:2048]), ["yg", "h1%d" % b], ["h1%d" % b])
                        P.add("dve", lambda e, b=b: e.tensor_copy(out=h1[b][:, 512:1024], in_=ydb[:]), ["ydb", "h1%d" % b], ["h1%d" % b])
                    elif kd_ == "rv":
                        P.add("dve", lambda e, b=b, R=R: e.tensor_copy(out=h1[b][:, 0:512], in_=R[:, 3584:4096]), [rw, "h1%d" % b], ["h1%d" % b])
                        P.add("dve", lambda e, b=b: e.tensor_copy(out=h1[b][:, 512:1024], in_=ydb[:]), ["ydb", "h1%d" % b], ["h1%d" % b])
                    elif kd_ == "yn":
                        P.add("dve", lambda e, b=b: e.tensor_copy(out=h1[b][:, 0:512], in_=yn[:]), ["yn", "h1%d" % b], ["h1%d" % b])
                        P.add("dve", lambda e, b=b: e.tensor_copy(out=h1[b][:, 512:544], in_=gsm[:]), ["gsm", "gsmr", "h1%d" % b], ["h1%d" % b])
                        P.add("dve", lambda e, b=b: e.tensor_copy(out=h1[b][:, 640:768], in_=innerT[:]), ["innerT", "h1%d" % b], ["h1%d" % b])
                    dma("sp", hbuf.ap()[t * 128:(t + 1) * 128, :], h1[b][:], ["h1%d" % b], [("hbuf", t)], "h1%d" % b)
            P.barrier()

        def moe_ple(l, last):
            if skip():
                return
            with contextlib.ExitStack() as st:
                wr = sb(st, "wr", [128, 8, E], F32)
                dma("sp", wr[:], W["moe_w_router"].ap()[l].rearrange("(kc p) e -> p kc e", p=128), [], ["wr"], "wr")
                br = sb(st, "br", [128, E], F32)
                dma("sp", br[:], bc_row(W["moe_b_router"], l * E, E), [], ["br"], "br")
                iota_e = sb(st, "iota_e", [128, E], F32)
                dma("sp", iota_e[:], CD["iota_e"].ap(), [], ["iota_e"], "iota_e")
                ecap = sb(st, "ecap", [128, E], F32)
                P.add("dve", lambda e: e.tensor_scalar(out=ecap[:], in0=iota_e[:], scalar1=float(CAP), scalar2=None, op0=ALU.mult), ["iota_e"], ["ecap"])
                tri_f = sb(st, "tri_f", [128, 128], F32)
                tri_b = sb(st, "tri_b", [128, 128], BF16)
                dma("sp", tri_f[:], CD["tri"].ap(), [], ["tri_f"], "tri_f")
                P.add("dve", lambda e: e.tensor_copy(out=tri_b[:], in_=tri_f[:]), ["tri_f"], ["tri_b"])
                base = sb(st, "base", [128, E], F32)
                P.add("dve", lambda e: e.memset(base[:], 0.0), [], ["base"])
                hs = [sb(st, "hs", [128, D], F32) for _ in range(2)]
                hb = [sb(st, "hb", [128, D], BF16) for _ in range(2)]
                hT32 = sb(st, "hT32", [128, 8, 128], F32)
                lg = sb(st, "lg", [128, E], F32)
                mx = sb(st, "mx", [128, 8], F32)
                mi = sb(st, "mi", [128, 8], U32)
                mif = sb(st, "mif", [128, 8], F32)
                negm = sb(st, "negm", [128, 1], F32)
                ex = sb(st, "ex", [128, 4], F32)
                rs = sb(st, "rs", [128, 2], F32)
                maskf = sb(st, "maskf", [128, E], F32)
                maskb = sb(st, "maskb", [128, E], BF16)
                slotf = sb(st, "slotf", [128, E], F32)
                eq = sb(st, "eq", [128, E], F32)
                s4f = sb(st, "s4f", [128, 4], F32)
                sk = [[sb(st, "sk", [128, 1], I32) for _ in range(2)] for _ in range(TOPK)]
                for t in range(NT):
                    b = t % 2
                    hn = "hs%d" % b
                    dma("sp", hs[b][:], hbuf.ap()[t * 128:(t + 1) * 128, :], [("hbuf", t)], [hn], hn)
                    P.add("pool", lambda e, b=b: e.tensor_copy(out=hb[b][:], in_=hs[b][:]), [hn], ["hb%d" % b])
                    for half in range(2):
                        bank = 2 + half
                        for j in range(4):
                            kc = half * 4 + j
                            P.add("pe", lambda e, kc=kc, j=j, b=b, bank=bank: e.transpose(out=psf[bank][:, j * 128:(j + 1) * 128],
                                                                                         in_=hs[b][:, kc * 128:(kc + 1) * 128], identity=ident_f[:]),
                                  [hn, "ident_f"], [PSF[bank]])
                        P.add("act", lambda e, half=half, bank=bank: e.copy(out=hT32[:, half * 4:(half + 1) * 4, :],
                                                                             in_=psf[bank][:, :].rearrange("p (j t) -> p j t", t=128)),
                              [PSF[bank]], ["hT32"])
                    for kc in range(8):
                        P.add("pe", lambda e, kc=kc: e.matmul(psf[0][:, 0:E], lhsT=hT32[:, kc, :], rhs=wr[:, kc, :], start=(kc == 0), stop=(kc == 7)),
                              ["hT32", "wr"], [PSF[0]])
                    P.add("dve", lambda e: e.tensor_tensor(out=lg[:], in0=psf[0][:, 0:E], in1=br[:], op=ALU.add), [PSF[0], "br"], ["lg"])
                    P.add("dve", lambda e: e.max(out=mx[:], in_=lg[:]), ["lg"], ["mx"])
                    P.add("dve", lambda e: e.max_index(out=mi[:], in_max=mx[:], in_values=lg[:]), ["lg", "mx"], ["mi"])
                    P.add("dve", lambda e: e.tensor_copy(out=mif[:], in_=mi[:]), ["mi"], ["mif"])
                    P.add("dve", lambda e: e.tensor_scalar(out=negm[:], in0=mx[:, 0:1], scalar1=-1.0, scalar2=None, op0=ALU.mult), ["mx"], ["negm"])
                    P.add("dve", lambda e: e.tensor_scalar(out=ex[:], in0=mx[:, 0:4], scalar1=negm[:, 0:1], scalar2=None, op0=ALU.add), ["mx", "negm"], ["ex"])
                    P.add("act", lambda e: e.activation(out=ex[:], in_=ex[:], func=AF.Exp), ["ex"], ["ex"])
                    P.add("dve", lambda e: e.reduce_sum(out=rs[:, 0:1], in_=ex[:], axis=mybir.AxisListType.X), ["ex"], ["rs"])
                    P.add("dve", lambda e: e.reciprocal(out=rs[:, 1:2], in_=rs[:, 0:1]), ["rs"], ["rs2"])
                    P.add("dve", lambda e, t=t: e.tensor_scalar(out=gates_s[:, t, :], in0=ex[:], scalar1=rs[:, 1:2], scalar2=None, op0=ALU.mult),
                          ["ex", "rs2"], ["gates_s"])
                    P.add("dve", lambda e: e.tensor_scalar(out=maskf[:], in0=lg[:], scalar1=mx[:, 3:4], scalar2=None, op0=ALU.is_ge), ["lg", "mx"], ["maskf"])
                    P.add("dve", lambda e: e.tensor_copy(out=maskb[:], in_=maskf[:]), ["maskf"], ["maskb"])
                    P.add("pe", lambda e: e.matmul(psf[1][:, 0:E], lhsT=tri_b[:], rhs=maskb[:], start=True, stop=True), ["tri_b", "maskb"], [PSF[1]])
                    P.add("pe", lambda e: e.matmul(psf[1][:, 64:64 + E], lhsT=ones_b[:], rhs=maskb[:], start=True, stop=True), ["ones_b", "maskb"], [PSF[1]])
                    P.add("dve", lambda e: e.tensor_tensor(out=slotf[:], in0=psf[1][:, 0:E], in1=base[:], op=ALU.add), [PSF[1], "base"], ["slotf"])
                    P.add("dve", lambda e: e.tensor_tensor(out=slotf[:], in0=slotf[:], in1=ecap[:], op=ALU.add), ["slotf", "ecap"], ["slotf"])
                    P.add("dve", lambda e: e.tensor_tensor(out=base[:], in0=base[:], in1=psf[1][:, 64:64 + E], op=ALU.add), [PSF[1], "base", "slotf"], ["base"])
                    for k in range(TOPK):
                        P.add("dve", lambda e, k=k: e.tensor_scalar(out=eq[:], in0=iota_e[:], scalar1=mif[:, k:k + 1], scalar2=None, op0=ALU.is_equal),
                              ["iota_e", "mif"], ["eq"])
                        P.add("dve", lambda e: e.tensor_tensor(out=eq[:], in0=eq[:], in1=slotf[:], op=ALU.mult), ["eq", "slotf"], ["eq"])
                        P.add("dve", lambda e, k=k: e.reduce_sum(out=s4f[:, k:k + 1], in_=eq[:], axis=mybir.AxisListType.X), ["eq"], ["s4f"])
                    P.add("dve", lambda e, t=t: e.tensor_copy(out=slots_i[:, t, :], in_=s4f[:]), ["s4f"], ["slots_i"])
                    for k in range(TOPK):
                        P.add("dve", lambda e, t=t, k=k, b=b: e.tensor_copy(out=sk[k][b][:, :], in_=s4f[:, k:k + 1]), ["s4f"], ["sk%d_%d" % (k, b)])
                        for c0 in (0, 512):
                            P.add("pool", lambda e, t=t, k=k, b=b, c0=c0: e.indirect_dma_start(
                                out=xs_h[c0 // 512].ap(), out_offset=bass.IndirectOffsetOnAxis(ap=sk[k][b][:, :], axis=0),
                                in_=hb[b][:, c0:c0 + 512], in_offset=None),
                                ["sk%d_%d" % (k, b), "hb%d" % b], ["xs", "idma"], dma_key="hb%d" % b)
            P.barrier()
            if skip():
                return
            with contextlib.ExitStack() as st:
                wgu = [sb(st, "wgu", [128, 8, 2048], BF16) for _ in range(2)]
                wdn = [sb(st, "wdn", [128, 8, D], BF16) for _ in range(2)]
                stage = [sb(st, "wst", [128, 2048], F32) for _ in range(2)]
                bgu = [sb(st, "bgu", [128, 16], F32) for _ in range(2)]
                bdf = [sb(st, "bdf", [1, D], F32) for _ in range(2)]
                bdb = [sb(st, "bdb", [1, D], BF16) for _ in range(2)]
                xr = [sb(st, "xr", [128, D], BF16) for _ in range(2)]
                xT = sb(st, "xT", [128, 8, CAP], BF16)
                actT = sb(st, "actT", [128, 8, CAP], BF16)
                gc = [sb(st, "gc", [128, 512], F32) for _ in range(2)]
                sg = [sb(st, "sg", [128, 512], F32) for _ in range(2)]
                uc = [sb(st, "uc", [128, 512], F32) for _ in range(2)]
                yo = [sb(st, "yo", [128, D], F32) for _ in range(2)]
                cnt = 0
                for ex_ in range(E):
                    eb = ex_ % 2
                    load_w_bf16(st, wgu[eb], "wgu%d" % eb, W["moe_w_gate_up"].ap()[l, ex_], 8, 2048, stage, 2)
                    load_w_bf16(st, wdn[eb], "wdn%d" % eb, W["moe_w_down"].ap()[l, ex_], 8, D, stage, 2)
                    dma("sp", bgu[eb][:], W["moe_b_gate_up"].ap()[l, ex_], [], ["bgu%d" % eb], "bgu%d" % eb)
                    dma("sp", bdf[eb][:], W["moe_b_down"].ap()[l, ex_:ex_ + 1, :], [], ["bdf%d" % eb], "bdf%d" % eb)
                    P.add("dve", lambda e, eb=eb: e.tensor_copy(out=bdb[eb][:], in_=bdf[eb][:]), ["bdf%d" % eb], ["bdb%d" % eb])
                    for s_ in range(CT):
                        xb = cnt % 2
                        cnt += 1
                        r0 = ex_ * CAP + s_ * 128
                        for i_ in range(2):
                            dma("sp", xr[xb][:, i_ * 512:(i_ + 1) * 512], xs_h[i_].ap()[r0:r0 + 128, :], ["xs"], ["xr%d" % xb], "xr%d" % xb)
                        pb = rr["ps"] % 2
                        rr["ps"] += 1
                        for j in range(8):
                            P.add("pe", lambda e, j=j, pb=pb, xb=xb: e.transpose(out=psb[pb][:, j * 128:(j + 1) * 128], in_=xr[xb][:, j * 128:(j + 1) * 128],
                                                                                 identity=ident_b[:]), ["xr%d" % xb, "ident_b"], [PSB[pb]])
                        P.add("act", lambda e, pb=pb, s_=s_: e.copy(out=xT[:, :, s_ * 128:(s_ + 1) * 128], in_=psb[pb][:, :].rearrange("p (j t) -> p j t", t=128)),
                              [PSB[pb]], ["xT"])
                    for fc in range(8):
                        for n0 in range(0, CAP, 512):
                            n1 = min(CAP, n0 + 512)
                            nn = n1 - n0
                            w_ = (fc * 8 + n0 // 512) % 2
                            bg_, bu_ = (0, 1) if w_ == 0 else (2, 3)
                            for kc in range(8):
                                P.add("pe", lambda e, kc=kc, fc=fc, n0=n0, n1=n1, bg_=bg_, eb=eb, nn=nn: e.matmul(
                                    psf[bg_][:, 0:nn], lhsT=wgu[eb][:, kc, fc * 128:(fc + 1) * 128], rhs=xT[:, kc, n0:n1], start=(kc == 0), stop=(kc == 7)),
                                    ["wgu%d" % eb, "xT"], [PSF[bg_]])
                            for kc in range(8):
                                P.add("pe", lambda e, kc=kc, fc=fc, n0=n0, n1=n1, bu_=bu_, eb=eb, nn=nn: e.matmul(
                                    psf[bu_][:, 0:nn], lhsT=wgu[eb][:, kc, 1024 + fc * 128:1024 + (fc + 1) * 128], rhs=xT[:, kc, n0:n1], start=(kc == 0), stop=(kc == 7)),
                                    ["wgu%d" % eb, "xT"], [PSF[bu_]])
                            P.add("dve", lambda e, fc=fc, bg_=bg_, eb=eb, nn=nn, w_=w_: e.tensor_scalar(out=gc[w_][:, 0:nn], in0=psf[bg_][:, 0:nn], scalar1=bgu[eb][:, fc:fc + 1],
                                                                                                      scalar2=7.0, op0=ALU.add, op1=ALU.min), [PSF[bg_], "bgu%d" % eb], ["gc%d" % w_])
                            P.add("act", lambda e, nn=nn, w_=w_: e.activation(out=sg[w_][:, 0:nn], in_=gc[w_][:, 0:nn], func=AF.Sigmoid, scale=1.702), ["gc%d" % w_], ["sg%d" % w_])
                            P.add("dve", lambda e, fc=fc, bu_=bu_, eb=eb, nn=nn, w_=w_: e.tensor_scalar(out=uc[w_][:, 0:nn], in0=psf[bu_][:, 0:nn], scalar1=bgu[eb][:, 8 + fc:9 + fc],
                                                                                                      scalar2=-7.0, op0=ALU.add, op1=ALU.max), [PSF[bu_], "bgu%d" % eb], ["uc%d" % w_])
                            P.add("pool", lambda e, nn=nn, w_=w_: e.tensor_scalar(out=uc[w_][:, 0:nn], in0=uc[w_][:, 0:nn], scalar1=7.0, scalar2=1.0, op0=ALU.min, op1=ALU.add),
                                  ["uc%d" % w_], ["uc%d" % w_])
                            P.add("pool", lambda e, nn=nn, w_=w_: e.tensor_tensor(out=gc[w_][:, 0:nn], in0=gc[w_][:, 0:nn], in1=sg[w_][:, 0:nn], op=ALU.mult),
                                  ["gc%d" % w_, "sg%d" % w_], ["gc%d" % w_])
                            P.add("pool", lambda e, fc=fc, n0=n0, n1=n1, nn=nn, w_=w_: e.tensor_tensor(out=actT[:, fc, n0:n1], in0=gc[w_][:, 0:nn], in1=uc[w_][:, 0:nn], op=ALU.mult),
                                  ["gc%d" % w_, "uc%d" % w_], ["actT"])
                    for s_ in range(CT):
                        yb = s_ % 2
                        for nh in range(2):
                            bank = 4 + nh
                            for fc in range(8):
                                P.add("pe", lambda e, fc=fc, s_=s_, nh=nh, bank=bank, eb=eb: e.matmul(
                                    psf[bank][:, :], lhsT=actT[:, fc, s_ * 128:(s_ + 1) * 128], rhs=wdn[eb][:, fc, nh * 512:(nh + 1) * 512], start=(fc == 0), stop=False),
                                    ["actT", "wdn%d" % eb], [PSF[bank]])
                            P.add("pe", lambda e, nh=nh, bank=bank, eb=eb: e.matmul(psf[bank][:, :], lhsT=ones_b[0:1, :], rhs=bdb[eb][0:1, nh * 512:(nh + 1) * 512],
                                                                                  start=False, stop=True), ["ones_b", "bdb%d" % eb], [PSF[bank]])
                            P.add("act", lambda e, nh=nh, bank=bank, yb=yb: e.copy(out=yo[yb][:, nh * 512:(nh + 1) * 512], in_=psf[bank][:, :]), [PSF[bank]], ["yo%d" % yb])
                        r0 = ex_ * CAP + s_ * 128
                        for i_ in range(4):
                            dma("sp", ys_q[i_].ap()[r0:r0 + 128, :], yo[yb][:, i_ * 256:(i_ + 1) * 256], ["yo%d" % yb], ["ys"], "yo%d_%d" % (yb, i_))
            P.barrier()
            if skip():
                return
            with contextlib.ExitStack() as st:
                wg = sb(st, "wg", [128, 8, D], BF16)
                wp = sb(st, "wp", [128, 2, D], BF16)
                stage = [sb(st, "wst", [128, 1024], F32) for _ in range(2)]
                load_w_bf16(st, wg, "wg", W["ple_w_gate"].ap()[l], 8, D, stage, 2)
                load_w_bf16(st, wp, "wp", W["ple_w_proj"].ap()[l], 2, D, stage, 2)
                ln_g = sb(st, "ln_g", [128, D], F32)
                ln_b = sb(st, "ln_b", [128, D], F32)
                dma("sp", ln_g[:], bc_row(W["ln_ffn_g"], l * D, D), [], ["ln_g"], "ln_g")
                dma("sp", ln_b[:], bc_row(W["ln_ffn_b"], l * D, D), [], ["ln_b"], "ln_b")
                yk = [[sb(st, "yk", [128, 256], F32) for _ in range(4)] for _ in range(4)]
                hs = [sb(st, "hs", [128, D], F32) for _ in range(2)]
                ps_ = [sb(st, "ps_", [128, 256], F32) for _ in range(2)]
                pb_ = sb(st, "pb_", [128, 256], BF16)
                pT = sb(st, "pT", [128, 2, 128], BF16)
                acc = sb(st, "acc", [128, D], F32)
                z = sb(st, "z", [128, D], F32)
                h2 = sb(st, "h2", [128, D], F32)
                h2b = sb(st, "h2b", [128, D], BF16)
                h2T = sb(st, "h2T", [128, 8, 128], BF16)
                sgm = sb(st, "sgm", [128, 512], F32)
                h3 = [sb(st, "h3", [128, D], F32) for _ in range(2)]
                small = sb(st, "small", [128, 32], F32)
                ck = [sb(st, "ck", [128, 1], I32) for _ in range(TOPK)]
                for t in range(NT):
                    b = t % 2
                    hn = "hs%d" % b
                    dma("sp", hs[b][:], hbuf.ap()[t * 128:(t + 1) * 128, :], [("hbuf", t)], [hn], hn)
                    dma("sp", ps_[b][:], p_d.ap()[l, t * 128:(t + 1) * 128, :], [], ["ps_%d" % b], "ps_%d" % b)
                    for k in range(TOPK):
                        P.add("dve", lambda e, t=t, k=k: e.tensor_copy(out=ck[k][:, :], in_=slots_i[:, t, k:k + 1]), ["slots_i"], ["ck%d" % k])
                        for i_ in range(4):
                            P.add("pool", lambda e, t=t, k=k, i_=i_: e.indirect_dma_start(
                                out=yk[k][i_][:, :], out_offset=None, in_=ys_q[i_].ap(), in_offset=bass.IndirectOffsetOnAxis(ap=ck[k][:, :], axis=0)), ["ck%d" % k, "ys"], ["yk%d" % k, "idma"], dma_key="yk%d" % k)
                    for i_ in range(4):
                        cs = slice(i_ * 256, (i_ + 1) * 256)
                        P.add("dve", lambda e, t=t, i_=i_, cs=cs: e.tensor_scalar(out=acc[:, cs], in0=yk[0][i_][:], scalar1=gates_s[:, t, 0:1], scalar2=None, op0=ALU.mult),
                              ["yk0", "gates_s"], ["acc"])
                        for k in range(1, TOPK):
                            P.add("dve", lambda e, t=t, k=k, i_=i_, cs=cs: e.scalar_tensor_tensor(out=acc[:, cs], in0=yk[k][i_][:], scalar=gates_s[:, t, k:k + 1], in1=acc[:, cs],
                                                                                            op0=ALU.mult, op1=ALU.add), ["yk%d" % k, "gates_s", "acc"], ["acc"])
                    P.add("dve", lambda e, b=b: e.scalar_tensor_tensor(out=z[:], in0=hs[b][:], scalar=ALPHA, in1=acc[:], op0=ALU.mult, op1=ALU.add),
                          [hn, "acc"], ["z"])
                    layer_norm(z, "z", h2, "h2", ln_g, ln_b, ["ln_g", "ln_b"], small, "small")
                    P.add("act", lambda e: e.copy(out=h2b[:], in_=h2[:]), ["h2"], ["h2b"])
                    transpose_blocks(h2b, "h2b", 8, h2T, "h2T")
                    P.add("act", lambda e, b=b: e.copy(out=pb_[:], in_=ps_[b][:]), ["ps_%d" % b], ["pb_"])
                    transpose_blocks(pb_, "pb_", 2, pT, "pT")
                    for nh in range(2):
                        linear(h2T, "h2T", 8, wg, "wg", nh * 512, (nh + 1) * 512, nh)
                        linear(pT, "pT", 2, wp, "wp", nh * 512, (nh + 1) * 512, 2 + nh)
                        P.add("act", lambda e, nh=nh: e.activation(out=sgm[:], in_=psf[nh][:, :], func=AF.Sigmoid), [PSF[nh]], ["sgm"])
                        P.add("dve", lambda e, nh=nh: e.tensor_tensor(out=sgm[:], in0=sgm[:], in1=psf[2 + nh][:, :], op=ALU.mult), ["sgm", PSF[2 + nh]], ["sgm"])
                        P.add("pool", lambda e, nh=nh, b=b: e.tensor_tensor(out=h3[b][:, nh * 512:(nh + 1) * 512], in0=sgm[:], in1=h2[:, nh * 512:(nh + 1) * 512], op=ALU.add),
                              ["sgm", "h2"], ["h3%d" % b])
                    if last:
                        fin.append(dma("sp", y_d.ap()[t * 128:(t + 1) * 128, :], h3[b][:], ["h3%d" % b], [("y", t)], "h3%d" % b))
                    else:
                        dma("sp", hbuf.ap()[t * 128:(t + 1) * 128, :], h3[b][:], ["h3%d" % b], [("hbuf", t)], "h3%d" % b)
            P.barrier()
            if dbg:
                dma("sp", dbg_d.ap()[l], (y_d if last else hbuf).ap(), [], [], "dbg")
                P.barrier()

        kmax_bc = sb(root, "kmax_bc", [128, 1], F32)

        def sumsq(ps_ap, n, out_col, junk, psname, wname):
            P.add("act", lambda e: e.activation(out=junk[:, 0:n], in_=ps_ap, func=AF.Square), [psname], ["junk"])
            P.add("dve", lambda e: e.reduce_sum(out=out_col, in_=junk[:, 0:n], axis=mybir.AxisListType.X), ["junk"], [wname])

        def rms_norm_ps(ps_ap, psname, n, g_t, gname, out_b, oname, junk, small, sname, col):
            sumsq(ps_ap, n, small[:, col:col + 1], junk, psname, sname)
            rstd_from(small[:, col:col + 1], eps_6, small[:, col + 2:col + 3], small[:, col + 1:col + 2], [sname], sname + "r", scale=1.0 / n)
            P.add("dve", lambda e: e.tensor_scalar(out=junk[:, 0:n], in0=ps_ap, scalar1=small[:, col + 2:col + 3], scalar2=None, op0=ALU.mult),
                  [psname, sname + "r"], ["junk"])
            P.add("pool", lambda e: e.tensor_tensor(out=out_b, in0=junk[:, 0:n], in1=g_t[:, 0:n], op=ALU.mult), ["junk", gname], [oname])

        def mla_kv():
            if skip():
                return
            with contextlib.ExitStack() as st:
                wa = sb(st, "wa", [128, 8, 320], BF16)
                wb = sb(st, "wb", [128, 2, 2048], BF16)
                stage = [sb(st, "wst", [128, 2048], F32) for _ in range(2)]
                load_w_bf16(st, wa, "wa", W["mla_w_kv_a"].ap(), 8, 320, stage, 2)
                load_w_bf16(st, wb, "wb", W["mla_w_kv_b"].ap(), 2, 2048, stage, 2)
                g_t = sb(st, "kvg", [128, 256], F32)
                dma("sp", g_t[:], bc_row(W["mla_kv_norm_g"], 0, 256), [], ["kvg"], "kvg")
                hs = [sb(st, "hs", [128, D], F32) for _ in range(2)]
                hb = sb(st, "hb", [128, D], BF16)
                hT = sb(st, "hT", [128, 8, 128], BF16)
                junk = sb(st, "junk", [128, 512], F32)
                small = sb(st, "small", [128, 32], F32)
                cb = sb(st, "cb", [128, 256], BF16)
                cT = sb(st, "cT", [128, 2, 128], BF16)
                cos_t = sb(st, "cos", [128, 32], F32)
                sin_t = sb(st, "sin", [128, 32], F32)
                tf = sb(st, "tf", [128, 32], F32)
                ti = sb(st, "ti", [128, 32], I32)
                t1 = sb(st, "t1", [128, 32], F32)
                t2 = sb(st, "t2", [128, 32], F32)
                kn = [sb(st, "kn", [128, D], BF16) for _ in range(2)]
                knT = [sb(st, "knT", [128, 8, 128], BF16) for _ in range(2)]
                vx = [sb(st, "vx", [128, 8, 130], BF16) for _ in range(2)]
                kr = sb(st, "kr", [128, 128], BF16)
                krT = [sb(st, "krT", [128, 1, 128], BF16) for _ in range(2)]
                ksq = sb(st, "ksq", [128, 16], F32)
                kmx = sb(st, "kmx", [128, 8], F32)
                P.add("dve", lambda e: e.memset(kmx[:], 0.0), [], ["kmx"])
                P.add("pool", lambda e: e.tensor_copy(out=kr[:], in_=zeros_f[:, 0:128]), ["zeros_f"], ["kr"])
                P.add("pool", lambda e: e.tensor_copy(out=kr[:, 64:65], in_=ones_f[:, 0:1]), ["kr", "ones_f"], ["kr"])
                for b in range(2):
                    for h_ in range(8):
                        P.add("pool", lambda e, b=b, h_=h_: e.tensor_copy(out=vx[b][:, h_, 128:130], in_=ones_f[:, 0:2]), ["ones_f"], ["vx%d" % b])
                P.last_w["inv"] = P.last_w.get("inv_m")
                for t in range(NT):
                    b = t % 2
                    hn = "hs%d" % b
                    dma("sp", hs[b][:], hbuf.ap()[t * 128:(t + 1) * 128, :], [("hbuf", t)], [hn], hn)
                    P.add("pool", lambda e, b=b: e.tensor_copy(out=hb[:], in_=hs[b][:]), [hn], ["hb"])
                    transpose_blocks(hb, "hb", 8, hT, "hT")
                    rope_tables(t, inv_m, 32, cos_t, sin_t, tf, ti, "kv")
                    linear(hT, "hT", 8, wa, "wa", 0, 320, 4)
                    rms_norm_ps(psf[4][:, 0:256], PSF[4], 256, g_t, "kvg", cb[:], "cb", junk, small, "small", 0)
                    rope_apply(psf[4][:, 256:288], psf[4][:, 288:320], kr[:, 0:32], kr[:, 32:64], cos_t, sin_t, 32, t1, t2, [PSF[4]], "kr", "kv")
                    sumsq(psf[4][:, 256:320], 64, ksq[:, 8:9], junk, PSF[4], "ksq8")
                    P.add("pe", lambda e: e.transpose(out=psb[0][:, 0:128], in_=kr[:, 0:128], identity=ident_b[:]), ["kr", "ident_b"], [PSB[0]])
                    P.add("act", lambda e, b=b: e.copy(out=krT[b][:, 0, :], in_=psb[0][:, 0:128]), [PSB[0]], ["krT%d" % b])
                    dma("sp", KR_d.ap()[:, t * 128:(t + 1) * 128], krT[b][0:65, 0, :], ["krT%d" % b], [("KR", t)], "krT%d" % b)
                    transpose_blocks(cb, "cb", 2, cT, "cT")
                    for n in range(4):
                        bank = n % 4
                        linear(cT, "cT", 2, wb, "wb", n * 512, (n + 1) * 512, bank)
                        for hh in range(2):
                            h = n * 2 + hh
                            P.add("act", lambda e, h=h, hh=hh, bank=bank, b=b: e.copy(out=kn[b][:, h * 128:(h + 1) * 128], in_=psf[bank][:, hh * 256: hh * 256 + 128]),
                                  [PSF[bank]], ["kn%d" % b])
                            sumsq(psf[bank][:, hh * 256: hh * 256 + 128], 128, ksq[:, h:h + 1], junk, PSF[bank], "ksq")
                            P.add("dve", lambda e, h=h, hh=hh, bank=bank, b=b: e.tensor_copy(out=vx[b][:, h, 0:128], in_=psf[bank][:, hh * 256 + 128: hh * 256 + 256]),
                                  [PSF[bank]], ["vx%d" % b])
                    P.add("dve", lambda e: e.tensor_scalar(out=ksq[:, 0:8], in0=ksq[:, 0:8], scalar1=ksq[:, 8:9], scalar2=None, op0=ALU.add), ["ksq", "ksq8"], ["ksq"])
                    P.add("dve", lambda e: e.tensor_tensor(out=kmx[:], in0=kmx[:], in1=ksq[:, 0:8], op=ALU.max), ["ksq", "kmx"], ["kmx"])
                    transpose_blocks(kn[b], "kn%d" % b, 8, knT[b], "knT%d" % b)
                    dma("sp", KT_d.ap()[:, :, t * 128:(t + 1) * 128].rearrange("h d t -> d h t"), knT[b][:], ["knT%d" % b], [("KT", t)], "knT%d" % b)
                    dma("sp", V_d.ap()[t * 128:(t + 1) * 128], vx[b][:], ["vx%d" % b], [("V", t)], "vx%d" % b)
                P.add("dve", lambda e: e.reduce_max(out=ksq[:, 9:10], in_=kmx[:], axis=mybir.AxisListType.X), ["kmx"], ["kmr"])
                P.add("pe", lambda e: e.transpose(out=psf[5][0:1, 0:128], in_=ksq[:, 9:10], identity=ident_f[:]), ["kmr", "ident_f"], [PSF[5]])
                P.add("dve", lambda e: e.reduce_max(out=small[0:1, 20:21], in_=psf[5][0:1, 0:128], axis=mybir.AxisListType.X), [PSF[5]], ["km1"])
                P.add("pe", lambda e: e.matmul(psf[5][:, 256:257], lhsT=ones_f[0:1, :], rhs=small[0:1, 20:21], start=True, stop=True), ["km1", "ones_f"], [PSF[5]])
                P.add("dve", lambda e: e.tensor_copy(out=kmax_bc[:], in_=psf[5][:, 256:257]), [PSF[5]], ["kmax_bc"])
            P.barrier()

        def mla_layer(l):
            j = l - NA
            if skip():
                return
            with contextlib.ExitStack() as st:
                wa = sb(st, "wqa", [128, 8, 256], BF16)
                wb = sb(st, "wqb", [128, 2, 1536], BF16)
                stage = [sb(st, "wst", [128, 1536], F32) for _ in range(2)]
                load_w_bf16(st, wa, "wqa", W["mla_w_q_a"].ap()[j], 8, 256, stage, 2)
                load_w_bf16(st, wb, "wqb", W["mla_w_q_b"].ap()[j], 2, 1536, stage, 2)
                g_t = sb(st, "qg", [128, 256], F32)
                dma("sp", g_t[:], bc_row(W["mla_q_norm_g"], j * 256, 256), [], ["qg"], "qg")
                hs = [sb(st, "hs", [128, D], F32) for _ in range(2)]
                hb = sb(st, "hb", [128, D], BF16)
                hT = sb(st, "hT", [128, 8, 128], BF16)
                junk = sb(st, "junk", [128, 512], F32)
                small = sb(st, "small", [128, 32], F32)
                cb = sb(st, "cb", [128, 256], BF16)
                cT = sb(st, "cT", [128, 2, 128], BF16)
                cos_t = sb(st, "cos", [128, 32], F32)
                sin_t = sb(st, "sin", [128, 32], F32)
                tf = sb(st, "tf", [128, 32], F32)
                ti = sb(st, "ti", [128, 32], I32)
                t1 = sb(st, "t1", [128, 32], F32)
                t2 = sb(st, "t2", [128, 32], F32)
                qn = sb(st, "qn", [128, D], BF16)
                qr = sb(st, "qr", [128, 1024], BF16)
                qsq = sb(st, "qsq", [128, 8], F32)
                qsh = sb(st, "qsh", [128, 8], F32)
                qnT = [sb(st, "qnT", [128, 8, 128], BF16) for _ in range(2)]
                qrT = [sb(st, "qrT", [128, 8, 128], BF16) for _ in range(2)]
                for h_ in range(8):
                    P.add("pool", lambda e, h_=h_: e.tensor_copy(out=qr[:, h_ * 128:(h_ + 1) * 128], in_=zeros_f[:, 0:128]), ["zeros_f"], ["qr"])
                P.last_w["inv"] = P.last_w.get("inv_m")
                for t in range(NT):
                    b = t % 2
                    hn = "hs%d" % b
                    dma("sp", hs[b][:], hbuf.ap()[t * 128:(t + 1) * 128, :], [("hbuf", t)], [hn], hn)
                    P.add("pool", lambda e, b=b: e.tensor_copy(out=hb[:], in_=hs[b][:]), [hn], ["hb"])
                    transpose_blocks(hb, "hb", 8, hT, "hT")
                    rope_tables(t, inv_m, 32, cos_t, sin_t, tf, ti, "q")
                    linear(hT, "hT", 8, wa, "wqa", 0, 256, 4)
                    rms_norm_ps(psf[4][:, 0:256], PSF[4], 256, g_t, "qg", cb[:], "cb", junk, small, "small", 0)
                    transpose_blocks(cb, "cb", 2, cT, "cT")
                    for n in range(3):
                        linear(cT, "cT", 2, wb, "wqb", n * 512, (n + 1) * 512, n)
                    for h in range(8):
                        c0 = h * 192
                        def seg(a, bnd):
                            bk = a // 512
                            assert (bnd - 1) // 512 == bk
                            return psf[bk][:, a - bk * 512: bnd - bk * 512], PSF[bk]
                        a = c0
                        while a < c0 + 128:
                            bnd = min(c0 + 128, (a // 512 + 1) * 512)
                            ap_, nm = seg(a, bnd)
                            P.add("act", lambda e, ap_=ap_, a=a, bnd=bnd, h=h, c0=c0: e.copy(out=qn[:, h * 128 + (a - c0): h * 128 + (bnd - c0)], in_=ap_), [nm], ["qn"])
                            a = bnd
                        a = c0
                        pieces = []
                        while a < c0 + 192:
                            bnd = min(c0 + 192, (a // 512 + 1) * 512)
                            pieces.append((a, bnd))
                            a = bnd
                        for pi, (a, bnd) in enumerate(pieces):
                            ap_, nm = seg(a, bnd)
                            sumsq(ap_, bnd - a, small[:, 8 + pi: 9 + pi], junk, nm, "sq%d" % pi)
                        if len(pieces) == 2:
                            P.add("dve", lambda e, h=h: e.tensor_tensor(out=qsq[:, h:h + 1], in0=small[:, 8:9], in1=small[:, 9:10], op=ALU.add), ["sq0", "sq1"], ["qsq"])
                        else:
                            P.add("dve", lambda e, h=h: e.tensor_copy(out=qsq[:, h:h + 1], in_=small[:, 8:9]), ["sq0"], ["qsq"])
                        r1, n1_ = seg(c0 + 128, c0 + 160)
                        r2, n2_ = seg(c0 + 160, c0 + 192)
                        rope_apply(r1, r2, qr[:, h * 128:h * 128 + 32], qr[:, h * 128 + 32:h * 128 + 64], cos_t, sin_t, 32, t1, t2, list({n1_, n2_}), "qr", "q")
                    P.add("dve", lambda e: e.tensor_scalar(out=qsh[:], in0=qsq[:], scalar1=kmax_bc[:, 0:1], scalar2=1e-30, op0=ALU.mult, op1=ALU.add), ["qsq", "kmax_bc"], ["qsh"])
                    P.add("act", lambda e: e.activation(out=qsh[:], in_=qsh[:], func=AF.Ln), ["qsh"], ["qsh"])
                    P.add("act", lambda e: e.activation(out=qsh[:], in_=qsh[:], func=AF.Exp, scale=0.5), ["qsh"], ["qsh"])
                    for h in range(8):
                        P.add("dve", lambda e, h=h: e.tensor_scalar(out=qr[:, h * 128 + 64:h * 128 + 65], in0=qsh[:, h:h + 1], scalar1=-1.0, scalar2=None, op0=ALU.mult), ["qsh", "qr"], ["qr"])
                    transpose_blocks(qn, "qn", 8, qnT[b], "qnT%d" % b)
                    transpose_blocks(qr, "qr", 8, qrT[b], "qrT%d" % b, src_off=0)
                    dma("sp", QT_d.ap()[:, :, t * 128:(t + 1) * 128].rearrange("h d t -> d h t"), qnT[b][:], ["qnT%d" % b], [("QT", t)], "qnT%d" % b)
                    dma("sp", QR_d.ap()[:, :, t * 128:(t + 1) * 128].rearrange("h d t -> d h t"), qrT[b][0:65, :, :], ["qrT%d" % b], [("QR", t)], "qrT%d" % b)
            P.barrier()
            if skip():
                return
            with contextlib.ExitStack() as st:
                krT = sb(st, "krTa", [65, S], BF16)
                dma("sp", krT[:], KR_d.ap(), [], ["krTa"], "krTa")
                causal_f = sb(st, "causal_f", [128, 128], F32)
                causal = sb(st, "causal", [128, 128], BF16)
                dma("sp", causal_f[:], CD["causal"].ap(), [], ["causal_f"], "causal_f")
                P.add("dve", lambda e: e.tensor_copy(out=causal[:], in_=causal_f[:]), ["causal_f"], ["causal"])
                KT = [sb(st, "KTh", [128, S], BF16) for _ in range(2)]
                QT = [sb(st, "QTh", [128, S], BF16) for _ in range(2)]
                QR = [sb(st, "QRh", [65, S], BF16) for _ in range(2)]
                Vh = [sb(st, "Vh", [128, NT, 130], BF16) for _ in range(2)]
                pT = [sb(st, "pTa", [128, 512], BF16) for _ in range(2)]
                ob = [sb(st, "ob", [128, 128], BF16) for _ in range(2)]
                rsum = sb(st, "rsum", [128, 2], F32)
                QG = 512 if S >= 512 else S
                NQB = QG // 128
                it = 0
                oc = 0
                for h in range(8):
                    hb_ = h % 2
                    dma("sp", KT[hb_][:], KT_d.ap()[h], [], ["KT%d" % hb_], "KT%d" % hb_)
                    dma("sp", QT[hb_][:], QT_d.ap()[h], [], ["QT%d" % hb_], "QT%d" % hb_)
                    dma("sp", QR[hb_][:], QR_d.ap()[h], [], ["QR%d" % hb_], "QR%d" % hb_)
                    dma("sp", Vh[hb_][:], V_d.ap()[:, h, :].rearrange("(t p) c -> p t c", p=128), [], ["Vh%d" % hb_], "Vh%d" % hb_)
                    for qg in range(S // QG):
                        nkb = (qg + 1) * NQB
                        for kb in range(nkb):
                            sbk = 4 + it % 2
                            pb = it % 2
                            it += 1
                            P.add("pe", lambda e, kb=kb, qg=qg, sbk=sbk, hb_=hb_: e.matmul(psf[sbk][:, 0:QG], lhsT=KT[hb_][:, kb * 128:(kb + 1) * 128],
                                                                                        rhs=QT[hb_][:, qg * QG:(qg + 1) * QG], start=True, stop=False),
                                  ["KT%d" % hb_, "QT%d" % hb_], [PSF[sbk]])
                            P.add("pe", lambda e, kb=kb, qg=qg, sbk=sbk, hb_=hb_: e.matmul(psf[sbk][:, 0:QG], lhsT=krT[:, kb * 128:(kb + 1) * 128],
                                                                                        rhs=QR[hb_][:, qg * QG:(qg + 1) * QG], start=False, stop=True),
                                  ["krTa", "QR%d" % hb_], [PSF[sbk]])
                            P.add("act", lambda e, sbk=sbk, pb=pb: e.activation(out=pT[pb][:, 0:QG], in_=psf[sbk][:, 0:QG], func=AF.Exp, scale=SCALE),
                                  [PSF[sbk]], ["pTa%d" % pb])
                            for qb in range(NQB):
                                gq = qg * NQB + qb
                                if kb > gq:
                                    continue
                                if kb == gq:
                                    P.add("dve", lambda e, pb=pb, qb=qb: e.tensor_tensor(out=pT[pb][:, qb * 128:(qb + 1) * 128], in0=pT[pb][:, qb * 128:(qb + 1) * 128],
                                                                                         in1=causal[:], op=ALU.mult), ["pTa%d" % pb, "causal"], ["pTa%d" % pb])
                                P.add("pe", lambda e, pb=pb, qb=qb, kb=kb, gq=gq, hb_=hb_: e.matmul(psf[qb][:, 0:129], lhsT=pT[pb][:, qb * 128:(qb + 1) * 128],
                                                                                                  rhs=Vh[hb_][:, kb, 0:129], start=(kb == 0), stop=(kb == gq)),
                                      ["pTa%d" % pb, "Vh%d" % hb_], [PSF[qb]])
                                if kb == gq:
                                    o_ = oc % 2
                                    oc += 1
                                    P.add("dve", lambda e, qb=qb: e.reciprocal(out=rsum[:, 0:1], in_=psf[qb][:, 128:129]), [PSF[qb]], ["rsum"])
                                    P.add("dve", lambda e, qb=qb, o_=o_: e.tensor_scalar(out=ob[o_][:], in0=psf[qb][:, 0:128], scalar1=rsum[:, 0:1], scalar2=None, op0=ALU.mult),
                                          [PSF[qb], "rsum"], ["ob%d" % o_])
                                    dma("sp", O_d.ap()[gq * 128:(gq + 1) * 128, h * 128:(h + 1) * 128], ob[o_][:], ["ob%d" % o_], [("O", gq)], "ob%d" % o_)
            P.barrier()
            if skip():
                return
            with contextlib.ExitStack() as st:
                wo = sb(st, "wo", [128, 8, D], BF16)
                stage = [sb(st, "wst", [128, 1024], F32) for _ in range(2)]
                load_w_bf16(st, wo, "wo", W["mla_w_o"].ap()[j], 8, D, stage, 2)
                ln_g = sb(st, "ln_g", [128, D], F32)
                ln_b = sb(st, "ln_b", [128, D], F32)
                dma("sp", ln_g[:], bc_row(W["ln_mix_g"], l * D, D), [], ["ln_g"], "ln_g")
                dma("sp", ln_b[:], bc_row(W["ln_mix_b"], l * D, D), [], ["ln_b"], "ln_b")
                hs = [sb(st, "hs", [128, D], F32) for _ in range(2)]
                orow = [sb(st, "orow", [128, D], BF16) for _ in range(2)]
                oT = sb(st, "oT", [128, 8, 128], BF16)
                z = sb(st, "z", [128, D], F32)
                h1 = [sb(st, "h1", [128, D], F32) for _ in range(2)]
                small = sb(st, "small", [128, 32], F32)
                for t in range(NT):
                    b = t % 2
                    hn = "hs%d" % b
                    dma("sp", hs[b][:], hbuf.ap()[t * 128:(t + 1) * 128, :], [("hbuf", t)], [hn], hn)
                    dma("sp", orow[b][:], O_d.ap()[t * 128:(t + 1) * 128, :], [], ["orow%d" % b], "orow%d" % b)
                    transpose_blocks(orow[b], "orow%d" % b, 8, oT, "oT")
                    for nh in range(2):
                        linear(oT, "oT", 8, wo, "wo", nh * 512, (nh + 1) * 512, nh)
                        P.add("dve", lambda e, nh=nh, b=b: e.scalar_tensor_tensor(out=z[:, nh * 512:(nh + 1) * 512], in0=hs[b][:, nh * 512:(nh + 1) * 512],
                                                                                  scalar=ALPHA, in1=psf[nh][:, :], op0=ALU.mult, op1=ALU.add), [hn, PSF[nh]], ["z"])
                    layer_norm(z, "z", h1[b], "h1%d" % b, ln_g, ln_b, ["ln_g", "ln_b"], small, "small")
                    dma("sp", hbuf.ap()[t * 128:(t + 1) * 128, :], h1[b][:], ["h1%d" % b], [("hbuf", t)], "h1%d" % b)
            P.barrier()

        fin = []
        for l in range(DEPTH):
            if l < NA:
                retention_layer(l)
            else:
                if l == NA:
                    mla_kv()
                mla_layer(l)
            moe_ple(l, l == DEPTH - 1)
        if LIMIT < 999:
            fin.append(dma('sp', y_d.ap(), hbuf.ap(), [], [], 'ydbg'))
            if dbg:
                fin.append(dma('sp', dq_d.ap(), qkvg_d.ap(), [], [], 'ydbg2'))
        P.emit(final_ops=fin)
    return nc, consts_np


_CACHE = {}


def run(inputs, S, E, DEPTH, NA, CAP, cores, dbg=False):
    key = (S, E, DEPTH, NA, CAP, dbg)
    if key not in _CACHE:
        _CACHE[key] = build(S, E, DEPTH, NA, CAP, dbg)
    nc, consts_np = _CACHE[key]
    NT = S // 128
    f32 = lambda a: np.ascontiguousarray(np.asarray(a), dtype=np.float32)
    shared = {}
    for k in ("ret_w_in", "ret_gn_g", "ret_gn_b", "ret_w_out", "mla_w_kv_a", "mla_w_kv_b", "mla_w_q_a", "mla_q_norm_g",
              "mla_w_q_b", "mla_w_o", "ln_mix_g", "ln_mix_b", "ln_ffn_g", "ln_ffn_b", "moe_w_router", "moe_b_router",
              "moe_w_gate_up", "moe_b_gate_up", "moe_w_down", "moe_b_down", "ple_w_gate", "ple_w_proj"):
        shared[k] = f32(inputs[k])
    shared["mla_kv_norm_g"] = f32(inputs["mla_kv_norm_g"]).reshape(1, 256)
    bgu_ = shared["moe_b_gate_up"]
    shared["moe_b_gate_up"] = np.ascontiguousarray(bgu_.reshape(bgu_.shape[0], bgu_.shape[1], 16, 128).transpose(0, 1, 3, 2))
    for k, v in consts_np.items():
        shared["c_" + k] = f32(v)
    x = f32(inputs["x"])
    p = f32(inputs["p"])
    pos = np.asarray(inputs["positions"]).astype(np.int32)
    in_maps = []
    for c in range(cores):
        m = dict(shared)
        m["x"] = x[c]
        m["p"] = np.ascontiguousarray(p[:, c])
        m["pos_tm"] = np.ascontiguousarray(pos[c].reshape(NT, 128).T)
        in_maps.append(m)
    res = run_bass_kernel_spmd(nc, in_maps, core_ids=list(range(cores)))
    return res.results


def kernel(**inputs):
    S, E, DEPTH, NA = 8192, 32, 4, 2
    CAP = 1536
    res = run(inputs, S, E, DEPTH, NA, CAP, cores=4)
    return np.stack([r["y"] for r in res], axis=0).astype(np.float32)
```

```python
import contextlib
import numpy as np
import concourse.bass as bass
import concourse.mybir as mybir
from concourse.bass_utils import run_bass_kernel_spmd

F32 = mybir.dt.float32
BF16 = mybir.dt.bfloat16
I32 = mybir.dt.int32
U32 = mybir.dt.uint32
AF = mybir.ActivationFunctionType
ALU = mybir.AluOpType

D = 1024
SEM_ROT = 30000
DN_EPS = 1e-5


import types


def _snap(fn):
    if fn.__closure__ is None:
        return fn
    cells = []
    for c in fn.__closure__:
        try:
            cells.append(types.CellType(c.cell_contents))
        except ValueError:
            cells.append(c)
    g = types.FunctionType(fn.__code__, fn.__globals__, fn.__name__, fn.__defaults__, tuple(cells))
    g.__kwdefaults__ = fn.__kwdefaults__
    return g


class Op:
    __slots__ = ("eng", "fn", "deps", "signal", "event", "dma_key")

    def __init__(self, eng, fn, deps, dma_key):
        self.eng = eng
        self.fn = fn
        self.deps = deps
        self.signal = dma_key is not None
        self.event = None
        self.dma_key = dma_key


class Prog:
    ENGS = ("pe", "act", "dve", "pool", "sp")

    def __init__(self, nc):
        self.nc = nc
        self.ops = {e: [] for e in self.ENGS}
        self.last_w = {}
        self.readers = {}
        self.all_ops = []
        self.pending = {}
        self.last_dma = {}

    def add(self, eng, fn, reads=(), writes=(), dma_key=None):
        pr = [r for r in reads if isinstance(r, str) and r.startswith(("psf", "psb"))]
        if pr:
            reads = [r for r in reads if r not in pr]
            writes = list(writes) + [r for r in pr if r not in writes]
        deps = []
        for r in reads:
            w = self.last_w.get(r)
            if w is not None:
                deps.append(w)
        for w_ in writes:
            w = self.last_w.get(w_)
            if w is not None:
                deps.append(w)
            deps.extend(self.readers.get(w_, ()))
        if eng in self.pending:
            deps.extend(self.pending.pop(eng))
        op = Op(eng, _snap(fn), deps, dma_key)
        for r in reads:
            self.readers.setdefault(r, []).append(op)
        for w_ in writes:
            self.last_w[w_] = op
            self.readers[w_] = []
        self.ops[eng].append(op)
        self.all_ops.append(op)
        if dma_key is not None:
            self.last_dma[dma_key] = op
        return op

    def barrier(self):
        deps = [self.ops[e][-1] for e in self.ENGS if self.ops[e]]
        deps += list(self.last_dma.values())
        for e in self.ENGS:
            self.pending[e] = list(deps) + self.pending.get(e, [])
        self.last_w = {}
        self.readers = {}

    def emit(self, final_ops=()):
        nc = self.nc
        for op in self.all_ops:
            for d in op.deps:
                if d is op:
                    continue
                if d.eng == "pe" and op.eng == "pe" and d.dma_key is None and op.dma_key is None:
                    continue
                d.signal = True
        for op in final_ops:
            op.signal = True
        eng_cnt = {e: 0 for e in self.ENGS}
        key_cnt = {}
        names = []
        for op in self.all_ops:
            if op.dma_key is not None:
                k = ("dma", op.dma_key)
                key_cnt[k] = key_cnt.get(k, 0) + 16
                op.event = (k, key_cnt[k])
                if k not in names:
                    names.append(k)
        for e in self.ENGS:
            for op in self.ops[e]:
                if op.dma_key is not None:
                    continue
                elif op.signal:
                    eng_cnt[e] += 1
                    c = eng_cnt[e]
                    k = ("eng", e, (c - 1) // SEM_ROT)
                    op.event = (k, (c - 1) % SEM_ROT + 1)
                else:
                    continue
                if op.event[0] not in names:
                    names.append(op.event[0])
        self.n_sems = len(names)
        with contextlib.ExitStack() as st:
            sems = {}
            for i, k in enumerate(names):
                sems[k] = st.enter_context(nc.semaphore("s%d" % i))
            block = st.enter_context(nc.Block())
            engmap = {"pe": block.tensor, "act": block.scalar, "dve": block.vector,
                      "pool": block.gpsimd, "sp": block.sync}
            for e in self.ENGS:
                ops = self.ops[e]
                if not ops and e != "sp":
                    continue

                def body(eng, ops=ops, e=e):
                    known = {}
                    for op in ops:
                        need = {}
                        for d in op.deps:
                            if d is op or d.event is None:
                                continue
                            if d.eng == "pe" and e == "pe" and d.dma_key is None and op.dma_key is None:
                                continue
                            k, v = d.event
                            if need.get(k, 0) < v:
                                need[k] = v
                        for k, v in need.items():
                            if known.get(k, 0) < v:
                                eng.wait_ge(sems[k], v)
                                known[k] = v
                        ins = op.fn(eng)
                        if op.event is not None:
                            ins.then_inc(sems[op.event[0]], 16 if op.dma_key is not None else 1)
                    if e == "sp":
                        for op in final_ops:
                            k, v = op.event
                            if known.get(k, 0) < v:
                                eng.wait_ge(sems[k], v)
                                known[k] = v
                engmap[e](body)


def model_consts(E):
    H, dk, C = 4, 256, 128
    log_g = np.log(1.0 - 2.0 ** (-5.0 - np.arange(H, dtype=np.float64)))
    idx = np.arange(C, dtype=np.float64)
    diff = idx[:, None] - idx[None, :]
    intra = np.where(diff >= 0, np.exp(log_g[:, None, None] * np.maximum(diff, 0.0)), 0.0)
    maskT = np.transpose(intra, (0, 2, 1)) * dk ** -0.5
    decay_q = np.exp(log_g[None, :] * (idx[:, None] + 1.0))
    decay_k = np.exp(log_g[None, :] * (C - 1.0 - idx[:, None])) * dk ** -0.5
    decay_chunk = np.exp(log_g * C)
    inv_r = (10000.0 ** (-np.arange(0, 256, 2, dtype=np.float32) / np.float32(256))).astype(np.float32)
    inv_m = (10000.0 ** (-np.arange(0, 64, 2, dtype=np.float32) / np.float32(64))).astype(np.float32)
    c = {}
    c["ident"] = np.eye(128, dtype=np.float32)
    c["maskT"] = np.ascontiguousarray(np.transpose(maskT, (1, 0, 2))).astype(np.float32)
    c["dq"] = decay_q.astype(np.float32)
    c["dk"] = decay_k.astype(np.float32)
    c["inv_r"] = np.tile(inv_r[None, :], (128, 1)).astype(np.float32)
    c["inv_m"] = np.tile(inv_m[None, :], (128, 1)).astype(np.float32)
    c["causal"] = (np.arange(128)[:, None] <= np.arange(128)[None, :]).astype(np.float32)
    c["tri"] = (np.arange(128)[:, None] < np.arange(128)[None, :]).astype(np.float32)
    c["iota_e"] = np.tile(np.arange(E, dtype=np.float32)[None, :], (128, 1))
    return c, [float(v) for v in decay_chunk]


CONST_SHAPES = lambda E: {"ident": [128, 128], "maskT": [128, 4, 128], "dq": [128, 4], "dk": [128, 4],
                          "inv_r": [128, 128], "inv_m": [128, 32], "causal": [128, 128], "tri": [128, 128],
                          "iota_e": [128, E]}


def build(S, E, DEPTH, NA, CAP, dbg=False):
    import os
    LIMIT = int(os.environ.get('KLIMIT', '999'))
    PH = [0]

    def skip():
        PH[0] += 1
        return PH[0] > LIMIT
    NT = S // 128
    NB = DEPTH - NA
    TOPK = 4
    ALPHA = float((2 * DEPTH) ** 0.25)
    SCALE = float(192 ** -0.5)
    NSLOT = E * CAP
    CT = CAP // 128
    consts_np, dchunk = model_consts(E)

    nc = bass.Bass("TRN2", target_bir_lowering=False)

    def din(name, shape, dt=F32):
        return nc.dram_tensor(name, list(shape), dt, kind="ExternalInput")

    x_d = din("x", [S, D])
    p_d = din("p", [DEPTH, S, 256])
    pos_d = din("pos_tm", [128, NT], I32)
    W = {}
    W["ret_w_in"] = din("ret_w_in", [NA, D, 6144])
    W["ret_gn_g"] = din("ret_gn_g", [NA, 2048])
    W["ret_gn_b"] = din("ret_gn_b", [NA, 2048])
    W["ret_w_out"] = din("ret_w_out", [NA, 2048, D])
    W["mla_w_kv_a"] = din("mla_w_kv_a", [D, 320])
    W["mla_kv_norm_g"] = din("mla_kv_norm_g", [1, 256])
    W["mla_w_kv_b"] = din("mla_w_kv_b", [256, 2048])
    W["mla_w_q_a"] = din("mla_w_q_a", [NB, D, 256])
    W["mla_q_norm_g"] = din("mla_q_norm_g", [NB, 256])
    W["mla_w_q_b"] = din("mla_w_q_b", [NB, 256, 1536])
    W["mla_w_o"] = din("mla_w_o", [NB, D, D])
    for n in ("ln_mix_g", "ln_mix_b", "ln_ffn_g", "ln_ffn_b"):
        W[n] = din(n, [DEPTH, D])
    W["moe_w_router"] = din("moe_w_router", [DEPTH, D, E])
    W["moe_b_router"] = din("moe_b_router", [DEPTH, E])
    W["moe_w_gate_up"] = din("moe_w_gate_up", [DEPTH, E, D, 2048])
    W["moe_b_gate_up"] = din("moe_b_gate_up", [DEPTH, E, 128, 16])
    W["moe_w_down"] = din("moe_w_down", [DEPTH, E, D, D])
    W["moe_b_down"] = din("moe_b_down", [DEPTH, E, D])
    W["ple_w_gate"] = din("ple_w_gate", [DEPTH, D, D])
    W["ple_w_proj"] = din("ple_w_proj", [DEPTH, 256, D])
    CD = {k: din("c_" + k, shp) for k, shp in CONST_SHAPES(E).items()}
    y_d = nc.dram_tensor("y", [S, D], F32, kind="ExternalOutput")
    dbg_d = nc.dram_tensor("dbg", [DEPTH, S, D], F32, kind="ExternalOutput") if dbg else None
    dq_d = nc.dram_tensor("dq", [S, 6144], BF16, kind="ExternalOutput") if dbg else None
    dq2_d = nc.dram_tensor("dq2", [S, 6144], BF16, kind="ExternalOutput") if dbg else None

    hbuf = nc.dram_tensor("hbuf", [S, D], F32)
    qkvg_d = nc.dram_tensor("qkvg", [S, 6144], BF16)
    xs_d = nc.dram_tensor("xs", [NSLOT, D], BF16)
    ys_d = nc.dram_tensor("ys", [NSLOT, D], F32)
    KT_d = nc.dram_tensor("KT", [8, 128, S], BF16)
    KR_d = nc.dram_tensor("KR", [65, S], BF16)
    V_d = nc.dram_tensor("Vx", [S, 8, 130], BF16)
    QT_d = nc.dram_tensor("QT", [8, 128, S], BF16)
    QR_d = nc.dram_tensor("QR", [8, 65, S], BF16)
    O_d = nc.dram_tensor("Oa", [S, D], BF16)

    P = Prog(nc)
    uid = [0]

    def bc_row(handle, row_off, n):
        return bass.AP(handle, row_off, [[0, 128], [1, n]])

    with contextlib.ExitStack() as root:
        sbstate = {"cur": 0, "persist": 0, "st": None}
        DTB = {F32: 4, BF16: 2, I32: 4, U32: 4}

        def sb(st, name, shape, dt):
            uid[0] += 1
            nb = DTB[dt]
            for d_ in shape[1:]:
                nb *= d_
            nb = (nb + 63) // 64 * 64
            if True:
                return nc.alloc_sbuf_tensor("%s_%d" % (name, uid[0]), list(shape), dt) if st is root else st.enter_context(nc.sbuf_tensor("%s_%d" % (name, uid[0]), list(shape), dt))
            if st is root:
                assert sbstate["st"] is None
                off = sbstate["persist"]
                sbstate["persist"] += nb
            else:
                if sbstate["st"] is not st:
                    sbstate["st"] = st
                    sbstate["cur"] = sbstate["persist"]
                off = sbstate["cur"]
                sbstate["cur"] += nb
                assert sbstate["cur"] <= 190 * 1024, ("SBUF overflow", name, sbstate["cur"])
            return nc.alloc_sbuf_tensor_at("%s_%d" % (name, uid[0]), list(shape), dt, offset=off)

        psf = [root.enter_context(nc.psum_tensor("psf%d" % i, [128, 512], F32)) for i in range(6)]
        psb = [root.enter_context(nc.psum_tensor("psb%d" % i, [128, 1024], BF16)) for i in range(2)]
        PSF = ["psf%d" % i for i in range(6)]
        PSB = ["psb%d" % i for i in range(2)]

        ident_f = sb(root, "ident_f", [128, 128], F32)
        ident_b = sb(root, "ident_b", [128, 128], BF16)
        ones_b = sb(root, "ones_b", [128, 128], BF16)
        ones_f = sb(root, "ones_f", [128, 128], F32)
        eps_ln = sb(root, "eps_ln", [128, 1], F32)
        eps_6 = sb(root, "eps_6", [128, 1], F32)
        posf = sb(root, "posf", [128, NT], F32)
        posi = sb(root, "posi", [128, NT], I32)
        inv_r = sb(root, "inv_r", [128, 128], F32)
        inv_m = sb(root, "inv_m", [128, 32], F32)
        slots_i = sb(root, "slots_i", [128, NT, TOPK], I32)
        gates_s = sb(root, "gates_s", [128, NT, TOPK], F32)

        def dma(eng, out, in_, reads, writes, key):
            return P.add(eng, lambda e: e.dma_start(out=out, in_=in_), reads, writes, dma_key=key)

        dma("sp", ident_f[:], CD["ident"].ap(), [], ["ident_f"], "ident_f")
        dma("sp", posi[:], pos_d.ap(), [], ["posi"], "posi")
        dma("sp", inv_r[:], CD["inv_r"].ap(), [], ["inv_r"], "inv_r")
        dma("sp", inv_m[:], CD["inv_m"].ap(), [], ["inv_m"], "inv_m")
        P.add("dve", lambda e: e.tensor_copy(out=ident_b[:], in_=ident_f[:]), ["ident_f"], ["ident_b"])
        P.add("dve", lambda e: e.tensor_copy(out=posf[:], in_=posi[:]), ["posi"], ["posf"])
        zeros_f = sb(root, "zeros_f", [128, 1024], F32)
        P.add("dve", lambda e: e.memset(zeros_f[:], 0.0), [], ["zeros_f"])
        P.add("pool", lambda e: e.memset(ones_f[:], 1.0), [], ["ones_f"])
        P.add("pool", lambda e: e.tensor_copy(out=ones_b[:], in_=ones_f[:]), ["ones_f"], ["ones_b"])
        P.add("pool", lambda e: e.memset(eps_ln[:], DN_EPS), [], ["eps_ln"])
        P.add("pool", lambda e: e.memset(eps_6[:], 1e-6), [], ["eps_6"])
        dma("sp", hbuf.ap(), x_d.ap(), [], ["hbuf"], "x2h")
        P.barrier()

        rr = {"cast": 0, "ps": 0}

        def load_w_bf16(st, dst, dst_name, src2d, KC, N, stage, nstage):
            CH = stage[0].shape[1]
            i = 0
            for kc in range(KC):
                for n0 in range(0, N, CH):
                    n1 = min(N, n0 + CH)
                    b = rr["cast"] % nstage
                    rr["cast"] += 1
                    sname = "wst%d" % b
                    dma("sp", stage[b][:, 0:n1 - n0], src2d[kc * 128:(kc + 1) * 128, n0:n1], [], [sname], sname)
                    eng = "pool" if (rr["cast"] % 2) else "act"
                    if eng == "pool":
                        P.add("pool", lambda e, b=b, kc=kc, n0=n0, n1=n1: e.tensor_copy(out=dst[:, kc, n0:n1], in_=stage[b][:, 0:n1 - n0]),
                              [sname], [dst_name])
                    else:
                        P.add("act", lambda e, b=b, kc=kc, n0=n0, n1=n1: e.copy(out=dst[:, kc, n0:n1], in_=stage[b][:, 0:n1 - n0]),
                              [sname], [dst_name])
                    i += 1

        def transpose_blocks(src, src_name, nblk, dst, dst_name, rows=128, blkw=128, src_off=0):
            for g0 in range(0, nblk, 8):
                g1 = min(nblk, g0 + 8)
                pb = rr["ps"] % 2
                rr["ps"] += 1
                for j in range(g0, g1):
                    P.add("pe", lambda e, j=j, pb=pb, g0=g0: e.transpose(
                        out=psb[pb][0:blkw, (j - g0) * 128:(j - g0 + 1) * 128],
                        in_=src[:, src_off + j * blkw: src_off + (j + 1) * blkw], identity=ident_b[:]),
                        [src_name, "ident_b"], [PSB[pb]])
                P.add("act", lambda e, pb=pb, g0=g0, g1=g1: e.copy(
                    out=dst[0:blkw, g0:g1, :], in_=psb[pb][0:blkw, 0:(g1 - g0) * 128].rearrange("p (j t) -> p j t", t=128)),
                    [PSB[pb]], [dst_name])

        def rstd_from(var_ap, eps_tile, out_ap, tmp_ap, names_r, name_w, scale=1.0):
            epsv = DN_EPS if eps_tile is eps_ln else 1e-6
            P.add("dve", lambda e: e.tensor_scalar(out=tmp_ap, in0=var_ap, scalar1=float(scale), scalar2=float(epsv), op0=ALU.mult, op1=ALU.add),
                  list(names_r), [name_w + "_t"])
            P.add("act", lambda e: e.activation(out=tmp_ap, in_=tmp_ap, func=AF.Ln), [name_w + "_t"], [name_w + "_t"])
            P.add("act", lambda e: e.activation(out=out_ap, in_=tmp_ap, func=AF.Exp, scale=-0.5),
                  [name_w + "_t"], [name_w])

        def layer_norm(z, zname, out, oname, g_t, b_t, gb_names, small, sname):
            for c in range(2):
                P.add("dve", lambda e, c=c: e.bn_stats(out=small[:, c * 6:(c + 1) * 6], in_=z[:, c * 512:(c + 1) * 512]),
                      [zname], [sname])
            P.add("dve", lambda e: e.bn_aggr(out=small[:, 12:14], in_=small[:, 0:12].rearrange("p (c s) -> p c s", s=6)),
                  [sname], [sname])
            rstd_from(small[:, 13:14], eps_ln, small[:, 15:16], small[:, 14:15], [sname], sname + "r")
            P.add("dve", lambda e: e.tensor_scalar(out=out[:], in0=z[:], scalar1=small[:, 12:13], scalar2=small[:, 15:16],
                                                   op0=ALU.subtract, op1=ALU.mult), [zname, sname, sname + "r"], [oname])
            P.add("pool", lambda e: e.tensor_tensor(out=out[:], in0=out[:], in1=g_t[:], op=ALU.mult), [oname, gb_names[0]], [oname])
            P.add("pool", lambda e: e.tensor_tensor(out=out[:], in0=out[:], in1=b_t[:], op=ALU.add), [oname, gb_names[1]], [oname])

        def rope_tables(t, inv_t, nf, cos_t, sin_t, tmpf, tmpi, tag):
            for which, dst, shift in (("s", sin_t, 0.0), ("c", cos_t, float(np.pi / 2))):
                nm = tag + which
                P.add("dve", lambda e, dst=dst, shift=shift: e.tensor_scalar(
                    out=dst[:, 0:nf], in0=inv_t[:, 0:nf], scalar1=posf[:, t:t + 1], scalar2=shift, op0=ALU.mult, op1=ALU.add),
                    ["posf", "inv"], [nm])
                P.add("dve", lambda e, dst=dst: e.tensor_scalar(out=tmpf[:, 0:nf], in0=dst[:, 0:nf], scalar1=float(1 / (2 * np.pi)),
                                                                scalar2=None, op0=ALU.mult), [nm], [tag + "tf"])
                P.add("dve", lambda e: e.tensor_copy(out=tmpi[:, 0:nf], in_=tmpf[:, 0:nf]), [tag + "tf"], [tag + "ti"])
                P.add("dve", lambda e: e.tensor_copy(out=tmpf[:, 0:nf], in_=tmpi[:, 0:nf]), [tag + "ti"], [tag + "tf"])
                P.add("dve", lambda e, dst=dst: e.scalar_tensor_tensor(out=dst[:, 0:nf], in0=tmpf[:, 0:nf], scalar=float(-2 * np.pi),
                                                                       in1=dst[:, 0:nf], op0=ALU.mult, op1=ALU.add), [tag + "tf", nm], [nm])
                P.add("dve", lambda e, dst=dst: e.tensor_scalar(out=tmpf[:, 0:nf], in0=dst[:, 0:nf], scalar1=float(np.pi), scalar2=float(-2 * np.pi),
                                                                op0=ALU.is_gt, op1=ALU.mult), [nm], [tag + "tf"])
                P.add("dve", lambda e, dst=dst: e.tensor_tensor(out=dst[:, 0:nf], in0=dst[:, 0:nf], in1=tmpf[:, 0:nf], op=ALU.add), [nm, tag + "tf"], [nm])
                P.add("dve", lambda e, dst=dst: e.tensor_scalar(out=tmpf[:, 0:nf], in0=dst[:, 0:nf], scalar1=float(-np.pi), scalar2=float(2 * np.pi),
                                                                op0=ALU.is_lt, op1=ALU.mult), [nm], [tag + "tf"])
                P.add("dve", lambda e, dst=dst: e.tensor_tensor(out=dst[:, 0:nf], in0=dst[:, 0:nf], in1=tmpf[:, 0:nf], op=ALU.add), [nm, tag + "tf"], [nm])
                P.add("act", lambda e, dst=dst: e.activation(out=dst[:, 0:nf], in_=dst[:, 0:nf], func=AF.Sin), [nm], [nm])

        def rope_apply(x1, x2, o1, o2, cos_t, sin_t, nf, t1, t2, rnames, wname, tag):
            P.add("dve", lambda e: e.tensor_tensor(out=t1[:, 0:nf], in0=x1, in1=cos_t[:, 0:nf], op=ALU.mult), rnames + [tag + "c"], [tag + "t1"])
            P.add("dve", lambda e: e.tensor_tensor(out=t2[:, 0:nf], in0=x2, in1=sin_t[:, 0:nf], op=ALU.mult), rnames + [tag + "s"], [tag + "t2"])
            P.add("pool", lambda e: e.tensor_tensor(out=o1, in0=t1[:, 0:nf], in1=t2[:, 0:nf], op=ALU.subtract), [tag + "t1", tag + "t2"], [wname])
            P.add("dve", lambda e: e.tensor_tensor(out=t1[:, 0:nf], in0=x1, in1=sin_t[:, 0:nf], op=ALU.mult), rnames + [tag + "s"], [tag + "t1"])
            P.add("dve", lambda e: e.tensor_tensor(out=t2[:, 0:nf], in0=x2, in1=cos_t[:, 0:nf], op=ALU.mult), rnames + [tag + "c"], [tag + "t2"])
            P.add("pool", lambda e: e.tensor_tensor(out=o2, in0=t1[:, 0:nf], in1=t2[:, 0:nf], op=ALU.add), [tag + "t1", tag + "t2"], [wname])

        def linear(xT, xT_name, KC, w, w_name, n0, n1, bank, extra=None):
            for kc in range(KC):
                P.add("pe", lambda e, kc=kc: e.matmul(psf[bank][:, 0:n1 - n0], lhsT=xT[:, kc, :], rhs=w[:, kc, n0:n1],
                                                     start=(kc == 0), stop=(kc == KC - 1 and extra is None)),
                      [xT_name, w_name], [PSF[bank]])

        def retention_layer(l):
            if skip():
                return
            with contextlib.ExitStack() as st:
                win = sb(st, "win", [128, 8, 6144], BF16)
                stage = [sb(st, "wst", [128, 2048], F32) for _ in range(2)]
                load_w_bf16(st, win, "win", W["ret_w_in"].ap()[l], 8, 6144, stage, 2)
                hs = [sb(st, "hs", [128, D], F32) for _ in range(2)]
                hb = sb(st, "hb", [128, D], BF16)
                hT = sb(st, "hT", [128, 8, 128], BF16)
                row = [sb(st, "row", [128, 6144], BF16) for _ in range(2)]
                cos_t = sb(st, "cos", [128, 128], F32)
                sin_t = sb(st, "sin", [128, 128], F32)
                tf = sb(st, "tf", [128, 128], F32)
                ti = sb(st, "ti", [128, 128], I32)
                t1 = sb(st, "t1", [128, 128], F32)
                t2 = sb(st, "t2", [128, 128], F32)
                P.last_w["inv"] = P.last_w.get("inv_r")
                for t in range(NT):
                    b = t % 2
                    dma("sp", hs[b][:], hbuf.ap()[t * 128:(t + 1) * 128, :], [("hbuf", t)], ["hs%d" % b], "hs%d" % b)
                    P.add("pool", lambda e, b=b: e.tensor_copy(out=hb[:], in_=hs[b][:]), ["hs%d" % b], ["hb"])
                    transpose_blocks(hb, "hb", 8, hT, "hT")
                    rope_tables(t, inv_r, 128, cos_t, sin_t, tf, ti, "rp")
                    rw = "row%d" % b
                    for n in range(12):
                        bank = n % 4
                        linear(hT, "hT", 8, win, "win", n * 512, (n + 1) * 512, bank)
                        if n < 4:
                            for hh in range(2):
                                c0 = hh * 256
                                rope_apply(psf[bank][:, c0:c0 + 128], psf[bank][:, c0 + 128:c0 + 256],
                                           row[b][:, n * 512 + c0: n * 512 + c0 + 128], row[b][:, n * 512 + c0 + 128: n * 512 + c0 + 256],
                                           cos_t, sin_t, 128, t1, t2, [PSF[bank]], rw, "rp")
                        elif n < 8:
                            P.add("act", lambda e, n=n, b=b, bank=bank: e.copy(out=row[b][:, n * 512:(n + 1) * 512], in_=psf[bank][:, :]),
                                  [PSF[bank]], [rw])
                        else:
                            P.add("act", lambda e, n=n, b=b, bank=bank: e.activation(out=row[b][:, n * 512:(n + 1) * 512], in_=psf[bank][:, :], func=AF.Silu),
                                  [PSF[bank]], [rw])
                    dma("act", qkvg_d.ap()[t * 128:(t + 1) * 128, :], row[b][:], [rw], [("qkvg", t)], rw)
            P.barrier()
            if dbg and l == 0:
                dma("sp", dq2_d.ap(), qkvg_d.ap(), [], [], "dq2dbg")
                P.barrier()
            if skip():
                return
            with contextlib.ExitStack() as st:
                wout = sb(st, "wout", [128, 16, D], BF16)
                stage = [sb(st, "wst", [128, 1024], F32) for _ in range(2)]
                load_w_bf16(st, wout, "wout", W["ret_w_out"].ap()[l], 16, D, stage, 2)
                gn_g = sb(st, "gn_g", [128, 2048], F32)
                gn_b = sb(st, "gn_b", [128, 2048], F32)
                ln_g = sb(st, "ln_g", [128, D], F32)
                ln_b = sb(st, "ln_b", [128, D], F32)
                dma("sp", gn_g[:], bc_row(W["ret_gn_g"], l * 2048, 2048), [], ["gn_g"], "gn_g")
                dma("sp", gn_b[:], bc_row(W["ret_gn_b"], l * 2048, 2048), [], ["gn_b"], "gn_b")
                dma("sp", ln_g[:], bc_row(W["ln_mix_g"], l * D, D), [], ["ln_g"], "ln_g")
                dma("sp", ln_b[:], bc_row(W["ln_mix_b"], l * D, D), [], ["ln_b"], "ln_b")
                maskT = sb(st, "maskT", [128, 4, 128], F32)
                dq = sb(st, "dq", [128, 4], F32)
                dkc = sb(st, "dkc", [128, 4], F32)
                dma("sp", maskT[:], CD["maskT"].ap(), [], ["maskT"], "maskT")
                dma("sp", dq[:], CD["dq"].ap(), [], ["dq"], "dq")
                dma("sp", dkc[:], CD["dk"].ap(), [], ["dkc"], "dkc")
                Sf = sb(st, "Sf", [128, 8, 512], F32)
                Sb = sb(st, "Sb", [128, 8, 512], BF16)
                P.add("dve", lambda e: e.memset(Sf[:], 0.0), [], ["Sf"])
                for i_ in range(8):
                    P.add("dve", lambda e, i_=i_: e.tensor_copy(out=Sb[:, i_, :], in_=zeros_f[:, 0:512]), ["zeros_f"], ["Sb"])
                row = [sb(st, "row", [128, 6144], BF16) for _ in range(2)]
                hs = [sb(st, "hs", [128, D], F32) for _ in range(2)]
                qd = sb(st, "qd", [128, D], BF16)
                kd = sb(st, "kd", [128, D], BF16)
                qT = sb(st, "qT", [128, 8, 128], BF16)
                qdT = sb(st, "qdT", [128, 8, 128], BF16)
                kT = sb(st, "kT", [128, 8, 128], BF16)
                innerT = sb(st, "innerT", [128, 128], BF16)
                yn = sb(st, "yn", [128, 512], F32)
                yg = sb(st, "yg", [128, 2048], BF16)
                ygT = sb(st, "ygT", [128, 16, 128], BF16)
                z = sb(st, "z", [128, D], F32)
                h1 = [sb(st, "h1", [128, D], F32) for _ in range(2)]
                small = sb(st, "small", [128, 32], F32)
                gsm = sb(st, "gsm", [128, 32], F32)
                ydb = sb(st, "ydb", [128, 512], F32)
                for t in range(NT):
                    b = t % 2
                    rw = "row%d" % b
                    R = row[b]
                    dma("sp", R[:], qkvg_d.ap()[t * 128:(t + 1) * 128, :], [("qkvg", t)], [rw], rw)
                    dma("sp", hs[b][:], hbuf.ap()[t * 128:(t + 1) * 128, :], [("hbuf", t)], ["hs%d" % b], "hs%d" % b)
                    for h in range(4):
                        P.add("dve", lambda e, h=h, R=R: e.tensor_scalar(out=qd[:, h * 256:(h + 1) * 256], in0=R[:, h * 256:(h + 1) * 256],
                                                                          scalar1=dq[:, h:h + 1], scalar2=None, op0=ALU.mult), [rw, "dq"], ["qd"])
                        P.add("pool", lambda e, h=h, R=R: e.tensor_scalar(out=kd[:, h * 256:(h + 1) * 256], in0=R[:, 1024 + h * 256:1024 + (h + 1) * 256],
                                                                           scalar1=dkc[:, h:h + 1], scalar2=None, op0=ALU.mult), [rw, "dkc"], ["kd"])
                    transpose_blocks(R, rw, 8, qT, "qT", src_off=0)
                    transpose_blocks(R, rw, 8, kT, "kT", src_off=1024)
                    transpose_blocks(qd, "qd", 8, qdT, "qdT")
                    for h in range(4):
                        vs = R[:, 2048 + h * 512: 2048 + (h + 1) * 512]
                        for j in range(2):
                            P.add("pe", lambda e, h=h, j=j: e.matmul(psf[4][:, 0:128], lhsT=kT[:, 2 * h + j, :], rhs=qT[:, 2 * h + j, :],
                                                                    start=(j == 0), stop=(j == 1)), ["kT", "qT"], [PSF[4]])
                        P.add("dve", lambda e, h=h: e.tensor_tensor(out=innerT[:], in0=psf[4][:, 0:128], in1=maskT[:, h, :], op=ALU.mult),
                              [PSF[4], "maskT"], ["innerT"])
                        yb = h % 2
                        NOACC = os.environ.get("KNOACC", "") == "1"
                        P.add("pe", lambda e, vs=vs, yb=yb: e.matmul(psf[yb][:, :], lhsT=innerT[:], rhs=vs, start=True, stop=NOACC),
                              ["innerT", rw], [PSF[yb]])
                        for j in range(0 if NOACC else 2):
                            P.add("pe", lambda e, h=h, j=j, yb=yb: e.matmul(psf[yb][:, :], lhsT=qdT[:, 2 * h + j, :], rhs=Sb[:, 2 * h + j, :],
                                                                           start=False, stop=(j == 1)), ["qdT", "Sb"], [PSF[yb]])
                        if os.environ.get("KDBG", "") in ("y", "rv") and h == 3:
                            P.add("dve", lambda e, yb=yb, b=b: e.tensor_copy(out=ydb[:], in_=psf[yb][:, :]), [PSF[yb]], ["ydb"])
                        P.add("dve", lambda e, yb=yb: e.bn_stats(out=gsm[:, 0:6], in_=psf[yb][:, :]), [PSF[yb]], ["gsm"])
                        P.add("dve", lambda e: e.bn_aggr(out=gsm[:, 6:8], in_=gsm[:, 0:6]), ["gsm"], ["gsm"])
                        rstd_from(gsm[:, 7:8], eps_6, gsm[:, 9:10], gsm[:, 8:9], ["gsm"], "gsmr")
                        P.add("dve", lambda e, yb=yb: e.tensor_scalar(out=yn[:], in0=psf[yb][:, :], scalar1=gsm[:, 6:7], scalar2=gsm[:, 9:10],
                                                                      op0=ALU.subtract, op1=ALU.mult), [PSF[yb], "gsm", "gsmr"], ["yn"])
                        P.add("pool", lambda e, h=h: e.tensor_tensor(out=yn[:], in0=yn[:], in1=gn_g[:, h * 512:(h + 1) * 512], op=ALU.mult), ["yn", "gn_g"], ["yn"])
                        P.add("pool", lambda e, h=h: e.tensor_tensor(out=yn[:], in0=yn[:], in1=gn_b[:, h * 512:(h + 1) * 512], op=ALU.add), ["yn", "gn_b"], ["yn"])
                        P.add("pool", lambda e, h=h, R=R: e.tensor_tensor(out=yg[:, h * 512:(h + 1) * 512], in0=yn[:], in1=R[:, 4096 + h * 512:4096 + (h + 1) * 512],
                                                                           op=ALU.mult), ["yn", rw], ["yg"])
                        for j in range(2):
                            sbk = 2 + j
                            P.add("pe", lambda e, h=h, j=j, vs=vs, sbk=sbk: e.matmul(psf[sbk][:, :], lhsT=kd[:, h * 256 + j * 128: h * 256 + (j + 1) * 128],
                                                                                    rhs=vs, start=True, stop=True), ["kd", rw], [PSF[sbk]])
                            P.add("dve", lambda e, h=h, j=j, sbk=sbk: e.scalar_tensor_tensor(out=Sf[:, 2 * h + j, :], in0=Sf[:, 2 * h + j, :], scalar=dchunk[h],
                                                                                             in1=psf[sbk][:, :], op0=ALU.mult, op1=ALU.add),
                                  ["Sf", PSF[sbk]], ["Sf"])
                            P.add("dve", lambda e, h=h, j=j: e.tensor_copy(out=Sb[:, 2 * h + j, :], in_=Sf[:, 2 * h + j, :]), ["Sf"], ["Sb"])
                    transpose_blocks(yg, "yg", 16, ygT, "ygT")
                    for nh in range(2):
                        for fc in range(16):
                            P.add("pe", lambda e, nh=nh, fc=fc: e.matmul(psf[nh][:, :], lhsT=ygT[:, fc, :], rhs=wout[:, fc, nh * 512:(nh + 1) * 512],
                                                                        start=(fc == 0), stop=(fc == 15)), ["ygT", "wout"], [PSF[nh]])
                        P.add("dve", lambda e, nh=nh, b=b: e.scalar_tensor_tensor(out=z[:, nh * 512:(nh + 1) * 512], in0=hs[b][:, nh * 512:(nh + 1) * 512],
                                                                                  scalar=ALPHA, in1=psf[nh][:, :], op0=ALU.mult, op1=ALU.add),
                              ["hs%d" % b, PSF[nh]], ["z"])
                    layer_norm(z, "z", h1[b], "h1%d" % b, ln_g, ln_b, ["ln_g", "ln_b"], small, "small")
                    kd_ = os.environ.get("KDBG", "")
                    if kd_ == "z":
                        P.add("dve", lambda e, b=b: e.tensor_copy(out=h1[b][:], in_=z[:]), ["z", "h1%d" % b], ["h1%d" % b])
                    elif kd_ == "yg":
                        P.add("dve", lambda e, b=b: e.tensor_copy(out=h1[b][:], in_=yg[:, 0:1024]), ["yg", "h1%d" % b], ["h1%d" % b])
                    elif kd_ == "y":
                        P.add("dve", lambda e, b=b: e.tensor_copy(out=h1[b][:, 0:512], in_=yg[:, 1536:2048]), ["yg", "h1%d" % b], ["h1%d" % b])
                        P.add("dve", lambda e, b=b: e.tensor_copy(out=h1[b][:, 512:1024], in_=ydb[:]), ["ydb", "h1%d" % b], ["h1%d" % b])
                    elif kd_ == "rv":
                        P.add("dve", lambda e, b=b, R=R: e.tensor_copy(out=h1[b][:, 0:512], in_=R[:, 3584:4096]), [rw, "h1%d" % b], ["h1%d" % b])
                        P.add("dve", lambda e, b=b: e.tensor_copy(out=h1[b][:, 512:1024], in_=ydb[:]), ["ydb", "h1%d" % b], ["h1%d" % b])
                    elif kd_ == "yn":
                        P.add("dve", lambda e, b=b: e.tensor_copy(out=h1[b][:, 0:512], in_=yn[:]), ["yn", "h1%d" % b], ["h1%d" % b])
                        P.add("dve", lambda e, b=b: e.tensor_copy(out=h1[b][:, 512:544], in_=gsm[:]), ["gsm", "gsmr", "h1%d" % b], ["h1%d" % b])
                        P.add("dve", lambda e, b=b: e.tensor_copy(out=h1[b][:, 640:768], in_=innerT[:]), ["innerT", "h1%d" % b], ["h1%d" % b])
                    dma("pool", hbuf.ap()[t * 128:(t + 1) * 128, :], h1[b][:], ["h1%d" % b], [("hbuf", t)], "h1%d" % b)
            P.barrier()

        def moe_ple(l, last):
            if skip():
                return
            with contextlib.ExitStack() as st:
                wr = sb(st, "wr", [128, 8, E], F32)
                dma("sp", wr[:], W["moe_w_router"].ap()[l].rearrange("(kc p) e -> p kc e", p=128), [], ["wr"], "wr")
                br = sb(st, "br", [128, E], F32)
                dma("sp", br[:], bc_row(W["moe_b_router"], l * E, E), [], ["br"], "br")
                iota_e = sb(st, "iota_e", [128, E], F32)
                dma("sp", iota_e[:], CD["iota_e"].ap(), [], ["iota_e"], "iota_e")
                ecap = sb(st, "ecap", [128, E], F32)
                P.add("dve", lambda e: e.tensor_scalar(out=ecap[:], in0=iota_e[:], scalar1=float(CAP), scalar2=None, op0=ALU.mult), ["iota_e"], ["ecap"])
                tri_f = sb(st, "tri_f", [128, 128], F32)
                tri_b = sb(st, "tri_b", [128, 128], BF16)
                dma("sp", tri_f[:], CD["tri"].ap(), [], ["tri_f"], "tri_f")
                P.add("dve", lambda e: e.tensor_copy(out=tri_b[:], in_=tri_f[:]), ["tri_f"], ["tri_b"])
                base = sb(st, "base", [128, E], F32)
                P.add("dve", lambda e: e.memset(base[:], 0.0), [], ["base"])
                hs = [sb(st, "hs", [128, D], F32) for _ in range(2)]
                hb = [sb(st, "hb", [128, D], BF16) for _ in range(2)]
                hT32 = sb(st, "hT32", [128, 8, 128], F32)
                lg = sb(st, "lg", [128, E], F32)
                mx = sb(st, "mx", [128, 8], F32)
                mi = sb(st, "mi", [128, 8], U32)
                mif = sb(st, "mif", [128, 8], F32)
                negm = sb(st, "negm", [128, 1], F32)
                ex = sb(st, "ex", [128, 4], F32)
                rs = sb(st, "rs", [128, 2], F32)
                maskf = sb(st, "maskf", [128, E], F32)
                maskb = sb(st, "maskb", [128, E], BF16)
                slotf = sb(st, "slotf", [128, E], F32)
                eq = sb(st, "eq", [128, E], F32)
                s4f = sb(st, "s4f", [128, 4], F32)
                sk = [[sb(st, "sk", [128, 1], I32) for _ in range(2)] for _ in range(TOPK)]
                for t in range(NT):
                    b = t % 2
                    hn = "hs%d" % b
                    dma("sp", hs[b][:], hbuf.ap()[t * 128:(t + 1) * 128, :], [("hbuf", t)], [hn], hn)
                    P.add("pool", lambda e, b=b: e.tensor_copy(out=hb[b][:], in_=hs[b][:]), [hn], ["hb%d" % b])
                    for half in range(2):
                        bank = 2 + half
                        for j in range(4):
                            kc = half * 4 + j
                            P.add("pe", lambda e, kc=kc, j=j, b=b, bank=bank: e.transpose(out=psf[bank][:, j * 128:(j + 1) * 128],
                                                                                         in_=hs[b][:, kc * 128:(kc + 1) * 128], identity=ident_f[:]),
                                  [hn, "ident_f"], [PSF[bank]])
                        P.add("act", lambda e, half=half, bank=bank: e.copy(out=hT32[:, half * 4:(half + 1) * 4, :],
                                                                             in_=psf[bank][:, :].rearrange("p (j t) -> p j t", t=128)),
                              [PSF[bank]], ["hT32"])
                    for kc in range(8):
                        P.add("pe", lambda e, kc=kc: e.matmul(psf[0][:, 0:E], lhsT=hT32[:, kc, :], rhs=wr[:, kc, :], start=(kc == 0), stop=(kc == 7)),
                              ["hT32", "wr"], [PSF[0]])
                    P.add("dve", lambda e: e.tensor_tensor(out=lg[:], in0=psf[0][:, 0:E], in1=br[:], op=ALU.add), [PSF[0], "br"], ["lg"])
                    P.add("dve", lambda e: e.max(out=mx[:], in_=lg[:]), ["lg"], ["mx"])
                    P.add("dve", lambda e: e.max_index(out=mi[:], in_max=mx[:], in_values=lg[:]), ["lg", "mx"], ["mi"])
                    P.add("dve", lambda e: e.tensor_copy(out=mif[:], in_=mi[:]), ["mi"], ["mif"])
                    P.add("dve", lambda e: e.tensor_scalar(out=negm[:], in0=mx[:, 0:1], scalar1=-1.0, scalar2=None, op0=ALU.mult), ["mx"], ["negm"])
                    P.add("dve", lambda e: e.tensor_scalar(out=ex[:], in0=mx[:, 0:4], scalar1=negm[:, 0:1], scalar2=None, op0=ALU.add), ["mx", "negm"], ["ex"])
                    P.add("act", lambda e: e.activation(out=ex[:], in_=ex[:], func=AF.Exp), ["ex"], ["ex"])
                    P.add("dve", lambda e: e.reduce_sum(out=rs[:, 0:1], in_=ex[:], axis=mybir.AxisListType.X), ["ex"], ["rs"])
                    P.add("dve", lambda e: e.reciprocal(out=rs[:, 1:2], in_=rs[:, 0:1]), ["rs"], ["rs2"])
                    P.add("dve", lambda e, t=t: e.tensor_scalar(out=gates_s[:, t, :], in0=ex[:], scalar1=rs[:, 1:2], scalar2=None, op0=ALU.mult),
                          ["ex", "rs2"], ["gates_s"])
                    P.add("dve", lambda e: e.tensor_scalar(out=maskf[:], in0=lg[:], scalar1=mx[:, 3:4], scalar2=None, op0=ALU.is_ge), ["lg", "mx"], ["maskf"])
                    P.add("dve", lambda e: e.tensor_copy(out=maskb[:], in_=maskf[:]), ["maskf"], ["maskb"])
                    P.add("pe", lambda e: e.matmul(psf[1][:, 0:E], lhsT=tri_b[:], rhs=maskb[:], start=True, stop=True), ["tri_b", "maskb"], [PSF[1]])
                    P.add("pe", lambda e: e.matmul(psf[1][:, 64:64 + E], lhsT=ones_b[:], rhs=maskb[:], start=True, stop=True), ["ones_b", "maskb"], [PSF[1]])
                    P.add("dve", lambda e: e.tensor_tensor(out=slotf[:], in0=psf[1][:, 0:E], in1=base[:], op=ALU.add), [PSF[1], "base"], ["slotf"])
                    P.add("dve", lambda e: e.tensor_tensor(out=slotf[:], in0=slotf[:], in1=ecap[:], op=ALU.add), ["slotf", "ecap"], ["slotf"])
                    P.add("dve", lambda e: e.tensor_tensor(out=base[:], in0=base[:], in1=psf[1][:, 64:64 + E], op=ALU.add), [PSF[1], "base", "slotf"], ["base"])
                    for k in range(TOPK):
                        P.add("dve", lambda e, k=k: e.tensor_scalar(out=eq[:], in0=iota_e[:], scalar1=mif[:, k:k + 1], scalar2=None, op0=ALU.is_equal),
                              ["iota_e", "mif"], ["eq"])
                        P.add("dve", lambda e: e.tensor_tensor(out=eq[:], in0=eq[:], in1=slotf[:], op=ALU.mult), ["eq", "slotf"], ["eq"])
                        P.add("dve", lambda e, k=k: e.reduce_sum(out=s4f[:, k:k + 1], in_=eq[:], axis=mybir.AxisListType.X), ["eq"], ["s4f"])
                    P.add("dve", lambda e, t=t: e.tensor_copy(out=slots_i[:, t, :], in_=s4f[:]), ["s4f"], ["slots_i"])
                    for k in range(TOPK):
                        P.add("dve", lambda e, t=t, k=k, b=b: e.tensor_copy(out=sk[k][b][:, :], in_=s4f[:, k:k + 1]), ["s4f"], ["sk%d_%d" % (k, b)])
                        P.add("pool", lambda e, t=t, k=k, b=b: e.indirect_dma_start(
                            out=xs_d.ap(), out_offset=bass.IndirectOffsetOnAxis(ap=sk[k][b][:, :], axis=0),
                            in_=hb[b][:, :], in_offset=None),
                            ["sk%d_%d" % (k, b), "hb%d" % b], ["xs"], dma_key="hb%d" % b)
            P.barrier()
            if skip():
                return
            with contextlib.ExitStack() as st:
                wgu = [sb(st, "wgu", [128, 8, 2048], BF16) for _ in range(2)]
                wdn = [sb(st, "wdn", [128, 8, D], BF16) for _ in range(2)]
                stage = [sb(st, "wst", [128, 2048], F32) for _ in range(3)]
                bgu = [sb(st, "bgu", [128, 16], F32) for _ in range(2)]
                bdf = [sb(st, "bdf", [1, D], F32) for _ in range(2)]
                bdb = [sb(st, "bdb", [1, D], BF16) for _ in range(2)]
                xr = [sb(st, "xr", [128, D], BF16) for _ in range(2)]
                xT = sb(st, "xT", [128, 8, CAP], BF16)
                actT = sb(st, "actT", [128, 8, CAP], BF16)
                gc = [sb(st, "gc", [128, 512], F32) for _ in range(2)]
                sg = [sb(st, "sg", [128, 512], F32) for _ in range(2)]
                uc = [sb(st, "uc", [128, 512], F32) for _ in range(2)]
                yo = [sb(st, "yo", [128, D], F32) for _ in range(2)]
                cnt = 0
                for ex_ in range(E):
                    eb = ex_ % 2
                    load_w_bf16(st, wgu[eb], "wgu%d" % eb, W["moe_w_gate_up"].ap()[l, ex_], 8, 2048, stage, 3)
                    load_w_bf16(st, wdn[eb], "wdn%d" % eb, W["moe_w_down"].ap()[l, ex_], 8, D, stage, 3)
                    dma("sp", bgu[eb][:], W["moe_b_gate_up"].ap()[l, ex_], [], ["bgu%d" % eb], "bgu%d" % eb)
                    dma("sp", bdf[eb][:], W["moe_b_down"].ap()[l, ex_:ex_ + 1, :], [], ["bdf%d" % eb], "bdf%d" % eb)
                    P.add("dve", lambda e, eb=eb: e.tensor_copy(out=bdb[eb][:], in_=bdf[eb][:]), ["bdf%d" % eb], ["bdb%d" % eb])
                    for s_ in range(CT):
                        xb = cnt % 2
                        cnt += 1
                        r0 = ex_ * CAP + s_ * 128
                        dma("sp", xr[xb][:], xs_d.ap()[r0:r0 + 128, :], ["xs"], ["xr%d" % xb], "xr%d" % xb)
                        pb = rr["ps"] % 2
                        rr["ps"] += 1
                        for j in range(8):
                            P.add("pe", lambda e, j=j, pb=pb, xb=xb: e.transpose(out=psb[pb][:, j * 128:(j + 1) * 128], in_=xr[xb][:, j * 128:(j + 1) * 128],
                                                                                 identity=ident_b[:]), ["xr%d" % xb, "ident_b"], [PSB[pb]])
                        P.add("act", lambda e, pb=pb, s_=s_: e.copy(out=xT[:, :, s_ * 128:(s_ + 1) * 128], in_=psb[pb][:, :].rearrange("p (j t) -> p j t", t=128)),
                              [PSB[pb]], ["xT"])
                    for fc in range(8):
                        for n0 in range(0, CAP, 512):
                            n1 = min(CAP, n0 + 512)
                            nn = n1 - n0
                            w_ = (fc * 8 + n0 // 512) % 2
                            bg_, bu_ = (0, 1) if w_ == 0 else (2, 3)
                            for kc in range(8):
                                P.add("pe", lambda e, kc=kc, fc=fc, n0=n0, n1=n1, bg_=bg_, eb=eb, nn=nn: e.matmul(
                                    psf[bg_][:, 0:nn], lhsT=wgu[eb][:, kc, fc * 128:(fc + 1) * 128], rhs=xT[:, kc, n0:n1], start=(kc == 0), stop=(kc == 7)),
                                    ["wgu%d" % eb, "xT"], [PSF[bg_]])
                            for kc in range(8):
                                P.add("pe", lambda e, kc=kc, fc=fc, n0=n0, n1=n1, bu_=bu_, eb=eb, nn=nn: e.matmul(
                                    psf[bu_][:, 0:nn], lhsT=wgu[eb][:, kc, 1024 + fc * 128:1024 + (fc + 1) * 128], rhs=xT[:, kc, n0:n1], start=(kc == 0), stop=(kc == 7)),
                                    ["wgu%d" % eb, "xT"], [PSF[bu_]])
                            P.add("dve", lambda e, fc=fc, bg_=bg_, eb=eb, nn=nn, w_=w_: e.tensor_scalar(out=gc[w_][:, 0:nn], in0=psf[bg_][:, 0:nn], scalar1=bgu[eb][:, fc:fc + 1],
                                                                                                      scalar2=7.0, op0=ALU.add, op1=ALU.min), [PSF[bg_], "bgu%d" % eb], ["gc%d" % w_])
                            P.add("act", lambda e, nn=nn, w_=w_: e.activation(out=sg[w_][:, 0:nn], in_=gc[w_][:, 0:nn], func=AF.Sigmoid, scale=1.702), ["gc%d" % w_], ["sg%d" % w_])
                            P.add("dve", lambda e, fc=fc, bu_=bu_, eb=eb, nn=nn, w_=w_: e.tensor_scalar(out=uc[w_][:, 0:nn], in0=psf[bu_][:, 0:nn], scalar1=bgu[eb][:, 8 + fc:9 + fc],
                                                                                                      scalar2=-7.0, op0=ALU.add, op1=ALU.max), [PSF[bu_], "bgu%d" % eb], ["uc%d" % w_])
                            P.add("pool", lambda e, nn=nn, w_=w_: e.tensor_scalar(out=uc[w_][:, 0:nn], in0=uc[w_][:, 0:nn], scalar1=7.0, scalar2=1.0, op0=ALU.min, op1=ALU.add),
                                  ["uc%d" % w_], ["uc%d" % w_])
                            P.add("pool", lambda e, nn=nn, w_=w_: e.tensor_tensor(out=gc[w_][:, 0:nn], in0=gc[w_][:, 0:nn], in1=sg[w_][:, 0:nn], op=ALU.mult),
                                  ["gc%d" % w_, "sg%d" % w_], ["gc%d" % w_])
                            P.add("pool", lambda e, fc=fc, n0=n0, n1=n1, nn=nn, w_=w_: e.tensor_tensor(out=actT[:, fc, n0:n1], in0=gc[w_][:, 0:nn], in1=uc[w_][:, 0:nn], op=ALU.mult),
                                  ["gc%d" % w_, "uc%d" % w_], ["actT"])
                    for s_ in range(CT):
                        yb = s_ % 2
                        for nh in range(2):
                            bank = 4 + nh
                            for fc in range(8):
                                P.add("pe", lambda e, fc=fc, s_=s_, nh=nh, bank=bank, eb=eb: e.matmul(
                                    psf[bank][:, :], lhsT=actT[:, fc, s_ * 128:(s_ + 1) * 128], rhs=wdn[eb][:, fc, nh * 512:(nh + 1) * 512], start=(fc == 0), stop=False),
                                    ["actT", "wdn%d" % eb], [PSF[bank]])
                            P.add("pe", lambda e, nh=nh, bank=bank, eb=eb: e.matmul(psf[bank][:, :], lhsT=ones_b[0:1, :], rhs=bdb[eb][0:1, nh * 512:(nh + 1) * 512],
                                                                                  start=False, stop=True), ["ones_b", "bdb%d" % eb], [PSF[bank]])
                            P.add("act", lambda e, nh=nh, bank=bank, yb=yb: e.copy(out=yo[yb][:, nh * 512:(nh + 1) * 512], in_=psf[bank][:, :]), [PSF[bank]], ["yo%d" % yb])
                        r0 = ex_ * CAP + s_ * 128
                        dma("act", ys_d.ap()[r0:r0 + 128, :], yo[yb][:], ["yo%d" % yb], ["ys"], "yo%d" % yb)
            P.barrier()
            if skip():
                return
            with contextlib.ExitStack() as st:
                wg = sb(st, "wg", [128, 8, D], BF16)
                wp = sb(st, "wp", [128, 2, D], BF16)
                stage = [sb(st, "wst", [128, 1024], F32) for _ in range(2)]
                load_w_bf16(st, wg, "wg", W["ple_w_gate"].ap()[l], 8, D, stage, 2)
                load_w_bf16(st, wp, "wp", W["ple_w_proj"].ap()[l], 2, D, stage, 2)
                ln_g = sb(st, "ln_g", [128, D], F32)
                ln_b = sb(st, "ln_b", [128, D], F32)
                dma("sp", ln_g[:], bc_row(W["ln_ffn_g"], l * D, D), [], ["ln_g"], "ln_g")
                dma("sp", ln_b[:], bc_row(W["ln_ffn_b"], l * D, D), [], ["ln_b"], "ln_b")
                yk = [sb(st, "yk", [128, D], F32) for _ in range(4)]
                hs = [sb(st, "hs", [128, D], F32) for _ in range(2)]
                ps_ = [sb(st, "ps_", [128, 256], F32) for _ in range(2)]
                pb_ = sb(st, "pb_", [128, 256], BF16)
                pT = sb(st, "pT", [128, 2, 128], BF16)
                acc = sb(st, "acc", [128, D], F32)
                z = sb(st, "z", [128, D], F32)
                h2 = sb(st, "h2", [128, D], F32)
                h2b = sb(st, "h2b", [128, D], BF16)
                h2T = sb(st, "h2T", [128, 8, 128], BF16)
                sgm = sb(st, "sgm", [128, 512], F32)
                h3 = [sb(st, "h3", [128, D], F32) for _ in range(2)]
                small = sb(st, "small", [128, 32], F32)
                ck = [sb(st, "ck", [128, 1], I32) for _ in range(TOPK)]
                for t in range(NT):
                    b = t % 2
                    hn = "hs%d" % b
                    dma("sp", hs[b][:], hbuf.ap()[t * 128:(t + 1) * 128, :], [("hbuf", t)], [hn], hn)
                    dma("sp", ps_[b][:], p_d.ap()[l, t * 128:(t + 1) * 128, :], [], ["ps_%d" % b], "ps_%d" % b)
                    for k in range(TOPK):
                        P.add("dve", lambda e, t=t, k=k: e.tensor_copy(out=ck[k][:, :], in_=slots_i[:, t, k:k + 1]), ["slots_i"], ["ck%d" % k])
                        P.add("pool", lambda e, t=t, k=k: e.indirect_dma_start(
                            out=yk[k][:, :], out_offset=None, in_=ys_d.ap(), in_offset=bass.IndirectOffsetOnAxis(ap=ck[k][:, :], axis=0)),
                            ["ck%d" % k, "ys"], ["yk%d" % k], dma_key="yk%d" % k)
                    P.add("dve", lambda e, t=t: e.tensor_scalar(out=acc[:], in0=yk[0][:], scalar1=gates_s[:, t, 0:1], scalar2=None, op0=ALU.mult),
                          ["yk0", "gates_s"], ["acc"])
                    for k in range(1, TOPK):
                        P.add("dve", lambda e, t=t, k=k: e.scalar_tensor_tensor(out=acc[:], in0=yk[k][:], scalar=gates_s[:, t, k:k + 1], in1=acc[:],
                                                                                op0=ALU.mult, op1=ALU.add), ["yk%d" % k, "gates_s", "acc"], ["acc"])
                    P.add("dve", lambda e, b=b: e.scalar_tensor_tensor(out=z[:], in0=hs[b][:], scalar=ALPHA, in1=acc[:], op0=ALU.mult, op1=ALU.add),
                          [hn, "acc"], ["z"])
                    layer_norm(z, "z", h2, "h2", ln_g, ln_b, ["ln_g", "ln_b"], small, "small")
                    P.add("act", lambda e: e.copy(out=h2b[:], in_=h2[:]), ["h2"], ["h2b"])
                    transpose_blocks(h2b, "h2b", 8, h2T, "h2T")
                    P.add("act", lambda e, b=b: e.copy(out=pb_[:], in_=ps_[b][:]), ["ps_%d" % b], ["pb_"])
                    transpose_blocks(pb_, "pb_", 2, pT, "pT")
                    for nh in range(2):
                        linear(h2T, "h2T", 8, wg, "wg", nh * 512, (nh + 1) * 512, nh)
                        linear(pT, "pT", 2, wp, "wp", nh * 512, (nh + 1) * 512, 2 + nh)
                        P.add("act", lambda e, nh=nh: e.activation(out=sgm[:], in_=psf[nh][:, :], func=AF.Sigmoid), [PSF[nh]], ["sgm"])
                        P.add("dve", lambda e, nh=nh: e.tensor_tensor(out=sgm[:], in0=sgm[:], in1=psf[2 + nh][:, :], op=ALU.mult), ["sgm", PSF[2 + nh]], ["sgm"])
                        P.add("pool", lambda e, nh=nh, b=b: e.tensor_tensor(out=h3[b][:, nh * 512:(nh + 1) * 512], in0=sgm[:], in1=h2[:, nh * 512:(nh + 1) * 512], op=ALU.add),
                              ["sgm", "h2"], ["h3%d" % b])
                    if last:
                        fin.append(dma("pool", y_d.ap()[t * 128:(t + 1) * 128, :], h3[b][:], ["h3%d" % b], [("y", t)], "h3%d" % b))
                    else:
                        dma("pool", hbuf.ap()[t * 128:(t + 1) * 128, :], h3[b][:], ["h3%d" % b], [("hbuf", t)], "h3%d" % b)
            P.barrier()
            if dbg:
                dma("sp", dbg_d.ap()[l], (y_d if last else hbuf).ap(), [], [], "dbg")
                P.barrier()

        kmax_bc = sb(root, "kmax_bc", [128, 1], F32)

        def sumsq(ps_ap, n, out_col, junk, psname, wname):
            P.add("act", lambda e: e.activation(out=junk[:, 0:n], in_=ps_ap, func=AF.Square), [psname], ["junk"])
            P.add("dve", lambda e: e.reduce_sum(out=out_col, in_=junk[:, 0:n], axis=mybir.AxisListType.X), ["junk"], [wname])

        def rms_norm_ps(ps_ap, psname, n, g_t, gname, out_b, oname, junk, small, sname, col):
            sumsq(ps_ap, n, small[:, col:col + 1], junk, psname, sname)
            rstd_from(small[:, col:col + 1], eps_6, small[:, col + 2:col + 3], small[:, col + 1:col + 2], [sname], sname + "r", scale=1.0 / n)
            P.add("dve", lambda e: e.tensor_scalar(out=junk[:, 0:n], in0=ps_ap, scalar1=small[:, col + 2:col + 3], scalar2=None, op0=ALU.mult),
                  [psname, sname + "r"], ["junk"])
            P.add("pool", lambda e: e.tensor_tensor(out=out_b, in0=junk[:, 0:n], in1=g_t[:, 0:n], op=ALU.mult), ["junk", gname], [oname])

        def mla_kv():
            if skip():
                return
            with contextlib.ExitStack() as st:
                wa = sb(st, "wa", [128, 8, 320], BF16)
                wb = sb(st, "wb", [128, 2, 2048], BF16)
                stage = [sb(st, "wst", [128, 2048], F32) for _ in range(2)]
                load_w_bf16(st, wa, "wa", W["mla_w_kv_a"].ap(), 8, 320, stage, 2)
                load_w_bf16(st, wb, "wb", W["mla_w_kv_b"].ap(), 2, 2048, stage, 2)
                g_t = sb(st, "kvg", [128, 256], F32)
                dma("sp", g_t[:], bc_row(W["mla_kv_norm_g"], 0, 256), [], ["kvg"], "kvg")
                hs = [sb(st, "hs", [128, D], F32) for _ in range(2)]
                hb = sb(st, "hb", [128, D], BF16)
                hT = sb(st, "hT", [128, 8, 128], BF16)
                junk = sb(st, "junk", [128, 512], F32)
                small = sb(st, "small", [128, 32], F32)
                cb = sb(st, "cb", [128, 256], BF16)
                cT = sb(st, "cT", [128, 2, 128], BF16)
                cos_t = sb(st, "cos", [128, 32], F32)
                sin_t = sb(st, "sin", [128, 32], F32)
                tf = sb(st, "tf", [128, 32], F32)
                ti = sb(st, "ti", [128, 32], I32)
                t1 = sb(st, "t1", [128, 32], F32)
                t2 = sb(st, "t2", [128, 32], F32)
                kn = [sb(st, "kn", [128, D], BF16) for _ in range(2)]
                knT = [sb(st, "knT", [128, 8, 128], BF16) for _ in range(2)]
                vx = [sb(st, "vx", [128, 8, 130], BF16) for _ in range(2)]
                kr = sb(st, "kr", [128, 128], BF16)
                krT = [sb(st, "krT", [128, 1, 128], BF16) for _ in range(2)]
                ksq = sb(st, "ksq", [128, 16], F32)
                kmx = sb(st, "kmx", [128, 8], F32)
                P.add("dve", lambda e: e.memset(kmx[:], 0.0), [], ["kmx"])
                P.add("pool", lambda e: e.tensor_copy(out=kr[:], in_=zeros_f[:, 0:128]), ["zeros_f"], ["kr"])
                P.add("pool", lambda e: e.tensor_copy(out=kr[:, 64:65], in_=ones_f[:, 0:1]), ["kr", "ones_f"], ["kr"])
                for b in range(2):
                    for h_ in range(8):
                        P.add("pool", lambda e, b=b, h_=h_: e.tensor_copy(out=vx[b][:, h_, 128:130], in_=ones_f[:, 0:2]), ["ones_f"], ["vx%d" % b])
                P.last_w["inv"] = P.last_w.get("inv_m")
                for t in range(NT):
                    b = t % 2
                    hn = "hs%d" % b
                    dma("sp", hs[b][:], hbuf.ap()[t * 128:(t + 1) * 128, :], [("hbuf", t)], [hn], hn)
                    P.add("pool", lambda e, b=b: e.tensor_copy(out=hb[:], in_=hs[b][:]), [hn], ["hb"])
                    transpose_blocks(hb, "hb", 8, hT, "hT")
                    rope_tables(t, inv_m, 32, cos_t, sin_t, tf, ti, "kv")
                    linear(hT, "hT", 8, wa, "wa", 0, 320, 4)
                    rms_norm_ps(psf[4][:, 0:256], PSF[4], 256, g_t, "kvg", cb[:], "cb", junk, small, "small", 0)
                    rope_apply(psf[4][:, 256:288], psf[4][:, 288:320], kr[:, 0:32], kr[:, 32:64], cos_t, sin_t, 32, t1, t2, [PSF[4]], "kr", "kv")
                    sumsq(psf[4][:, 256:320], 64, ksq[:, 8:9], junk, PSF[4], "ksq8")
                    P.add("pe", lambda e: e.transpose(out=psb[0][:, 0:128], in_=kr[:, 0:128], identity=ident_b[:]), ["kr", "ident_b"], [PSB[0]])
                    P.add("act", lambda e, b=b: e.copy(out=krT[b][:, 0, :], in_=psb[0][:, 0:128]), [PSB[0]], ["krT%d" % b])
                    dma("act", KR_d.ap()[:, t * 128:(t + 1) * 128], krT[b][0:65, 0, :], ["krT%d" % b], [("KR", t)], "krT%d" % b)
                    transpose_blocks(cb, "cb", 2, cT, "cT")
                    for n in range(4):
                        bank = n % 4
                        linear(cT, "cT", 2, wb, "wb", n * 512, (n + 1) * 512, bank)
                        for hh in range(2):
                            h = n * 2 + hh
                            P.add("act", lambda e, h=h, hh=hh, bank=bank, b=b: e.copy(out=kn[b][:, h * 128:(h + 1) * 128], in_=psf[bank][:, hh * 256: hh * 256 + 128]),
                                  [PSF[bank]], ["kn%d" % b])
                            sumsq(psf[bank][:, hh * 256: hh * 256 + 128], 128, ksq[:, h:h + 1], junk, PSF[bank], "ksq")
                            P.add("dve", lambda e, h=h, hh=hh, bank=bank, b=b: e.tensor_copy(out=vx[b][:, h, 0:128], in_=psf[bank][:, hh * 256 + 128: hh * 256 + 256]),
                                  [PSF[bank]], ["vx%d" % b])
                    P.add("dve", lambda e: e.tensor_scalar(out=ksq[:, 0:8], in0=ksq[:, 0:8], scalar1=ksq[:, 8:9], scalar2=None, op0=ALU.add), ["ksq", "ksq8"], ["ksq"])
                    P.add("dve", lambda e: e.tensor_tensor(out=kmx[:], in0=kmx[:], in1=ksq[:, 0:8], op=ALU.max), ["ksq", "kmx"], ["kmx"])
                    transpose_blocks(kn[b], "kn%d" % b, 8, knT[b], "knT%d" % b)
                    dma("act", KT_d.ap()[:, :, t * 128:(t + 1) * 128].rearrange("h d t -> d h t"), knT[b][:], ["knT%d" % b], [("KT", t)], "knT%d" % b)
                    dma("act", V_d.ap()[t * 128:(t + 1) * 128], vx[b][:], ["vx%d" % b], [("V", t)], "vx%d" % b)
                P.add("dve", lambda e: e.reduce_max(out=ksq[:, 9:10], in_=kmx[:], axis=mybir.AxisListType.X), ["kmx"], ["kmr"])
                P.add("pe", lambda e: e.transpose(out=psf[5][0:1, 0:128], in_=ksq[:, 9:10], identity=ident_f[:]), ["kmr", "ident_f"], [PSF[5]])
                P.add("dve", lambda e: e.reduce_max(out=small[0:1, 20:21], in_=psf[5][0:1, 0:128], axis=mybir.AxisListType.X), [PSF[5]], ["km1"])
                P.add("pe", lambda e: e.matmul(psf[5][:, 256:257], lhsT=ones_f[0:1, :], rhs=small[0:1, 20:21], start=True, stop=True), ["km1", "ones_f"], [PSF[5]])
                P.add("dve", lambda e: e.tensor_copy(out=kmax_bc[:], in_=psf[5][:, 256:257]), [PSF[5]], ["kmax_bc"])
            P.barrier()

        def mla_layer(l):
            j = l - NA
            if skip():
                return
            with contextlib.ExitStack() as st:
                wa = sb(st, "wqa", [128, 8, 256], BF16)
                wb = sb(st, "wqb", [128, 2, 1536], BF16)
                stage = [sb(st, "wst", [128, 1536], F32) for _ in range(2)]
                load_w_bf16(st, wa, "wqa", W["mla_w_q_a"].ap()[j], 8, 256, stage, 2)
                load_w_bf16(st, wb, "wqb", W["mla_w_q_b"].ap()[j], 2, 1536, stage, 2)
                g_t = sb(st, "qg", [128, 256], F32)
                dma("sp", g_t[:], bc_row(W["mla_q_norm_g"], j * 256, 256), [], ["qg"], "qg")
                hs = [sb(st, "hs", [128, D], F32) for _ in range(2)]
                hb = sb(st, "hb", [128, D], BF16)
                hT = sb(st, "hT", [128, 8, 128], BF16)
                junk = sb(st, "junk", [128, 512], F32)
                small = sb(st, "small", [128, 32], F32)
                cb = sb(st, "cb", [128, 256], BF16)
                cT = sb(st, "cT", [128, 2, 128], BF16)
                cos_t = sb(st, "cos", [128, 32], F32)
                sin_t = sb(st, "sin", [128, 32], F32)
                tf = sb(st, "tf", [128, 32], F32)
                ti = sb(st, "ti", [128, 32], I32)
                t1 = sb(st, "t1", [128, 32], F32)
                t2 = sb(st, "t2", [128, 32], F32)
                qn = sb(st, "qn", [128, D], BF16)
                qr = sb(st, "qr", [128, 1024], BF16)
                qsq = sb(st, "qsq", [128, 8], F32)
                qsh = sb(st, "qsh", [128, 8], F32)
                qnT = [sb(st, "qnT", [128, 8, 128], BF16) for _ in range(2)]
                qrT = [sb(st, "qrT", [128, 8, 128], BF16) for _ in range(2)]
                for h_ in range(8):
                    P.add("pool", lambda e, h_=h_: e.tensor_copy(out=qr[:, h_ * 128:(h_ + 1) * 128], in_=zeros_f[:, 0:128]), ["zeros_f"], ["qr"])
                P.last_w["inv"] = P.last_w.get("inv_m")
                for t in range(NT):
                    b = t % 2
                    hn = "hs%d" % b
                    dma("sp", hs[b][:], hbuf.ap()[t * 128:(t + 1) * 128, :], [("hbuf", t)], [hn], hn)
                    P.add("pool", lambda e, b=b: e.tensor_copy(out=hb[:], in_=hs[b][:]), [hn], ["hb"])
                    transpose_blocks(hb, "hb", 8, hT, "hT")
                    rope_tables(t, inv_m, 32, cos_t, sin_t, tf, ti, "q")
                    linear(hT, "hT", 8, wa, "wqa", 0, 256, 4)
                    rms_norm_ps(psf[4][:, 0:256], PSF[4], 256, g_t, "qg", cb[:], "cb", junk, small, "small", 0)
                    transpose_blocks(cb, "cb", 2, cT, "cT")
                    for n in range(3):
                        linear(cT, "cT", 2, wb, "wqb", n * 512, (n + 1) * 512, n)
                    for h in range(8):
                        c0 = h * 192
                        def seg(a, bnd):
                            bk = a // 512
                            assert (bnd - 1) // 512 == bk
                            return psf[bk][:, a - bk * 512: bnd - bk * 512], PSF[bk]
                        a = c0
                        while a < c0 + 128:
                            bnd = min(c0 + 128, (a // 512 + 1) * 512)
                            ap_, nm = seg(a, bnd)
                            P.add("act", lambda e, ap_=ap_, a=a, bnd=bnd, h=h, c0=c0: e.copy(out=qn[:, h * 128 + (a - c0): h * 128 + (bnd - c0)], in_=ap_), [nm], ["qn"])
                            a = bnd
                        a = c0
                        pieces = []
                        while a < c0 + 192:
                            bnd = min(c0 + 192, (a // 512 + 1) * 512)
                            pieces.append((a, bnd))
                            a = bnd
                        for pi, (a, bnd) in enumerate(pieces):
                            ap_, nm = seg(a, bnd)
                            sumsq(ap_, bnd - a, small[:, 8 + pi: 9 + pi], junk, nm, "sq%d" % pi)
                        if len(pieces) == 2:
                            P.add("dve", lambda e, h=h: e.tensor_tensor(out=qsq[:, h:h + 1], in0=small[:, 8:9], in1=small[:, 9:10], op=ALU.add), ["sq0", "sq1"], ["qsq"])
                        else:
                            P.add("dve", lambda e, h=h: e.tensor_copy(out=qsq[:, h:h + 1], in_=small[:, 8:9]), ["sq0"], ["qsq"])
                        r1, n1_ = seg(c0 + 128, c0 + 160)
                        r2, n2_ = seg(c0 + 160, c0 + 192)
                        rope_apply(r1, r2, qr[:, h * 128:h * 128 + 32], qr[:, h * 128 + 32:h * 128 + 64], cos_t, sin_t, 32, t1, t2, list({n1_, n2_}), "qr", "q")
                    P.add("dve", lambda e: e.tensor_scalar(out=qsh[:], in0=qsq[:], scalar1=kmax_bc[:, 0:1], scalar2=1e-30, op0=ALU.mult, op1=ALU.add), ["qsq", "kmax_bc"], ["qsh"])
                    P.add("act", lambda e: e.activation(out=qsh[:], in_=qsh[:], func=AF.Ln), ["qsh"], ["qsh"])
                    P.add("act", lambda e: e.activation(out=qsh[:], in_=qsh[:], func=AF.Exp, scale=0.5), ["qsh"], ["qsh"])
                    for h in range(8):
                        P.add("dve", lambda e, h=h: e.tensor_scalar(out=qr[:, h * 128 + 64:h * 128 + 65], in0=qsh[:, h:h + 1], scalar1=-1.0, scalar2=None, op0=ALU.mult), ["qsh", "qr"], ["qr"])
                    transpose_blocks(qn, "qn", 8, qnT[b], "qnT%d" % b)
                    transpose_blocks(qr, "qr", 8, qrT[b], "qrT%d" % b, src_off=0)
                    dma("act", QT_d.ap()[:, :, t * 128:(t + 1) * 128].rearrange("h d t -> d h t"), qnT[b][:], ["qnT%d" % b], [("QT", t)], "qnT%d" % b)
                    dma("act", QR_d.ap()[:, :, t * 128:(t + 1) * 128].rearrange("h d t -> d h t"), qrT[b][0:65, :, :], ["qrT%d" % b], [("QR", t)], "qrT%d" % b)
            P.barrier()
            if skip():
                return
            with contextlib.ExitStack() as st:
                krT = sb(st, "krTa", [65, S], BF16)
                dma("sp", krT[:], KR_d.ap(), [], ["krTa"], "krTa")
                causal_f = sb(st, "causal_f", [128, 128], F32)
                causal = sb(st, "causal", [128, 128], BF16)
                dma("sp", causal_f[:], CD["causal"].ap(), [], ["causal_f"], "causal_f")
                P.add("dve", lambda e: e.tensor_copy(out=causal[:], in_=causal_f[:]), ["causal_f"], ["causal"])
                KT = [sb(st, "KTh", [128, S], BF16) for _ in range(2)]
                QT = [sb(st, "QTh", [128, S], BF16) for _ in range(2)]
                QR = [sb(st, "QRh", [65, S], BF16) for _ in range(2)]
                Vh = [sb(st, "Vh", [128, NT, 130], BF16) for _ in range(2)]
                pT = [sb(st, "pTa", [128, 512], BF16) for _ in range(2)]
                ob = [sb(st, "ob", [128, 128], BF16) for _ in range(2)]
                rsum = sb(st, "rsum", [128, 2], F32)
                QG = 512 if S >= 512 else S
                NQB = QG // 128
                it = 0
                oc = 0
                for h in range(8):
                    hb_ = h % 2
                    dma("sp", KT[hb_][:], KT_d.ap()[h], [], ["KT%d" % hb_], "KT%d" % hb_)
                    dma("sp", QT[hb_][:], QT_d.ap()[h], [], ["QT%d" % hb_], "QT%d" % hb_)
                    dma("sp", QR[hb_][:], QR_d.ap()[h], [], ["QR%d" % hb_], "QR%d" % hb_)
                    dma("sp", Vh[hb_][:], V_d.ap()[:, h, :].rearrange("(t p) c -> p t c", p=128), [], ["Vh%d" % hb_], "Vh%d" % hb_)
                    for qg in range(S // QG):
                        nkb = (qg + 1) * NQB
                        for kb in range(nkb):
                            sbk = 4 + it % 2
                            pb = it % 2
                            it += 1
                            P.add("pe", lambda e, kb=kb, qg=qg, sbk=sbk, hb_=hb_: e.matmul(psf[sbk][:, 0:QG], lhsT=KT[hb_][:, kb * 128:(kb + 1) * 128],
                                                                                        rhs=QT[hb_][:, qg * QG:(qg + 1) * QG], start=True, stop=False),
                                  ["KT%d" % hb_, "QT%d" % hb_], [PSF[sbk]])
                            P.add("pe", lambda e, kb=kb, qg=qg, sbk=sbk, hb_=hb_: e.matmul(psf[sbk][:, 0:QG], lhsT=krT[:, kb * 128:(kb + 1) * 128],
                                                                                        rhs=QR[hb_][:, qg * QG:(qg + 1) * QG], start=False, stop=True),
                                  ["krTa", "QR%d" % hb_], [PSF[sbk]])
                            P.add("act", lambda e, sbk=sbk, pb=pb: e.activation(out=pT[pb][:, 0:QG], in_=psf[sbk][:, 0:QG], func=AF.Exp, scale=SCALE),
                                  [PSF[sbk]], ["pTa%d" % pb])
                            for qb in range(NQB):
                                gq = qg * NQB + qb
                                if kb > gq:
                                    continue
                                if kb == gq:
                                    P.add("dve", lambda e, pb=pb, qb=qb: e.tensor_tensor(out=pT[pb][:, qb * 128:(qb + 1) * 128], in0=pT[pb][:, qb * 128:(qb + 1) * 128],
                                                                                         in1=causal[:], op=ALU.mult), ["pTa%d" % pb, "causal"], ["pTa%d" % pb])
                                P.add("pe", lambda e, pb=pb, qb=qb, kb=kb, gq=gq, hb_=hb_: e.matmul(psf[qb][:, 0:129], lhsT=pT[pb][:, qb * 128:(qb + 1) * 128],
                                                                                                  rhs=Vh[hb_][:, kb, 0:129], start=(kb == 0), stop=(kb == gq)),
                                      ["pTa%d" % pb, "Vh%d" % hb_], [PSF[qb]])
                                if kb == gq:
                                    o_ = oc % 2
                                    oc += 1
                                    P.add("dve", lambda e, qb=qb: e.reciprocal(out=rsum[:, 0:1], in_=psf[qb][:, 128:129]), [PSF[qb]], ["rsum"])
                                    P.add("dve", lambda e, qb=qb, o_=o_: e.tensor_scalar(out=ob[o_][:], in0=psf[qb][:, 0:128], scalar1=rsum[:, 0:1], scalar2=None, op0=ALU.mult),
                                          [PSF[qb], "rsum"], ["ob%d" % o_])
                                    dma("pool", O_d.ap()[gq * 128:(gq + 1) * 128, h * 128:(h + 1) * 128], ob[o_][:], ["ob%d" % o_], [("O", gq)], "ob%d" % o_)
            P.barrier()
            if skip():
                return
            with contextlib.ExitStack() as st:
                wo = sb(st, "wo", [128, 8, D], BF16)
                stage = [sb(st, "wst", [128, 1024], F32) for _ in range(2)]
                load_w_bf16(st, wo, "wo", W["mla_w_o"].ap()[j], 8, D, stage, 2)
                ln_g = sb(st, "ln_g", [128, D], F32)
                ln_b = sb(st, "ln_b", [128, D], F32)
                dma("sp", ln_g[:], bc_row(W["ln_mix_g"], l * D, D), [], ["ln_g"], "ln_g")
                dma("sp", ln_b[:], bc_row(W["ln_mix_b"], l * D, D), [], ["ln_b"], "ln_b")
                hs = [sb(st, "hs", [128, D], F32) for _ in range(2)]
                orow = [sb(st, "orow", [128, D], BF16) for _ in range(2)]
                oT = sb(st, "oT", [128, 8, 128], BF16)
                z = sb(st, "z", [128, D], F32)
                h1 = [sb(st, "h1", [128, D], F32) for _ in range(2)]
                small = sb(st, "small", [128, 32], F32)
                for t in range(NT):
                    b = t % 2
                    hn = "hs%d" % b
                    dma("sp", hs[b][:], hbuf.ap()[t * 128:(t + 1) * 128, :], [("hbuf", t)], [hn], hn)
                    dma("sp", orow[b][:], O_d.ap()[t * 128:(t + 1) * 128, :], [], ["orow%d" % b], "orow%d" % b)
                    transpose_blocks(orow[b], "orow%d" % b, 8, oT, "oT")
                    for nh in range(2):
                        linear(oT, "oT", 8, wo, "wo", nh * 512, (nh + 1) * 512, nh)
                        P.add("dve", lambda e, nh=nh, b=b: e.scalar_tensor_tensor(out=z[:, nh * 512:(nh + 1) * 512], in0=hs[b][:, nh * 512:(nh + 1) * 512],
                                                                                  scalar=ALPHA, in1=psf[nh][:, :], op0=ALU.mult, op1=ALU.add), [hn, PSF[nh]], ["z"])
                    layer_norm(z, "z", h1[b], "h1%d" % b, ln_g, ln_b, ["ln_g", "ln_b"], small, "small")
                    dma("pool", hbuf.ap()[t * 128:(t + 1) * 128, :], h1[b][:], ["h1%d" % b], [("hbuf", t)], "h1%d" % b)
            P.barrier()

        fin = []
        for l in range(DEPTH):
            if l < NA:
                retention_layer(l)
            else:
                if l == NA:
                    mla_kv()
                mla_layer(l)
            moe_ple(l, l == DEPTH - 1)
        if LIMIT < 999:
            fin.append(dma('sp', y_d.ap(), hbuf.ap(), [], [], 'ydbg'))
            if dbg:
                fin.append(dma('sp', dq_d.ap(), qkvg_d.ap(), [], [], 'ydbg2'))
        P.emit(final_ops=fin)
    return nc, consts_np


_CACHE = {}


def run(inputs, S, E, DEPTH, NA, CAP, cores, dbg=False):
    key = (S, E, DEPTH, NA, CAP, dbg)
    if key not in _CACHE:
        _CACHE[key] = build(S, E, DEPTH, NA, CAP, dbg)
    nc, consts_np = _CACHE[key]
    NT = S // 128
    f32 = lambda a: np.ascontiguousarray(np.asarray(a), dtype=np.float32)
    shared = {}
    for k in ("ret_w_in", "ret_gn_g", "ret_gn_b", "ret_w_out", "mla_w_kv_a", "mla_w_kv_b", "mla_w_q_a", "mla_q_norm_g",
              "mla_w_q_b", "mla_w_o", "ln_mix_g", "ln_mix_b", "ln_ffn_g", "ln_ffn_b", "moe_w_router", "moe_b_router",
              "moe_w_gate_up", "moe_b_gate_up", "moe_w_down", "moe_b_down", "ple_w_gate", "ple_w_proj"):
        shared[k] = f32(inputs[k])
    shared["mla_kv_norm_g"] = f32(inputs["mla_kv_norm_g"]).reshape(1, 256)
    bgu_ = shared["moe_b_gate_up"]
    shared["moe_b_gate_up"] = np.ascontiguousarray(bgu_.reshape(bgu_.shape[0], bgu_.shape[1], 16, 128).transpose(0, 1, 3, 2))
    for k, v in consts_np.items():
        shared["c_" + k] = f32(v)
    x = f32(inputs["x"])
    p = f32(inputs["p"])
    pos = np.asarray(inputs["positions"]).astype(np.int32)
    in_maps = []
    for c in range(cores):
        m = dict(shared)
        m["x"] = x[c]
        m["p"] = np.ascontiguousarray(p[:, c])
        m["pos_tm"] = np.ascontiguousarray(pos[c].reshape(NT, 128).T)
        in_maps.append(m)
    res = run_bass_kernel_spmd(nc, in_maps, core_ids=list(range(cores)))
    return res.results


def kernel(**inputs):
    S, E, DEPTH, NA = 8192, 32, 4, 2
    CAP = 1280
    res = run(inputs, S, E, DEPTH, NA, CAP, cores=4)
    return np.stack([r["y"] for r in res], axis=0).astype(np.float32)
```

```python
import contextlib
import numpy as np
import concourse.bass as bass
import concourse.mybir as mybir
from concourse.bass_utils import run_bass_kernel_spmd

F32 = mybir.dt.float32
BF16 = mybir.dt.bfloat16
I32 = mybir.dt.int32
U32 = mybir.dt.uint32
AF = mybir.ActivationFunctionType
ALU = mybir.AluOpType

D = 1024
SEM_ROT = 30000
DN_EPS = 1e-5


import types


def _snap(fn):
    if fn.__closure__ is None:
        return fn
    cells = []
    for c in fn.__closure__:
        try:
            cells.append(types.CellType(c.cell_contents))
        except ValueError:
            cells.append(c)
    g = types.FunctionType(fn.__code__, fn.__globals__, fn.__name__, fn.__defaults__, tuple(cells))
    g.__kwdefaults__ = fn.__kwdefaults__
    return g


class Op:
    __slots__ = ("eng", "fn", "deps", "signal", "event", "dma_key")

    def __init__(self, eng, fn, deps, dma_key):
        self.eng = eng
        self.fn = fn
        self.deps = deps
        self.signal = dma_key is not None
        self.event = None
        self.dma_key = dma_key


class Prog:
    ENGS = ("pe", "act", "dve", "pool", "sp")

    def __init__(self, nc):
        self.nc = nc
        self.ops = {e: [] for e in self.ENGS}
        self.last_w = {}
        self.readers = {}
        self.all_ops = []
        self.pending = {}
        self.last_dma = {}

    def add(self, eng, fn, reads=(), writes=(), dma_key=None):
        pr = [r for r in reads if isinstance(r, str) and r.startswith(("psf", "psb"))]
        if pr:
            reads = [r for r in reads if r not in pr]
            writes = list(writes) + [r for r in pr if r not in writes]
        deps = []
        for r in reads:
            w = self.last_w.get(r)
            if w is not None:
                deps.append(w)
        for w_ in writes:
            w = self.last_w.get(w_)
            if w is not None:
                deps.append(w)
            deps.extend(self.readers.get(w_, ()))
        if eng in self.pending:
            deps.extend(self.pending.pop(eng))
        op = Op(eng, _snap(fn), deps, dma_key)
        for r in reads:
            self.readers.setdefault(r, []).append(op)
        for w_ in writes:
            self.last_w[w_] = op
            self.readers[w_] = []
        self.ops[eng].append(op)
        self.all_ops.append(op)
        if dma_key is not None:
            self.last_dma[dma_key] = op
        return op

    def barrier(self):
        deps = [self.ops[e][-1] for e in self.ENGS if self.ops[e]]
        deps += list(self.last_dma.values())
        for e in self.ENGS:
            self.pending[e] = list(deps) + self.pending.get(e, [])
        self.last_w = {}
        self.readers = {}

    def emit(self, final_ops=()):
        nc = self.nc
        for op in self.all_ops:
            for d in op.deps:
                if d is op:
                    continue
                if d.eng == "pe" and op.eng == "pe" and d.dma_key is None and op.dma_key is None:
                    continue
                d.signal = True
        for op in final_ops:
            op.signal = True
        eng_cnt = {e: 0 for e in self.ENGS}
        key_cnt = {}
        names = []
        for op in self.all_ops:
            if op.dma_key is not None:
                k = ("dma", op.dma_key)
                key_cnt[k] = key_cnt.get(k, 0) + 16
                op.event = (k, key_cnt[k])
                if k not in names:
                    names.append(k)
        for e in self.ENGS:
            for op in self.ops[e]:
                if op.dma_key is not None:
                    continue
                elif op.signal:
                    eng_cnt[e] += 1
                    c = eng_cnt[e]
                    k = ("eng", e, (c - 1) // SEM_ROT)
                    op.event = (k, (c - 1) % SEM_ROT + 1)
                else:
                    continue
                if op.event[0] not in names:
                    names.append(op.event[0])
        self.n_sems = len(names)
        with contextlib.ExitStack() as st:
            sems = {}
            for i, k in enumerate(names):
                sems[k] = st.enter_context(nc.semaphore("s%d" % i))
            block = st.enter_context(nc.Block())
            engmap = {"pe": block.tensor, "act": block.scalar, "dve": block.vector,
                      "pool": block.gpsimd, "sp": block.sync}
            for e in self.ENGS:
                ops = self.ops[e]
                if not ops and e != "sp":
                    continue

                def body(eng, ops=ops, e=e):
                    known = {}
                    for op in ops:
                        need = {}
                        for d in op.deps:
                            if d is op or d.event is None:
                                continue
                            if d.eng == "pe" and e == "pe" and d.dma_key is None and op.dma_key is None:
                                continue
                            k, v = d.event
                            if need.get(k, 0) < v:
                                need[k] = v
                        for k, v in need.items():
                            if known.get(k, 0) < v:
                                eng.wait_ge(sems[k], v)
                                known[k] = v
                        ins = op.fn(eng)
                        if op.event is not None:
                            ins.then_inc(sems[op.event[0]], 16 if op.dma_key is not None else 1)
                    if e == "sp":
                        for op in final_ops:
                            k, v = op.event
                            if known.get(k, 0) < v:
                                eng.wait_ge(sems[k], v)
                                known[k] = v
                engmap[e](body)


def model_consts(E):
    H, dk, C = 4, 256, 128
    log_g = np.log(1.0 - 2.0 ** (-5.0 - np.arange(H, dtype=np.float64)))
    idx = np.arange(C, dtype=np.float64)
    diff = idx[:, None] - idx[None, :]
    intra = np.where(diff >= 0, np.exp(log_g[:, None, None] * np.maximum(diff, 0.0)), 0.0)
    maskT = np.transpose(intra, (0, 2, 1)) * dk ** -0.5
    decay_q = np.exp(log_g[None, :] * (idx[:, None] + 1.0))
    decay_k = np.exp(log_g[None, :] * (C - 1.0 - idx[:, None])) * dk ** -0.5
    decay_chunk = np.exp(log_g * C)
    inv_r = (10000.0 ** (-np.arange(0, 256, 2, dtype=np.float32) / np.float32(256))).astype(np.float32)
    inv_m = (10000.0 ** (-np.arange(0, 64, 2, dtype=np.float32) / np.float32(64))).astype(np.float32)
    c = {}
    c["ident"] = np.eye(128, dtype=np.float32)
    c["maskT"] = np.ascontiguousarray(np.transpose(maskT, (1, 0, 2))).astype(np.float32)
    c["dq"] = decay_q.astype(np.float32)
    c["dk"] = decay_k.astype(np.float32)
    c["inv_r"] = np.tile(inv_r[None, :], (128, 1)).astype(np.float32)
    c["inv_m"] = np.tile(inv_m[None, :], (128, 1)).astype(np.float32)
    c["causal"] = (np.arange(128)[:, None] <= np.arange(128)[None, :]).astype(np.float32)
    c["tri"] = (np.arange(128)[:, None] < np.arange(128)[None, :]).astype(np.float32)
    c["iota_e"] = np.tile(np.arange(E, dtype=np.float32)[None, :], (128, 1))
    return c, [float(v) for v in decay_chunk]


CONST_SHAPES = lambda E: {"ident": [128, 128], "maskT": [128, 4, 128], "dq": [128, 4], "dk": [128, 4],
                          "inv_r": [128, 128], "inv_m": [128, 32], "causal": [128, 128], "tri": [128, 128],
                          "iota_e": [128, E]}


def build(S, E, DEPTH, NA, CAP, dbg=False):
    import os
    LIMIT = int(os.environ.get('KLIMIT', '999'))
    PH = [0]

    def skip():
        PH[0] += 1
        return PH[0] > LIMIT
    NT = S // 128
    NB = DEPTH - NA
    TOPK = 4
    ALPHA = float((2 * DEPTH) ** 0.25)
    SCALE = float(192 ** -0.5)
    NSLOT = E * CAP
    CT = CAP // 128
    consts_np, dchunk = model_consts(E)

    nc = bass.Bass("TRN2", target_bir_lowering=False)

    def din(name, shape, dt=F32):
        return nc.dram_tensor(name, list(shape), dt, kind="ExternalInput")

    x_d = din("x", [S, D])
    p_d = din("p", [DEPTH, S, 256])
    pos_d = din("pos_tm", [128, NT], I32)
    W = {}
    W["ret_w_in"] = din("ret_w_in", [NA, D, 6144])
    W["ret_gn_g"] = din("ret_gn_g", [NA, 2048])
    W["ret_gn_b"] = din("ret_gn_b", [NA, 2048])
    W["ret_w_out"] = din("ret_w_out", [NA, 2048, D])
    W["mla_w_kv_a"] = din("mla_w_kv_a", [D, 320])
    W["mla_kv_norm_g"] = din("mla_kv_norm_g", [1, 256])
    W["mla_w_kv_b"] = din("mla_w_kv_b", [256, 2048])
    W["mla_w_q_a"] = din("mla_w_q_a", [NB, D, 256])
    W["mla_q_norm_g"] = din("mla_q_norm_g", [NB, 256])
    W["mla_w_q_b"] = din("mla_w_q_b", [NB, 256, 1536])
    W["mla_w_o"] = din("mla_w_o", [NB, D, D])
    for n in ("ln_mix_g", "ln_mix_b", "ln_ffn_g", "ln_ffn_b"):
        W[n] = din(n, [DEPTH, D])
    W["moe_w_router"] = din("moe_w_router", [DEPTH, D, E])
    W["moe_b_router"] = din("moe_b_router", [DEPTH, E])
    W["moe_w_gate_up"] = din("moe_w_gate_up", [DEPTH, E, D, 2048])
    W["moe_b_gate_up"] = din("moe_b_gate_up", [DEPTH, E, 128, 16])
    W["moe_w_down"] = din("moe_w_down", [DEPTH, E, D, D])
    W["moe_b_down"] = din("moe_b_down", [DEPTH, E, D])
    W["ple_w_gate"] = din("ple_w_gate", [DEPTH, D, D])
    W["ple_w_proj"] = din("ple_w_proj", [DEPTH, 256, D])
    CD = {k: din("c_" + k, shp) for k, shp in CONST_SHAPES(E).items()}
    y_d = nc.dram_tensor("y", [S, D], F32, kind="ExternalOutput")
    dbg_d = nc.dram_tensor("dbg", [DEPTH, S, D], F32, kind="ExternalOutput") if dbg else None
    dq_d = nc.dram_tensor("dq", [S, 6144], BF16, kind="ExternalOutput") if dbg else None
    dq2_d = nc.dram_tensor("dq2", [S, 6144], BF16, kind="ExternalOutput") if dbg else None

    hbuf = nc.dram_tensor("hbuf", [S, D], F32)
    qkvg_d = nc.dram_tensor("qkvg", [S, 6144], BF16)
    xs_d = nc.dram_tensor("xs", [NSLOT, D], BF16)
    ys_d = nc.dram_tensor("ys", [NSLOT, D], F32)
    KT_d = nc.dram_tensor("KT", [8, 128, S], BF16)
    KR_d = nc.dram_tensor("KR", [65, S], BF16)
    V_d = nc.dram_tensor("Vx", [S, 8, 130], BF16)
    QT_d = nc.dram_tensor("QT", [8, 128, S], BF16)
    QR_d = nc.dram_tensor("QR", [8, 65, S], BF16)
    O_d = nc.dram_tensor("Oa", [S, D], BF16)

    P = Prog(nc)
    uid = [0]

    def bc_row(handle, row_off, n):
        return bass.AP(handle, row_off, [[0, 128], [1, n]])

    with contextlib.ExitStack() as root:
        sbstate = {"cur": 0, "persist": 0, "st": None}
        DTB = {F32: 4, BF16: 2, I32: 4, U32: 4}

        def sb(st, name, shape, dt):
            uid[0] += 1
            nb = DTB[dt]
            for d_ in shape[1:]:
                nb *= d_
            nb = (nb + 63) // 64 * 64
            if True:
                return nc.alloc_sbuf_tensor("%s_%d" % (name, uid[0]), list(shape), dt) if st is root else st.enter_context(nc.sbuf_tensor("%s_%d" % (name, uid[0]), list(shape), dt))
            if st is root:
                assert sbstate["st"] is None
                off = sbstate["persist"]
                sbstate["persist"] += nb
            else:
                if sbstate["st"] is not st:
                    sbstate["st"] = st
                    sbstate["cur"] = sbstate["persist"]
                off = sbstate["cur"]
                sbstate["cur"] += nb
                assert sbstate["cur"] <= 190 * 1024, ("SBUF overflow", name, sbstate["cur"])
            return nc.alloc_sbuf_tensor_at("%s_%d" % (name, uid[0]), list(shape), dt, offset=off)

        psf = [root.enter_context(nc.psum_tensor("psf%d" % i, [128, 512], F32)) for i in range(6)]
        psb = [root.enter_context(nc.psum_tensor("psb%d" % i, [128, 1024], BF16)) for i in range(2)]
        PSF = ["psf%d" % i for i in range(6)]
        PSB = ["psb%d" % i for i in range(2)]

        ident_f = sb(root, "ident_f", [128, 128], F32)
        ident_b = sb(root, "ident_b", [128, 128], BF16)
        ones_b = sb(root, "ones_b", [128, 128], BF16)
        ones_f = sb(root, "ones_f", [128, 128], F32)
        eps_ln = sb(root, "eps_ln", [128, 1], F32)
        eps_6 = sb(root, "eps_6", [128, 1], F32)
        posf = sb(root, "posf", [128, NT], F32)
        posi = sb(root, "posi", [128, NT], I32)
        inv_r = sb(root, "inv_r", [128, 128], F32)
        inv_m = sb(root, "inv_m", [128, 32], F32)
        slots_i = sb(root, "slots_i", [128, NT, TOPK], I32)
        gates_s = sb(root, "gates_s", [128, NT, TOPK], F32)

        def dma(eng, out, in_, reads, writes, key):
            return P.add(eng, lambda e: e.dma_start(out=out, in_=in_), reads, writes, dma_key=key)

        dma("sp", ident_f[:], CD["ident"].ap(), [], ["ident_f"], "ident_f")
        dma("sp", posi[:], pos_d.ap(), [], ["posi"], "posi")
        dma("sp", inv_r[:], CD["inv_r"].ap(), [], ["inv_r"], "inv_r")
        dma("sp", inv_m[:], CD["inv_m"].ap(), [], ["inv_m"], "inv_m")
        P.add("dve", lambda e: e.tensor_copy(out=ident_b[:], in_=ident_f[:]), ["ident_f"], ["ident_b"])
        P.add("dve", lambda e: e.tensor_copy(out=posf[:], in_=posi[:]), ["posi"], ["posf"])
        zeros_f = sb(root, "zeros_f", [128, 1024], F32)
        P.add("dve", lambda e: e.memset(zeros_f[:], 0.0), [], ["zeros_f"])
        P.add("pool", lambda e: e.memset(ones_f[:], 1.0), [], ["ones_f"])
        P.add("pool", lambda e: e.tensor_copy(out=ones_b[:], in_=ones_f[:]), ["ones_f"], ["ones_b"])
        P.add("pool", lambda e: e.memset(eps_ln[:], DN_EPS), [], ["eps_ln"])
        P.add("pool", lambda e: e.memset(eps_6[:], 1e-6), [], ["eps_6"])
        dma("sp", hbuf.ap(), x_d.ap(), [], ["hbuf"], "x2h")
        P.barrier()

        rr = {"cast": 0, "ps": 0}

        def load_w_bf16(st, dst, dst_name, src2d, KC, N, stage, nstage, engs=("act", "pool")):
            CH = stage[0].shape[1]
            i = 0
            for kc in range(KC):
                for n0 in range(0, N, CH):
                    n1 = min(N, n0 + CH)
                    b = rr["cast"] % nstage
                    rr["cast"] += 1
                    sname = "wst%d" % b
                    dma("sp", stage[b][:, 0:n1 - n0], src2d[kc * 128:(kc + 1) * 128, n0:n1], [], [sname], sname)
                    eng = engs[rr["cast"] % len(engs)]
                    if eng == "act":
                        P.add("act", lambda e, b=b, kc=kc, n0=n0, n1=n1: e.copy(out=dst[:, kc, n0:n1], in_=stage[b][:, 0:n1 - n0]),
                              [sname], [dst_name])
                    else:
                        P.add(eng, lambda e, b=b, kc=kc, n0=n0, n1=n1: e.tensor_copy(out=dst[:, kc, n0:n1], in_=stage[b][:, 0:n1 - n0]),
                              [sname], [dst_name])
                    i += 1

        def transpose_blocks(src, src_name, nblk, dst, dst_name, rows=128, blkw=128, src_off=0):
            for g0 in range(0, nblk, 8):
                g1 = min(nblk, g0 + 8)
                pb = rr["ps"] % 2
                rr["ps"] += 1
                for j in range(g0, g1):
                    P.add("pe", lambda e, j=j, pb=pb, g0=g0: e.transpose(
                        out=psb[pb][0:blkw, (j - g0) * 128:(j - g0 + 1) * 128],
                        in_=src[:, src_off + j * blkw: src_off + (j + 1) * blkw], identity=ident_b[:]),
                        [src_name, "ident_b"], [PSB[pb]])
                P.add("act", lambda e, pb=pb, g0=g0, g1=g1: e.copy(
                    out=dst[0:blkw, g0:g1, :], in_=psb[pb][0:blkw, 0:(g1 - g0) * 128].rearrange("p (j t) -> p j t", t=128)),
                    [PSB[pb]], [dst_name])

        def rstd_from(var_ap, eps_tile, out_ap, tmp_ap, names_r, name_w, scale=1.0):
            epsv = DN_EPS if eps_tile is eps_ln else 1e-6
            P.add("dve", lambda e: e.tensor_scalar(out=tmp_ap, in0=var_ap, scalar1=float(scale), scalar2=float(epsv), op0=ALU.mult, op1=ALU.add),
                  list(names_r), [name_w + "_t"])
            P.add("act", lambda e: e.activation(out=tmp_ap, in_=tmp_ap, func=AF.Ln), [name_w + "_t"], [name_w + "_t"])
            P.add("act", lambda e: e.activation(out=out_ap, in_=tmp_ap, func=AF.Exp, scale=-0.5),
                  [name_w + "_t"], [name_w])

        def layer_norm(z, zname, out, oname, g_t, b_t, gb_names, small, sname):
            for c in range(2):
                P.add("dve", lambda e, c=c: e.bn_stats(out=small[:, c * 6:(c + 1) * 6], in_=z[:, c * 512:(c + 1) * 512]),
                      [zname], [sname])
            P.add("dve", lambda e: e.bn_aggr(out=small[:, 12:14], in_=small[:, 0:12].rearrange("p (c s) -> p c s", s=6)),
                  [sname], [sname])
            rstd_from(small[:, 13:14], eps_ln, small[:, 15:16], small[:, 14:15], [sname], sname + "r")
            P.add("dve", lambda e: e.tensor_scalar(out=out[:], in0=z[:], scalar1=small[:, 12:13], scalar2=small[:, 15:16],
                                                   op0=ALU.subtract, op1=ALU.mult), [zname, sname, sname + "r"], [oname])
            P.add("pool", lambda e: e.tensor_tensor(out=out[:], in0=out[:], in1=g_t[:], op=ALU.mult), [oname, gb_names[0]], [oname])
            P.add("pool", lambda e: e.tensor_tensor(out=out[:], in0=out[:], in1=b_t[:], op=ALU.add), [oname, gb_names[1]], [oname])

        def rope_tables(t, inv_t, nf, cos_t, sin_t, tmpf, tmpi, tag):
            for which, dst, shift in (("s", sin_t, 0.0), ("c", cos_t, float(np.pi / 2))):
                nm = tag + which
                P.add("dve", lambda e, dst=dst, shift=shift: e.tensor_scalar(
                    out=dst[:, 0:nf], in0=inv_t[:, 0:nf], scalar1=posf[:, t:t + 1], scalar2=shift, op0=ALU.mult, op1=ALU.add),
                    ["posf", "inv"], [nm])
                P.add("dve", lambda e, dst=dst: e.tensor_scalar(out=tmpf[:, 0:nf], in0=dst[:, 0:nf], scalar1=float(1 / (2 * np.pi)),
                                                                scalar2=None, op0=ALU.mult), [nm], [tag + "tf"])
                P.add("dve", lambda e: e.tensor_copy(out=tmpi[:, 0:nf], in_=tmpf[:, 0:nf]), [tag + "tf"], [tag + "ti"])
                P.add("dve", lambda e: e.tensor_copy(out=tmpf[:, 0:nf], in_=tmpi[:, 0:nf]), [tag + "ti"], [tag + "tf"])
                P.add("dve", lambda e, dst=dst: e.scalar_tensor_tensor(out=dst[:, 0:nf], in0=tmpf[:, 0:nf], scalar=float(-2 * np.pi),
                                                                       in1=dst[:, 0:nf], op0=ALU.mult, op1=ALU.add), [tag + "tf", nm], [nm])
                P.add("dve", lambda e, dst=dst: e.tensor_scalar(out=tmpf[:, 0:nf], in0=dst[:, 0:nf], scalar1=float(np.pi), scalar2=float(-2 * np.pi),
                                                                op0=ALU.is_gt, op1=ALU.mult), [nm], [tag + "tf"])
                P.add("dve", lambda e, dst=dst: e.tensor_tensor(out=dst[:, 0:nf], in0=dst[:, 0:nf], in1=tmpf[:, 0:nf], op=ALU.add), [nm, tag + "tf"], [nm])
                P.add("dve", lambda e, dst=dst: e.tensor_scalar(out=tmpf[:, 0:nf], in0=dst[:, 0:nf], scalar1=float(-np.pi), scalar2=float(2 * np.pi),
                                                                op0=ALU.is_lt, op1=ALU.mult), [nm], [tag + "tf"])
                P.add("dve", lambda e, dst=dst: e.tensor_tensor(out=dst[:, 0:nf], in0=dst[:, 0:nf], in1=tmpf[:, 0:nf], op=ALU.add), [nm, tag + "tf"], [nm])
                P.add("act", lambda e, dst=dst: e.activation(out=dst[:, 0:nf], in_=dst[:, 0:nf], func=AF.Sin), [nm], [nm])

        def rope_apply(x1, x2, o1, o2, cos_t, sin_t, nf, t1, t2, rnames, wname, tag):
            P.add("dve", lambda e: e.tensor_tensor(out=t1[:, 0:nf], in0=x1, in1=cos_t[:, 0:nf], op=ALU.mult), rnames + [tag + "c"], [tag + "t1"])
            P.add("dve", lambda e: e.tensor_tensor(out=t2[:, 0:nf], in0=x2, in1=sin_t[:, 0:nf], op=ALU.mult), rnames + [tag + "s"], [tag + "t2"])
            P.add("pool", lambda e: e.tensor_tensor(out=o1, in0=t1[:, 0:nf], in1=t2[:, 0:nf], op=ALU.subtract), [tag + "t1", tag + "t2"], [wname])
            P.add("dve", lambda e: e.tensor_tensor(out=t1[:, 0:nf], in0=x1, in1=sin_t[:, 0:nf], op=ALU.mult), rnames + [tag + "s"], [tag + "t1"])
            P.add("dve", lambda e: e.tensor_tensor(out=t2[:, 0:nf], in0=x2, in1=cos_t[:, 0:nf], op=ALU.mult), rnames + [tag + "c"], [tag + "t2"])
            P.add("pool", lambda e: e.tensor_tensor(out=o2, in0=t1[:, 0:nf], in1=t2[:, 0:nf], op=ALU.add), [tag + "t1", tag + "t2"], [wname])

        def linear(xT, xT_name, KC, w, w_name, n0, n1, bank, extra=None):
            for kc in range(KC):
                P.add("pe", lambda e, kc=kc: e.matmul(psf[bank][:, 0:n1 - n0], lhsT=xT[:, kc, :], rhs=w[:, kc, n0:n1],
                                                     start=(kc == 0), stop=(kc == KC - 1 and extra is None)),
                      [xT_name, w_name], [PSF[bank]])

        def retention_layer(l):
            if skip():
                return
            with contextlib.ExitStack() as st:
                win = sb(st, "win", [128, 8, 6144], BF16)
                stage = [sb(st, "wst", [128, 2048], F32) for _ in range(2)]
                load_w_bf16(st, win, "win", W["ret_w_in"].ap()[l], 8, 6144, stage, 2)
                hs = [sb(st, "hs", [128, D], F32) for _ in range(2)]
                hb = sb(st, "hb", [128, D], BF16)
                hT = sb(st, "hT", [128, 8, 128], BF16)
                row = [sb(st, "row", [128, 6144], BF16) for _ in range(2)]
                cos_t = sb(st, "cos", [128, 128], F32)
                sin_t = sb(st, "sin", [128, 128], F32)
                tf = sb(st, "tf", [128, 128], F32)
                ti = sb(st, "ti", [128, 128], I32)
                t1 = sb(st, "t1", [128, 128], F32)
                t2 = sb(st, "t2", [128, 128], F32)
                P.last_w["inv"] = P.last_w.get("inv_r")
                for t in range(NT):
                    b = t % 2
                    dma("sp", hs[b][:], hbuf.ap()[t * 128:(t + 1) * 128, :], [("hbuf", t)], ["hs%d" % b], "hs%d" % b)
                    P.add("pool", lambda e, b=b: e.tensor_copy(out=hb[:], in_=hs[b][:]), ["hs%d" % b], ["hb"])
                    transpose_blocks(hb, "hb", 8, hT, "hT")
                    rope_tables(t, inv_r, 128, cos_t, sin_t, tf, ti, "rp")
                    rw = "row%d" % b
                    for n in range(12):
                        bank = n % 4
                        linear(hT, "hT", 8, win, "win", n * 512, (n + 1) * 512, bank)
                        if n < 4:
                            for hh in range(2):
                                c0 = hh * 256
                                rope_apply(psf[bank][:, c0:c0 + 128], psf[bank][:, c0 + 128:c0 + 256],
                                           row[b][:, n * 512 + c0: n * 512 + c0 + 128], row[b][:, n * 512 + c0 + 128: n * 512 + c0 + 256],
                                           cos_t, sin_t, 128, t1, t2, [PSF[bank]], rw, "rp")
                        elif n < 8:
                            P.add("act", lambda e, n=n, b=b, bank=bank: e.copy(out=row[b][:, n * 512:(n + 1) * 512], in_=psf[bank][:, :]),
                                  [PSF[bank]], [rw])
                        else:
                            P.add("act", lambda e, n=n, b=b, bank=bank: e.activation(out=row[b][:, n * 512:(n + 1) * 512], in_=psf[bank][:, :], func=AF.Silu),
                                  [PSF[bank]], [rw])
                    dma("act", qkvg_d.ap()[t * 128:(t + 1) * 128, :], row[b][:], [rw], [("qkvg", t)], rw)
            P.barrier()
            if dbg and l == 0:
                dma("sp", dq2_d.ap(), qkvg_d.ap(), [], [], "dq2dbg")
                P.barrier()
            if skip():
                return
            with contextlib.ExitStack() as st:
                wout = sb(st, "wout", [128, 16, D], BF16)
                stage = [sb(st, "wst", [128, 1024], F32) for _ in range(2)]
                load_w_bf16(st, wout, "wout", W["ret_w_out"].ap()[l], 16, D, stage, 2)
                gn_g = sb(st, "gn_g", [128, 2048], F32)
                gn_b = sb(st, "gn_b", [128, 2048], F32)
                ln_g = sb(st, "ln_g", [128, D], F32)
                ln_b = sb(st, "ln_b", [128, D], F32)
                dma("sp", gn_g[:], bc_row(W["ret_gn_g"], l * 2048, 2048), [], ["gn_g"], "gn_g")
                dma("sp", gn_b[:], bc_row(W["ret_gn_b"], l * 2048, 2048), [], ["gn_b"], "gn_b")
                dma("sp", ln_g[:], bc_row(W["ln_mix_g"], l * D, D), [], ["ln_g"], "ln_g")
                dma("sp", ln_b[:], bc_row(W["ln_mix_b"], l * D, D), [], ["ln_b"], "ln_b")
                maskT = sb(st, "maskT", [128, 4, 128], F32)
                dq = sb(st, "dq", [128, 4], F32)
                dkc = sb(st, "dkc", [128, 4], F32)
                dma("sp", maskT[:], CD["maskT"].ap(), [], ["maskT"], "maskT")
                dma("sp", dq[:], CD["dq"].ap(), [], ["dq"], "dq")
                dma("sp", dkc[:], CD["dk"].ap(), [], ["dkc"], "dkc")
                Sf = sb(st, "Sf", [128, 8, 512], F32)
                Sb = sb(st, "Sb", [128, 8, 512], BF16)
                P.add("dve", lambda e: e.memset(Sf[:], 0.0), [], ["Sf"])
                for i_ in range(8):
                    P.add("dve", lambda e, i_=i_: e.tensor_copy(out=Sb[:, i_, :], in_=zeros_f[:, 0:512]), ["zeros_f"], ["Sb"])
                row = [sb(st, "row", [128, 6144], BF16) for _ in range(2)]
                hs = [sb(st, "hs", [128, D], F32) for _ in range(2)]
                qd = sb(st, "qd", [128, D], BF16)
                kd = sb(st, "kd", [128, D], BF16)
                qT = sb(st, "qT", [128, 8, 128], BF16)
                qdT = sb(st, "qdT", [128, 8, 128], BF16)
                kT = sb(st, "kT", [128, 8, 128], BF16)
                innerT = sb(st, "innerT", [128, 128], BF16)
                yn = sb(st, "yn", [128, 512], F32)
                yg = sb(st, "yg", [128, 2048], BF16)
                ygT = sb(st, "ygT", [128, 16, 128], BF16)
                z = sb(st, "z", [128, D], F32)
                h1 = [sb(st, "h1", [128, D], F32) for _ in range(2)]
                small = sb(st, "small", [128, 32], F32)
                gsm = sb(st, "gsm", [128, 32], F32)
                ydb = sb(st, "ydb", [128, 512], F32)
                for t in range(NT):
                    b = t % 2
                    rw = "row%d" % b
                    R = row[b]
                    dma("sp", R[:], qkvg_d.ap()[t * 128:(t + 1) * 128, :], [("qkvg", t)], [rw], rw)
                    dma("sp", hs[b][:], hbuf.ap()[t * 128:(t + 1) * 128, :], [("hbuf", t)], ["hs%d" % b], "hs%d" % b)
                    for h in range(4):
                        P.add("dve", lambda e, h=h, R=R: e.tensor_scalar(out=qd[:, h * 256:(h + 1) * 256], in0=R[:, h * 256:(h + 1) * 256],
                                                                          scalar1=dq[:, h:h + 1], scalar2=None, op0=ALU.mult), [rw, "dq"], ["qd"])
                        P.add("pool", lambda e, h=h, R=R: e.tensor_scalar(out=kd[:, h * 256:(h + 1) * 256], in0=R[:, 1024 + h * 256:1024 + (h + 1) * 256],
                                                                           scalar1=dkc[:, h:h + 1], scalar2=None, op0=ALU.mult), [rw, "dkc"], ["kd"])
                    transpose_blocks(R, rw, 8, qT, "qT", src_off=0)
                    transpose_blocks(R, rw, 8, kT, "kT", src_off=1024)
                    transpose_blocks(qd, "qd", 8, qdT, "qdT")
                    for h in range(4):
                        vs = R[:, 2048 + h * 512: 2048 + (h + 1) * 512]
                        for j in range(2):
                            P.add("pe", lambda e, h=h, j=j: e.matmul(psf[4][:, 0:128], lhsT=kT[:, 2 * h + j, :], rhs=qT[:, 2 * h + j, :],
                                                                    start=(j == 0), stop=(j == 1)), ["kT", "qT"], [PSF[4]])
                        P.add("dve", lambda e, h=h: e.tensor_tensor(out=innerT[:], in0=psf[4][:, 0:128], in1=maskT[:, h, :], op=ALU.mult),
                              [PSF[4], "maskT"], ["innerT"])
                        yb = h % 2
                        NOACC = os.environ.get("KNOACC", "") == "1"
                        P.add("pe", lambda e, vs=vs, yb=yb: e.matmul(psf[yb][:, :], lhsT=innerT[:], rhs=vs, start=True, stop=NOACC),
                              ["innerT", rw], [PSF[yb]])
                        for j in range(0 if NOACC else 2):
                            P.add("pe", lambda e, h=h, j=j, yb=yb: e.matmul(psf[yb][:, :], lhsT=qdT[:, 2 * h + j, :], rhs=Sb[:, 2 * h + j, :],
                                                                           start=False, stop=(j == 1)), ["qdT", "Sb"], [PSF[yb]])
                        if os.environ.get("KDBG", "") in ("y", "rv") and h == 3:
                            P.add("dve", lambda e, yb=yb, b=b: e.tensor_copy(out=ydb[:], in_=psf[yb][:, :]), [PSF[yb]], ["ydb"])
                        P.add("dve", lambda e, yb=yb: e.bn_stats(out=gsm[:, 0:6], in_=psf[yb][:, :]), [PSF[yb]], ["gsm"])
                        P.add("dve", lambda e: e.bn_aggr(out=gsm[:, 6:8], in_=gsm[:, 0:6]), ["gsm"], ["gsm"])
                        rstd_from(gsm[:, 7:8], eps_6, gsm[:, 9:10], gsm[:, 8:9], ["gsm"], "gsmr")
                        P.add("dve", lambda e, yb=yb: e.tensor_scalar(out=yn[:], in0=psf[yb][:, :], scalar1=gsm[:, 6:7], scalar2=gsm[:, 9:10],
                                                                      op0=ALU.subtract, op1=ALU.mult), [PSF[yb], "gsm", "gsmr"], ["yn"])
                        P.add("pool", lambda e, h=h: e.tensor_tensor(out=yn[:], in0=yn[:], in1=gn_g[:, h * 512:(h + 1) * 512], op=ALU.mult), ["yn", "gn_g"], ["yn"])
                        P.add("pool", lambda e, h=h: e.tensor_tensor(out=yn[:], in0=yn[:], in1=gn_b[:, h * 512:(h + 1) * 512], op=ALU.add), ["yn", "gn_b"], ["yn"])
                        P.add("pool", lambda e, h=h, R=R: e.tensor_tensor(out=yg[:, h * 512:(h + 1) * 512], in0=yn[:], in1=R[:, 4096 + h * 512:4096 + (h + 1) * 512],
                                                                           op=ALU.mult), ["yn", rw], ["yg"])
                        for j in range(2):
                            sbk = 2 + j
                            P.add("pe", lambda e, h=h, j=j, vs=vs, sbk=sbk: e.matmul(psf[sbk][:, :], lhsT=kd[:, h * 256 + j * 128: h * 256 + (j + 1) * 128],
                                                                                    rhs=vs, start=True, stop=True), ["kd", rw], [PSF[sbk]])
                            P.add("dve", lambda e, h=h, j=j, sbk=sbk: e.scalar_tensor_tensor(out=Sf[:, 2 * h + j, :], in0=Sf[:, 2 * h + j, :], scalar=dchunk[h],
                                                                                             in1=psf[sbk][:, :], op0=ALU.mult, op1=ALU.add),
                                  ["Sf", PSF[sbk]], ["Sf"])
                            P.add("dve", lambda e, h=h, j=j: e.tensor_copy(out=Sb[:, 2 * h + j, :], in_=Sf[:, 2 * h + j, :]), ["Sf"], ["Sb"])
                    transpose_blocks(yg, "yg", 16, ygT, "ygT")
                    for nh in range(2):
                        for fc in range(16):
                            P.add("pe", lambda e, nh=nh, fc=fc: e.matmul(psf[nh][:, :], lhsT=ygT[:, fc, :], rhs=wout[:, fc, nh * 512:(nh + 1) * 512],
                                                                        start=(fc == 0), stop=(fc == 15)), ["ygT", "wout"], [PSF[nh]])
                        P.add("dve", lambda e, nh=nh, b=b: e.scalar_tensor_tensor(out=z[:, nh * 512:(nh + 1) * 512], in0=hs[b][:, nh * 512:(nh + 1) * 512],
                                                                                  scalar=ALPHA, in1=psf[nh][:, :], op0=ALU.mult, op1=ALU.add),
                              ["hs%d" % b, PSF[nh]], ["z"])
                    layer_norm(z, "z", h1[b], "h1%d" % b, ln_g, ln_b, ["ln_g", "ln_b"], small, "small")
                    kd_ = os.environ.get("KDBG", "")
                    if kd_ == "z":
                        P.add("dve", lambda e, b=b: e.tensor_copy(out=h1[b][:], in_=z[:]), ["z", "h1%d" % b], ["h1%d" % b])
                    elif kd_ == "yg":
                        P.add("dve", lambda e, b=b: e.tensor_copy(out=h1[b][:], in_=yg[:, 0:1024]), ["yg", "h1%d" % b], ["h1%d" % b])
                    elif kd_ == "y":
                        P.add("dve", lambda e, b=b: e.tensor_copy(out=h1[b][:, 0:512], in_=yg[:, 1536:2048]), ["yg", "h1%d" % b], ["h1%d" % b])
                        P.add("dve", lambda e, b=b: e.tensor_copy(out=h1[b][:, 512:1024], in_=ydb[:]), ["ydb", "h1%d" % b], ["h1%d" % b])
                    elif kd_ == "rv":
                        P.add("dve", lambda e, b=b, R=R: e.tensor_copy(out=h1[b][:, 0:512], in_=R[:, 3584:4096]), [rw, "h1%d" % b], ["h1%d" % b])
                        P.add("dve", lambda e, b=b: e.tensor_copy(out=h1[b][:, 512:1024], in_=ydb[:]), ["ydb", "h1%d" % b], ["h1%d" % b])
                    elif kd_ == "yn":
                        P.add("dve", lambda e, b=b: e.tensor_copy(out=h1[b][:, 0:512], in_=yn[:]), ["yn", "h1%d" % b], ["h1%d" % b])
                        P.add("dve", lambda e, b=b: e.tensor_copy(out=h1[b][:, 512:544], in_=gsm[:]), ["gsm", "gsmr", "h1%d" % b], ["h1%d" % b])
                        P.add("dve", lambda e, b=b: e.tensor_copy(out=h1[b][:, 640:768], in_=innerT[:]), ["innerT", "h1%d" % b], ["h1%d" % b])
                    dma("pool", hbuf.ap()[t * 128:(t + 1) * 128, :], h1[b][:], ["h1%d" % b], [("hbuf", t)], "h1%d" % b)
            P.barrier()

        def moe_ple(l, last):
            if skip():
                return
            with contextlib.ExitStack() as st:
                wr = sb(st, "wr", [128, 8, E], F32)
                dma("sp", wr[:], W["moe_w_router"].ap()[l].rearrange("(kc p) e -> p kc e", p=128), [], ["wr"], "wr")
                br = sb(st, "br", [128, E], F32)
                dma("sp", br[:], bc_row(W["moe_b_router"], l * E, E), [], ["br"], "br")
                iota_e = sb(st, "iota_e", [128, E], F32)
                dma("sp", iota_e[:], CD["iota_e"].ap(), [], ["iota_e"], "iota_e")
                ecap = sb(st, "ecap", [128, E], F32)
                P.add("dve", lambda e: e.tensor_scalar(out=ecap[:], in0=iota_e[:], scalar1=float(CAP), scalar2=None, op0=ALU.mult), ["iota_e"], ["ecap"])
                tri_f = sb(st, "tri_f", [128, 128], F32)
                tri_b = sb(st, "tri_b", [128, 128], BF16)
                dma("sp", tri_f[:], CD["tri"].ap(), [], ["tri_f"], "tri_f")
                P.add("dve", lambda e: e.tensor_copy(out=tri_b[:], in_=tri_f[:]), ["tri_f"], ["tri_b"])
                base = sb(st, "base", [128, E], F32)
                P.add("dve", lambda e: e.memset(base[:], 0.0), [], ["base"])
                hs = [sb(st, "hs", [128, D], F32) for _ in range(2)]
                hb = [sb(st, "hb", [128, D], BF16) for _ in range(2)]
                hT32 = sb(st, "hT32", [128, 8, 128], F32)
                lg = sb(st, "lg", [128, E], F32)
                mx = sb(st, "mx", [128, 8], F32)
                mi = sb(st, "mi", [128, 8], U32)
                mif = sb(st, "mif", [128, 8], F32)
                negm = sb(st, "negm", [128, 1], F32)
                ex = sb(st, "ex", [128, 4], F32)
                rs = sb(st, "rs", [128, 2], F32)
                maskf = sb(st, "maskf", [128, E], F32)
                maskb = sb(st, "maskb", [128, E], BF16)
                slotf = sb(st, "slotf", [128, E], F32)
                eq = sb(st, "eq", [128, E], F32)
                s4f = sb(st, "s4f", [128, 4], F32)
                sk = [[sb(st, "sk", [128, 1], I32) for _ in range(2)] for _ in range(TOPK)]
                for t in range(NT):
                    b = t % 2
                    hn = "hs%d" % b
                    dma("sp", hs[b][:], hbuf.ap()[t * 128:(t + 1) * 128, :], [("hbuf", t)], [hn], hn)
                    P.add("pool", lambda e, b=b: e.tensor_copy(out=hb[b][:], in_=hs[b][:]), [hn], ["hb%d" % b])
                    for half in range(2):
                        bank = 2 + half
                        for j in range(4):
                            kc = half * 4 + j
                            P.add("pe", lambda e, kc=kc, j=j, b=b, bank=bank: e.transpose(out=psf[bank][:, j * 128:(j + 1) * 128],
                                                                                         in_=hs[b][:, kc * 128:(kc + 1) * 128], identity=ident_f[:]),
                                  [hn, "ident_f"], [PSF[bank]])
                        P.add("act", lambda e, half=half, bank=bank: e.copy(out=hT32[:, half * 4:(half + 1) * 4, :],
                                                                             in_=psf[bank][:, :].rearrange("p (j t) -> p j t", t=128)),
                              [PSF[bank]], ["hT32"])
                    for kc in range(8):
                        P.add("pe", lambda e, kc=kc: e.matmul(psf[0][:, 0:E], lhsT=hT32[:, kc, :], rhs=wr[:, kc, :], start=(kc == 0), stop=(kc == 7)),
                              ["hT32", "wr"], [PSF[0]])
                    P.add("dve", lambda e: e.tensor_tensor(out=lg[:], in0=psf[0][:, 0:E], in1=br[:], op=ALU.add), [PSF[0], "br"], ["lg"])
                    P.add("dve", lambda e: e.max(out=mx[:], in_=lg[:]), ["lg"], ["mx"])
                    P.add("dve", lambda e: e.max_index(out=mi[:], in_max=mx[:], in_values=lg[:]), ["lg", "mx"], ["mi"])
                    P.add("dve", lambda e: e.tensor_copy(out=mif[:], in_=mi[:]), ["mi"], ["mif"])
                    P.add("dve", lambda e: e.tensor_scalar(out=negm[:], in0=mx[:, 0:1], scalar1=-1.0, scalar2=None, op0=ALU.mult), ["mx"], ["negm"])
                    P.add("dve", lambda e: e.tensor_scalar(out=ex[:], in0=mx[:, 0:4], scalar1=negm[:, 0:1], scalar2=None, op0=ALU.add), ["mx", "negm"], ["ex"])
                    P.add("act", lambda e: e.activation(out=ex[:], in_=ex[:], func=AF.Exp), ["ex"], ["ex"])
                    P.add("dve", lambda e: e.reduce_sum(out=rs[:, 0:1], in_=ex[:], axis=mybir.AxisListType.X), ["ex"], ["rs"])
                    P.add("dve", lambda e: e.reciprocal(out=rs[:, 1:2], in_=rs[:, 0:1]), ["rs"], ["rs2"])
                    P.add("dve", lambda e, t=t: e.tensor_scalar(out=gates_s[:, t, :], in0=ex[:], scalar1=rs[:, 1:2], scalar2=None, op0=ALU.mult),
                          ["ex", "rs2"], ["gates_s"])
                    P.add("dve", lambda e: e.tensor_scalar(out=maskf[:], in0=lg[:], scalar1=mx[:, 3:4], scalar2=None, op0=ALU.is_ge), ["lg", "mx"], ["maskf"])
                    P.add("dve", lambda e: e.tensor_copy(out=maskb[:], in_=maskf[:]), ["maskf"], ["maskb"])
                    P.add("pe", lambda e: e.matmul(psf[1][:, 0:E], lhsT=tri_b[:], rhs=maskb[:], start=True, stop=True), ["tri_b", "maskb"], [PSF[1]])
                    P.add("pe", lambda e: e.matmul(psf[1][:, 64:64 + E], lhsT=ones_b[:], rhs=maskb[:], start=True, stop=True), ["ones_b", "maskb"], [PSF[1]])
                    P.add("dve", lambda e: e.tensor_tensor(out=slotf[:], in0=psf[1][:, 0:E], in1=base[:], op=ALU.add), [PSF[1], "base"], ["slotf"])
                    P.add("dve", lambda e: e.tensor_tensor(out=slotf[:], in0=slotf[:], in1=ecap[:], op=ALU.add), ["slotf", "ecap"], ["slotf"])
                    P.add("dve", lambda e: e.tensor_tensor(out=base[:], in0=base[:], in1=psf[1][:, 64:64 + E], op=ALU.add), [PSF[1], "base", "slotf"], ["base"])
                    for k in range(TOPK):
                        P.add("dve", lambda e, k=k: e.tensor_scalar(out=eq[:], in0=iota_e[:], scalar1=mif[:, k:k + 1], scalar2=None, op0=ALU.is_equal),
                              ["iota_e", "mif"], ["eq"])
                        P.add("dve", lambda e: e.tensor_tensor(out=eq[:], in0=eq[:], in1=slotf[:], op=ALU.mult), ["eq", "slotf"], ["eq"])
                        P.add("dve", lambda e, k=k: e.reduce_sum(out=s4f[:, k:k + 1], in_=eq[:], axis=mybir.AxisListType.X), ["eq"], ["s4f"])
                    P.add("dve", lambda e, t=t: e.tensor_copy(out=slots_i[:, t, :], in_=s4f[:]), ["s4f"], ["slots_i"])
                    for k in range(TOPK):
                        P.add("dve", lambda e, t=t, k=k, b=b: e.tensor_copy(out=sk[k][b][:, :], in_=s4f[:, k:k + 1]), ["s4f"], ["sk%d_%d" % (k, b)])
                        P.add("pool", lambda e, t=t, k=k, b=b: e.indirect_dma_start(
                            out=xs_d.ap(), out_offset=bass.IndirectOffsetOnAxis(ap=sk[k][b][:, :], axis=0),
                            in_=hb[b][:, :], in_offset=None),
                            ["sk%d_%d" % (k, b), "hb%d" % b], ["xs"], dma_key="hb%d" % b)
            P.barrier()
            if skip():
                return
            with contextlib.ExitStack() as st:
                wgu = [sb(st, "wgu", [128, 8, 2048], BF16) for _ in range(2)]
                wdn = [sb(st, "wdn", [128, 8, D], BF16) for _ in range(2)]
                stage = [sb(st, "wst", [128, 2048], F32) for _ in range(3)]
                bgu = [sb(st, "bgu", [128, 16], F32) for _ in range(2)]
                bdf = [sb(st, "bdf", [1, D], F32) for _ in range(2)]
                bdb = [sb(st, "bdb", [1, D], BF16) for _ in range(2)]
                xr = [sb(st, "xr", [128, D], BF16) for _ in range(2)]
                xT = sb(st, "xT", [128, 8, CAP], BF16)
                actT = sb(st, "actT", [128, 8, CAP], BF16)
                gc = [sb(st, "gc", [128, 512], F32) for _ in range(2)]
                sg = [sb(st, "sg", [128, 512], F32) for _ in range(2)]
                uc = [sb(st, "uc", [128, 512], F32) for _ in range(2)]
                yo = [sb(st, "yo", [128, D], F32) for _ in range(2)]
                cnt = 0
                for ex_ in range(E):
                    eb = ex_ % 2
                    load_w_bf16(st, wgu[eb], "wgu%d" % eb, W["moe_w_gate_up"].ap()[l, ex_], 8, 2048, stage, 3, engs=("act", "dve"))
                    load_w_bf16(st, wdn[eb], "wdn%d" % eb, W["moe_w_down"].ap()[l, ex_], 8, D, stage, 3, engs=("act", "dve"))
                    dma("sp", bgu[eb][:], W["moe_b_gate_up"].ap()[l, ex_], [], ["bgu%d" % eb], "bgu%d" % eb)
                    dma("sp", bdf[eb][:], W["moe_b_down"].ap()[l, ex_:ex_ + 1, :], [], ["bdf%d" % eb], "bdf%d" % eb)
                    P.add("dve", lambda e, eb=eb: e.tensor_copy(out=bdb[eb][:], in_=bdf[eb][:]), ["bdf%d" % eb], ["bdb%d" % eb])
                    for s_ in range(CT):
                        xb = cnt % 2
                        cnt += 1
                        r0 = ex_ * CAP + s_ * 128
                        dma("sp", xr[xb][:], xs_d.ap()[r0:r0 + 128, :], ["xs"], ["xr%d" % xb], "xr%d" % xb)
                        pb = rr["ps"] % 2
                        rr["ps"] += 1
                        for j in range(8):
                            P.add("pe", lambda e, j=j, pb=pb, xb=xb: e.transpose(out=psb[pb][:, j * 128:(j + 1) * 128], in_=xr[xb][:, j * 128:(j + 1) * 128],
                                                                                 identity=ident_b[:]), ["xr%d" % xb, "ident_b"], [PSB[pb]])
                        P.add("act", lambda e, pb=pb, s_=s_: e.copy(out=xT[:, :, s_ * 128:(s_ + 1) * 128], in_=psb[pb][:, :].rearrange("p (j t) -> p j t", t=128)),
                              [PSB[pb]], ["xT"])
                    for fc in range(8):
                        for n0 in range(0, CAP, 512):
                            n1 = min(CAP, n0 + 512)
                            nn = n1 - n0
                            w_ = (fc * 8 + n0 // 512) % 2
                            bg_, bu_ = (0, 1) if w_ == 0 else (2, 3)
                            for kc in range(8):
                                P.add("pe", lambda e, kc=kc, fc=fc, n0=n0, n1=n1, bg_=bg_, eb=eb, nn=nn: e.matmul(
                                    psf[bg_][:, 0:nn], lhsT=wgu[eb][:, kc, fc * 128:(fc + 1) * 128], rhs=xT[:, kc, n0:n1], start=(kc == 0), stop=(kc == 7)),
                                    ["wgu%d" % eb, "xT"], [PSF[bg_]])
                            for kc in range(8):
                                P.add("pe", lambda e, kc=kc, fc=fc, n0=n0, n1=n1, bu_=bu_, eb=eb, nn=nn: e.matmul(
                                    psf[bu_][:, 0:nn], lhsT=wgu[eb][:, kc, 1024 + fc * 128:1024 + (fc + 1) * 128], rhs=xT[:, kc, n0:n1], start=(kc == 0), stop=(kc == 7)),
                                    ["wgu%d" % eb, "xT"], [PSF[bu_]])
                            P.add("dve", lambda e, fc=fc, bg_=bg_, eb=eb, nn=nn, w_=w_: e.tensor_scalar(out=gc[w_][:, 0:nn], in0=psf[bg_][:, 0:nn], scalar1=bgu[eb][:, fc:fc + 1],
                                                                                                      scalar2=7.0, op0=ALU.add, op1=ALU.min), [PSF[bg_], "bgu%d" % eb], ["gc%d" % w_])
                            P.add("act", lambda e, nn=nn, w_=w_: e.activation(out=sg[w_][:, 0:nn], in_=gc[w_][:, 0:nn], func=AF.Sigmoid, scale=1.702), ["gc%d" % w_], ["sg%d" % w_])
                            P.add("dve", lambda e, fc=fc, bu_=bu_, eb=eb, nn=nn, w_=w_: e.tensor_scalar(out=uc[w_][:, 0:nn], in0=psf[bu_][:, 0:nn], scalar1=bgu[eb][:, 8 + fc:9 + fc],
                                                                                                      scalar2=-7.0, op0=ALU.add, op1=ALU.max), [PSF[bu_], "bgu%d" % eb], ["uc%d" % w_])
                            P.add("dve", lambda e, nn=nn, w_=w_: e.tensor_scalar(out=uc[w_][:, 0:nn], in0=uc[w_][:, 0:nn], scalar1=7.0, scalar2=1.0, op0=ALU.min, op1=ALU.add),
                                  ["uc%d" % w_], ["uc%d" % w_])
                            P.add("dve", lambda e, nn=nn, w_=w_: e.tensor_tensor(out=gc[w_][:, 0:nn], in0=gc[w_][:, 0:nn], in1=sg[w_][:, 0:nn], op=ALU.mult),
                                  ["gc%d" % w_, "sg%d" % w_], ["gc%d" % w_])
                            P.add("dve", lambda e, fc=fc, n0=n0, n1=n1, nn=nn, w_=w_: e.tensor_tensor(out=actT[:, fc, n0:n1], in0=gc[w_][:, 0:nn], in1=uc[w_][:, 0:nn], op=ALU.mult),
                                  ["gc%d" % w_, "uc%d" % w_], ["actT"])
                    for s_ in range(CT):
                        yb = s_ % 2
                        for nh in range(2):
                            bank = 4 + nh
                            for fc in range(8):
                                P.add("pe", lambda e, fc=fc, s_=s_, nh=nh, bank=bank, eb=eb: e.matmul(
                                    psf[bank][:, :], lhsT=actT[:, fc, s_ * 128:(s_ + 1) * 128], rhs=wdn[eb][:, fc, nh * 512:(nh + 1) * 512], start=(fc == 0), stop=False),
                                    ["actT", "wdn%d" % eb], [PSF[bank]])
                            P.add("pe", lambda e, nh=nh, bank=bank, eb=eb: e.matmul(psf[bank][:, :], lhsT=ones_b[0:1, :], rhs=bdb[eb][0:1, nh * 512:(nh + 1) * 512],
                                                                                  start=False, stop=True), ["ones_b", "bdb%d" % eb], [PSF[bank]])
                            P.add("act", lambda e, nh=nh, bank=bank, yb=yb: e.copy(out=yo[yb][:, nh * 512:(nh + 1) * 512], in_=psf[bank][:, :]), [PSF[bank]], ["yo%d" % yb])
                        r0 = ex_ * CAP + s_ * 128
                        dma("act", ys_d.ap()[r0:r0 + 128, :], yo[yb][:], ["yo%d" % yb], ["ys"], "yo%d" % yb)
            P.barrier()
            if skip():
                return
            with contextlib.ExitStack() as st:
                wg = sb(st, "wg", [128, 8, D], BF16)
                wp = sb(st, "wp", [128, 2, D], BF16)
                stage = [sb(st, "wst", [128, 1024], F32) for _ in range(2)]
                load_w_bf16(st, wg, "wg", W["ple_w_gate"].ap()[l], 8, D, stage, 2)
                load_w_bf16(st, wp, "wp", W["ple_w_proj"].ap()[l], 2, D, stage, 2)
                ln_g = sb(st, "ln_g", [128, D], F32)
                ln_b = sb(st, "ln_b", [128, D], F32)
                dma("sp", ln_g[:], bc_row(W["ln_ffn_g"], l * D, D), [], ["ln_g"], "ln_g")
                dma("sp", ln_b[:], bc_row(W["ln_ffn_b"], l * D, D), [], ["ln_b"], "ln_b")
                yk = [sb(st, "yk", [128, D], F32) for _ in range(4)]
                hs = [sb(st, "hs", [128, D], F32) for _ in range(2)]
                ps_ = [sb(st, "ps_", [128, 256], F32) for _ in range(2)]
                pb_ = sb(st, "pb_", [128, 256], BF16)
                pT = sb(st, "pT", [128, 2, 128], BF16)
                acc = sb(st, "acc", [128, D], F32)
                z = sb(st, "z", [128, D], F32)
                h2 = sb(st, "h2", [128, D], F32)
                h2b = sb(st, "h2b", [128, D], BF16)
                h2T = sb(st, "h2T", [128, 8, 128], BF16)
                sgm = sb(st, "sgm", [128, 512], F32)
                h3 = [sb(st, "h3", [128, D], F32) for _ in range(2)]
                small = sb(st, "small", [128, 32], F32)
                ck = [sb(st, "ck", [128, 1], I32) for _ in range(TOPK)]
                for t in range(NT):
                    b = t % 2
                    hn = "hs%d" % b
                    dma("sp", hs[b][:], hbuf.ap()[t * 128:(t + 1) * 128, :], [("hbuf", t)], [hn], hn)
                    dma("sp", ps_[b][:], p_d.ap()[l, t * 128:(t + 1) * 128, :], [], ["ps_%d" % b], "ps_%d" % b)
                    for k in range(TOPK):
                        P.add("dve", lambda e, t=t, k=k: e.tensor_copy(out=ck[k][:, :], in_=slots_i[:, t, k:k + 1]), ["slots_i"], ["ck%d" % k])
                        P.add("pool", lambda e, t=t, k=k: e.indirect_dma_start(
                            out=yk[k][:, :], out_offset=None, in_=ys_d.ap(), in_offset=bass.IndirectOffsetOnAxis(ap=ck[k][:, :], axis=0)),
                            ["ck%d" % k, "ys"], ["yk%d" % k], dma_key="yk%d" % k)
                    P.add("dve", lambda e, t=t: e.tensor_scalar(out=acc[:], in0=yk[0][:], scalar1=gates_s[:, t, 0:1], scalar2=None, op0=ALU.mult),
                          ["yk0", "gates_s"], ["acc"])
                    for k in range(1, TOPK):
                        P.add("dve", lambda e, t=t, k=k: e.scalar_tensor_tensor(out=acc[:], in0=yk[k][:], scalar=gates_s[:, t, k:k + 1], in1=acc[:],
                                                                                op0=ALU.mult, op1=ALU.add), ["yk%d" % k, "gates_s", "acc"], ["acc"])
                    P.add("dve", lambda e, b=b: e.scalar_tensor_tensor(out=z[:], in0=hs[b][:], scalar=ALPHA, in1=acc[:], op0=ALU.mult, op1=ALU.add),
                          [hn, "acc"], ["z"])
                    layer_norm(z, "z", h2, "h2", ln_g, ln_b, ["ln_g", "ln_b"], small, "small")
                    P.add("act", lambda e: e.copy(out=h2b[:], in_=h2[:]), ["h2"], ["h2b"])
                    transpose_blocks(h2b, "h2b", 8, h2T, "h2T")
                    P.add("act", lambda e, b=b: e.copy(out=pb_[:], in_=ps_[b][:]), ["ps_%d" % b], ["pb_"])
                    transpose_blocks(pb_, "pb_", 2, pT, "pT")
                    for nh in range(2):
                        linear(h2T, "h2T", 8, wg, "wg", nh * 512, (nh + 1) * 512, nh)
                        linear(pT, "pT", 2, wp, "wp", nh * 512, (nh + 1) * 512, 2 + nh)
                        P.add("act", lambda e, nh=nh: e.activation(out=sgm[:], in_=psf[nh][:, :], func=AF.Sigmoid), [PSF[nh]], ["sgm"])
                        P.add("dve", lambda e, nh=nh: e.tensor_tensor(out=sgm[:], in0=sgm[:], in1=psf[2 + nh][:, :], op=ALU.mult), ["sgm", PSF[2 + nh]], ["sgm"])
                        P.add("pool", lambda e, nh=nh, b=b: e.tensor_tensor(out=h3[b][:, nh * 512:(nh + 1) * 512], in0=sgm[:], in1=h2[:, nh * 512:(nh + 1) * 512], op=ALU.add),
                              ["sgm", "h2"], ["h3%d" % b])
                    if last:
                        fin.append(dma("pool", y_d.ap()[t * 128:(t + 1) * 128, :], h3[b][:], ["h3%d" % b], [("y", t)], "h3%d" % b))
                    else:
                        dma("pool", hbuf.ap()[t * 128:(t + 1) * 128, :], h3[b][:], ["h3%d" % b], [("hbuf", t)], "h3%d" % b)
            P.barrier()
            if dbg:
                dma("sp", dbg_d.ap()[l], (y_d if last else hbuf).ap(), [], [], "dbg")
                P.barrier()

        kmax_bc = sb(root, "kmax_bc", [128, 1], F32)

        def sumsq(ps_ap, n, out_col, junk, psname, wname):
            P.add("act", lambda e: e.activation(out=junk[:, 0:n], in_=ps_ap, func=AF.Square), [psname], ["junk"])
            P.add("dve", lambda e: e.reduce_sum(out=out_col, in_=junk[:, 0:n], axis=mybir.AxisListType.X), ["junk"], [wname])

        def rms_norm_ps(ps_ap, psname, n, g_t, gname, out_b, oname, junk, small, sname, col):
            sumsq(ps_ap, n, small[:, col:col + 1], junk, psname, sname)
            rstd_from(small[:, col:col + 1], eps_6, small[:, col + 2:col + 3], small[:, col + 1:col + 2], [sname], sname + "r", scale=1.0 / n)
            P.add("dve", lambda e: e.tensor_scalar(out=junk[:, 0:n], in0=ps_ap, scalar1=small[:, col + 2:col + 3], scalar2=None, op0=ALU.mult),
                  [psname, sname + "r"], ["junk"])
            P.add("pool", lambda e: e.tensor_tensor(out=out_b, in0=junk[:, 0:n], in1=g_t[:, 0:n], op=ALU.mult), ["junk", gname], [oname])

        def mla_kv():
            if skip():
                return
            with contextlib.ExitStack() as st:
                wa = sb(st, "wa", [128, 8, 320], BF16)
                wb = sb(st, "wb", [128, 2, 2048], BF16)
                stage = [sb(st, "wst", [128, 2048], F32) for _ in range(2)]
                load_w_bf16(st, wa, "wa", W["mla_w_kv_a"].ap(), 8, 320, stage, 2)
                load_w_bf16(st, wb, "wb", W["mla_w_kv_b"].ap(), 2, 2048, stage, 2)
                g_t = sb(st, "kvg", [128, 256], F32)
                dma("sp", g_t[:], bc_row(W["mla_kv_norm_g"], 0, 256), [], ["kvg"], "kvg")
                hs = [sb(st, "hs", [128, D], F32) for _ in range(2)]
                hb = sb(st, "hb", [128, D], BF16)
                hT = sb(st, "hT", [128, 8, 128], BF16)
                junk = sb(st, "junk", [128, 512], F32)
                small = sb(st, "small", [128, 32], F32)
                cb = sb(st, "cb", [128, 256], BF16)
                cT = sb(st, "cT", [128, 2, 128], BF16)
                cos_t = sb(st, "cos", [128, 32], F32)
                sin_t = sb(st, "sin", [128, 32], F32)
                tf = sb(st, "tf", [128, 32], F32)
                ti = sb(st, "ti", [128, 32], I32)
                t1 = sb(st, "t1", [128, 32], F32)
                t2 = sb(st, "t2", [128, 32], F32)
                kn = [sb(st, "kn", [128, D], BF16) for _ in range(2)]
                knT = [sb(st, "knT", [128, 8, 128], BF16) for _ in range(2)]
                vx = [sb(st, "vx", [128, 8, 130], BF16) for _ in range(2)]
                kr = sb(st, "kr", [128, 128], BF16)
                krT = [sb(st, "krT", [128, 1, 128], BF16) for _ in range(2)]
                ksq = sb(st, "ksq", [128, 16], F32)
                kmx = sb(st, "kmx", [128, 8], F32)
                P.add("dve", lambda e: e.memset(kmx[:], 0.0), [], ["kmx"])
                P.add("pool", lambda e: e.tensor_copy(out=kr[:], in_=zeros_f[:, 0:128]), ["zeros_f"], ["kr"])
                P.add("pool", lambda e: e.tensor_copy(out=kr[:, 64:65], in_=ones_f[:, 0:1]), ["kr", "ones_f"], ["kr"])
                for b in range(2):
                    for h_ in range(8):
                        P.add("pool", lambda e, b=b, h_=h_: e.tensor_copy(out=vx[b][:, h_, 128:130], in_=ones_f[:, 0:2]), ["ones_f"], ["vx%d" % b])
                P.last_w["inv"] = P.last_w.get("inv_m")
                for t in range(NT):
                    b = t % 2
                    hn = "hs%d" % b
                    dma("sp", hs[b][:], hbuf.ap()[t * 128:(t + 1) * 128, :], [("hbuf", t)], [hn], hn)
                    P.add("pool", lambda e, b=b: e.tensor_copy(out=hb[:], in_=hs[b][:]), [hn], ["hb"])
                    transpose_blocks(hb, "hb", 8, hT, "hT")
                    rope_tables(t, inv_m, 32, cos_t, sin_t, tf, ti, "kv")
                    linear(hT, "hT", 8, wa, "wa", 0, 320, 4)
                    rms_norm_ps(psf[4][:, 0:256], PSF[4], 256, g_t, "kvg", cb[:], "cb", junk, small, "small", 0)
                    rope_apply(psf[4][:, 256:288], psf[4][:, 288:320], kr[:, 0:32], kr[:, 32:64], cos_t, sin_t, 32, t1, t2, [PSF[4]], "kr", "kv")
                    sumsq(psf[4][:, 256:320], 64, ksq[:, 8:9], junk, PSF[4], "ksq8")
                    P.add("pe", lambda e: e.transpose(out=psb[0][:, 0:128], in_=kr[:, 0:128], identity=ident_b[:]), ["kr", "ident_b"], [PSB[0]])
                    P.add("act", lambda e, b=b: e.copy(out=krT[b][:, 0, :], in_=psb[0][:, 0:128]), [PSB[0]], ["krT%d" % b])
                    dma("act", KR_d.ap()[:, t * 128:(t + 1) * 128], krT[b][0:65, 0, :], ["krT%d" % b], [("KR", t)], "krT%d" % b)
                    transpose_blocks(cb, "cb", 2, cT, "cT")
                    for n in range(4):
                        bank = n % 4
                        linear(cT, "cT", 2, wb, "wb", n * 512, (n + 1) * 512, bank)
                        for hh in range(2):
                            h = n * 2 + hh
                            P.add("act", lambda e, h=h, hh=hh, bank=bank, b=b: e.copy(out=kn[b][:, h * 128:(h + 1) * 128], in_=psf[bank][:, hh * 256: hh * 256 + 128]),
                                  [PSF[bank]], ["kn%d" % b])
                            sumsq(psf[bank][:, hh * 256: hh * 256 + 128], 128, ksq[:, h:h + 1], junk, PSF[bank], "ksq")
                            P.add("dve", lambda e, h=h, hh=hh, bank=bank, b=b: e.tensor_copy(out=vx[b][:, h, 0:128], in_=psf[bank][:, hh * 256 + 128: hh * 256 + 256]),
                                  [PSF[bank]], ["vx%d" % b])
                    P.add("dve", lambda e: e.tensor_scalar(out=ksq[:, 0:8], in0=ksq[:, 0:8], scalar1=ksq[:, 8:9], scalar2=None, op0=ALU.add), ["ksq", "ksq8"], ["ksq"])
                    P.add("dve", lambda e: e.tensor_tensor(out=kmx[:], in0=kmx[:], in1=ksq[:, 0:8], op=ALU.max), ["ksq", "kmx"], ["kmx"])
                    transpose_blocks(kn[b], "kn%d" % b, 8, knT[b], "knT%d" % b)
                    dma("act", KT_d.ap()[:, :, t * 128:(t + 1) * 128].rearrange("h d t -> d h t"), knT[b][:], ["knT%d" % b], [("KT", t)], "knT%d" % b)
                    dma("act", V_d.ap()[t * 128:(t + 1) * 128], vx[b][:], ["vx%d" % b], [("V", t)], "vx%d" % b)
                P.add("dve", lambda e: e.reduce_max(out=ksq[:, 9:10], in_=kmx[:], axis=mybir.AxisListType.X), ["kmx"], ["kmr"])
                P.add("pe", lambda e: e.transpose(out=psf[5][0:1, 0:128], in_=ksq[:, 9:10], identity=ident_f[:]), ["kmr", "ident_f"], [PSF[5]])
                P.add("dve", lambda e: e.reduce_max(out=small[0:1, 20:21], in_=psf[5][0:1, 0:128], axis=mybir.AxisListType.X), [PSF[5]], ["km1"])
                P.add("pe", lambda e: e.matmul(psf[5][:, 256:257], lhsT=ones_f[0:1, :], rhs=small[0:1, 20:21], start=True, stop=True), ["km1", "ones_f"], [PSF[5]])
                P.add("dve", lambda e: e.tensor_copy(out=kmax_bc[:], in_=psf[5][:, 256:257]), [PSF[5]], ["kmax_bc"])
            P.barrier()

        def mla_layer(l):
            j = l - NA
            if skip():
                return
            with contextlib.ExitStack() as st:
                wa = sb(st, "wqa", [128, 8, 256], BF16)
                wb = sb(st, "wqb", [128, 2, 1536], BF16)
                stage = [sb(st, "wst", [128, 1536], F32) for _ in range(2)]
                load_w_bf16(st, wa, "wqa", W["mla_w_q_a"].ap()[j], 8, 256, stage, 2)
                load_w_bf16(st, wb, "wqb", W["mla_w_q_b"].ap()[j], 2, 1536, stage, 2)
                g_t = sb(st, "qg", [128, 256], F32)
                dma("sp", g_t[:], bc_row(W["mla_q_norm_g"], j * 256, 256), [], ["qg"], "qg")
                hs = [sb(st, "hs", [128, D], F32) for _ in range(2)]
                hb = sb(st, "hb", [128, D], BF16)
                hT = sb(st, "hT", [128, 8, 128], BF16)
                junk = sb(st, "junk", [128, 512], F32)
                small = sb(st, "small", [128, 32], F32)
                cb = sb(st, "cb", [128, 256], BF16)
                cT = sb(st, "cT", [128, 2, 128], BF16)
                cos_t = sb(st, "cos", [128, 32], F32)
                sin_t = sb(st, "sin", [128, 32], F32)
                tf = sb(st, "tf", [128, 32], F32)
                ti = sb(st, "ti", [128, 32], I32)
                t1 = sb(st, "t1", [128, 32], F32)
                t2 = sb(st, "t2", [128, 32], F32)
                qn = sb(st, "qn", [128, D], BF16)
                qr = sb(st, "qr", [128, 1024], BF16)
                qsq = sb(st, "qsq", [128, 8], F32)
                qsh = sb(st, "qsh", [128, 8], F32)
                qnT = [sb(st, "qnT", [128, 8, 128], BF16) for _ in range(2)]
                qrT = [sb(st, "qrT", [128, 8, 128], BF16) for _ in range(2)]
                for h_ in range(8):
                    P.add("pool", lambda e, h_=h_: e.tensor_copy(out=qr[:, h_ * 128:(h_ + 1) * 128], in_=zeros_f[:, 0:128]), ["zeros_f"], ["qr"])
                P.last_w["inv"] = P.last_w.get("inv_m")
                for t in range(NT):
                    b = t % 2
                    hn = "hs%d" % b
                    dma("sp", hs[b][:], hbuf.ap()[t * 128:(t + 1) * 128, :], [("hbuf", t)], [hn], hn)
                    P.add("pool", lambda e, b=b: e.tensor_copy(out=hb[:], in_=hs[b][:]), [hn], ["hb"])
                    transpose_blocks(hb, "hb", 8, hT, "hT")
                    rope_tables(t, inv_m, 32, cos_t, sin_t, tf, ti, "q")
                    linear(hT, "hT", 8, wa, "wqa", 0, 256, 4)
                    rms_norm_ps(psf[4][:, 0:256], PSF[4], 256, g_t, "qg", cb[:], "cb", junk, small, "small", 0)
                    transpose_blocks(cb, "cb", 2, cT, "cT")
                    for n in range(3):
                        linear(cT, "cT", 2, wb, "wqb", n * 512, (n + 1) * 512, n)
                    for h in range(8):
                        c0 = h * 192
                        def seg(a, bnd):
                            bk = a // 512
                            assert (bnd - 1) // 512 == bk
                            return psf[bk][:, a - bk * 512: bnd - bk * 512], PSF[bk]
                        a = c0
                        while a < c0 + 128:
                            bnd = min(c0 + 128, (a // 512 + 1) * 512)
                            ap_, nm = seg(a, bnd)
                            P.add("act", lambda e, ap_=ap_, a=a, bnd=bnd, h=h, c0=c0: e.copy(out=qn[:, h * 128 + (a - c0): h * 128 + (bnd - c0)], in_=ap_), [nm], ["qn"])
                            a = bnd
                        a = c0
                        pieces = []
                        while a < c0 + 192:
                            bnd = min(c0 + 192, (a // 512 + 1) * 512)
                            pieces.append((a, bnd))
                            a = bnd
                        for pi, (a, bnd) in enumerate(pieces):
                            ap_, nm = seg(a, bnd)
                            sumsq(ap_, bnd - a, small[:, 8 + pi: 9 + pi], junk, nm, "sq%d" % pi)
                        if len(pieces) == 2:
                            P.add("dve", lambda e, h=h: e.tensor_tensor(out=qsq[:, h:h + 1], in0=small[:, 8:9], in1=small[:, 9:10], op=ALU.add), ["sq0", "sq1"], ["qsq"])
                        else:
                            P.add("dve", lambda e, h=h: e.tensor_copy(out=qsq[:, h:h + 1], in_=small[:, 8:9]), ["sq0"], ["qsq"])
                        r1, n1_ = seg(c0 + 128, c0 + 160)
                        r2, n2_ = seg(c0 + 160, c0 + 192)
                        rope_apply(r1, r2, qr[:, h * 128:h * 128 + 32], qr[:, h * 128 + 32:h * 128 + 64], cos_t, sin_t, 32, t1, t2, list({n1_, n2_}), "qr", "q")
                    P.add("dve", lambda e: e.tensor_scalar(out=qsh[:], in0=qsq[:], scalar1=kmax_bc[:, 0:1], scalar2=1e-30, op0=ALU.mult, op1=ALU.add), ["qsq", "kmax_bc"], ["qsh"])
                    P.add("act", lambda e: e.activation(out=qsh[:], in_=qsh[:], func=AF.Ln), ["qsh"], ["qsh"])
                    P.add("act", lambda e: e.activation(out=qsh[:], in_=qsh[:], func=AF.Exp, scale=0.5), ["qsh"], ["qsh"])
                    for h in range(8):
                        P.add("dve", lambda e, h=h: e.tensor_scalar(out=qr[:, h * 128 + 64:h * 128 + 65], in0=qsh[:, h:h + 1], scalar1=-1.0, scalar2=None, op0=ALU.mult), ["qsh", "qr"], ["qr"])
                    transpose_blocks(qn, "qn", 8, qnT[b], "qnT%d" % b)
                    transpose_blocks(qr, "qr", 8, qrT[b], "qrT%d" % b, src_off=0)
                    dma("act", QT_d.ap()[:, :, t * 128:(t + 1) * 128].rearrange("h d t -> d h t"), qnT[b][:], ["qnT%d" % b], [("QT", t)], "qnT%d" % b)
                    dma("act", QR_d.ap()[:, :, t * 128:(t + 1) * 128].rearrange("h d t -> d h t"), qrT[b][0:65, :, :], ["qrT%d" % b], [("QR", t)], "qrT%d" % b)
            P.barrier()
            if skip():
                return
            with contextlib.ExitStack() as st:
                krT = sb(st, "krTa", [65, S], BF16)
                dma("sp", krT[:], KR_d.ap(), [], ["krTa"], "krTa")
                causal_f = sb(st, "causal_f", [128, 128], F32)
                causal = sb(st, "causal", [128, 128], BF16)
                dma("sp", causal_f[:], CD["causal"].ap(), [], ["causal_f"], "causal_f")
                P.add("dve", lambda e: e.tensor_copy(out=causal[:], in_=causal_f[:]), ["causal_f"], ["causal"])
                KT = [sb(st, "KTh", [128, S], BF16) for _ in range(2)]
                QT = [sb(st, "QTh", [128, S], BF16) for _ in range(2)]
                QR = [sb(st, "QRh", [65, S], BF16) for _ in range(2)]
                Vh = [sb(st, "Vh", [128, NT, 130], BF16) for _ in range(2)]
                pT = [sb(st, "pTa", [128, 512], BF16) for _ in range(2)]
                ob = [sb(st, "ob", [128, 128], BF16) for _ in range(2)]
                rsum = sb(st, "rsum", [128, 2], F32)
                QG = 512 if S >= 512 else S
                NQB = QG // 128
                it = 0
                oc = 0
                for h in range(8):
                    hb_ = h % 2
                    dma("sp", KT[hb_][:], KT_d.ap()[h], [], ["KT%d" % hb_], "KT%d" % hb_)
                    dma("sp", QT[hb_][:], QT_d.ap()[h], [], ["QT%d" % hb_], "QT%d" % hb_)
                    dma("sp", QR[hb_][:], QR_d.ap()[h], [], ["QR%d" % hb_], "QR%d" % hb_)
                    dma("sp", Vh[hb_][:], V_d.ap()[:, h, :].rearrange("(t p) c -> p t c", p=128), [], ["Vh%d" % hb_], "Vh%d" % hb_)
                    for qg in range(S // QG):
                        nkb = (qg + 1) * NQB
                        for kb in range(nkb):
                            sbk = 4 + it % 2
                            pb = it % 2
                            it += 1
                            P.add("pe", lambda e, kb=kb, qg=qg, sbk=sbk, hb_=hb_: e.matmul(psf[sbk][:, 0:QG], lhsT=KT[hb_][:, kb * 128:(kb + 1) * 128],
                                                                                        rhs=QT[hb_][:, qg * QG:(qg + 1) * QG], start=True, stop=False),
                                  ["KT%d" % hb_, "QT%d" % hb_], [PSF[sbk]])
                            P.add("pe", lambda e, kb=kb, qg=qg, sbk=sbk, hb_=hb_: e.matmul(psf[sbk][:, 0:QG], lhsT=krT[:, kb * 128:(kb + 1) * 128],
                                                                                        rhs=QR[hb_][:, qg * QG:(qg + 1) * QG], start=False, stop=True),
                                  ["krTa", "QR%d" % hb_], [PSF[sbk]])
                            P.add("act", lambda e, sbk=sbk, pb=pb: e.activation(out=pT[pb][:, 0:QG], in_=psf[sbk][:, 0:QG], func=AF.Exp, scale=SCALE),
                                  [PSF[sbk]], ["pTa%d" % pb])
                            for qb in range(NQB):
                                gq = qg * NQB + qb
                                if kb > gq:
                                    continue
                                if kb == gq:
                                    P.add("dve", lambda e, pb=pb, qb=qb: e.tensor_tensor(out=pT[pb][:, qb * 128:(qb + 1) * 128], in0=pT[pb][:, qb * 128:(qb + 1) * 128],
                                                                                         in1=causal[:], op=ALU.mult), ["pTa%d" % pb, "causal"], ["pTa%d" % pb])
                                P.add("pe", lambda e, pb=pb, qb=qb, kb=kb, gq=gq, hb_=hb_: e.matmul(psf[qb][:, 0:129], lhsT=pT[pb][:, qb * 128:(qb + 1) * 128],
                                                                                                  rhs=Vh[hb_][:, kb, 0:129], start=(kb == 0), stop=(kb == gq)),
                                      ["pTa%d" % pb, "Vh%d" % hb_], [PSF[qb]])
                                if kb == gq:
                                    o_ = oc % 2
                                    oc += 1
                                    P.add("dve", lambda e, qb=qb: e.reciprocal(out=rsum[:, 0:1], in_=psf[qb][:, 128:129]), [PSF[qb]], ["rsum"])
                                    P.add("dve", lambda e, qb=qb, o_=o_: e.tensor_scalar(out=ob[o_][:], in0=psf[qb][:, 0:128], scalar1=rsum[:, 0:1], scalar2=None, op0=ALU.mult),
                                          [PSF[qb], "rsum"], ["ob%d" % o_])
                                    dma("pool", O_d.ap()[gq * 128:(gq + 1) * 128, h * 128:(h + 1) * 128], ob[o_][:], ["ob%d" % o_], [("O", gq)], "ob%d" % o_)
            P.barrier()
            if skip():
                return
            with contextlib.ExitStack() as st:
                wo = sb(st, "wo", [128, 8, D], BF16)
                stage = [sb(st, "wst", [128, 1024], F32) for _ in range(2)]
                load_w_bf16(st, wo, "wo", W["mla_w_o"].ap()[j], 8, D, stage, 2)
                ln_g = sb(st, "ln_g", [128, D], F32)
                ln_b = sb(st, "ln_b", [128, D], F32)
                dma("sp", ln_g[:], bc_row(W["ln_mix_g"], l * D, D), [], ["ln_g"], "ln_g")
                dma("sp", ln_b[:], bc_row(W["ln_mix_b"], l * D, D), [], ["ln_b"], "ln_b")
                hs = [sb(st, "hs", [128, D], F32) for _ in range(2)]
                orow = [sb(st, "orow", [128, D], BF16) for _ in range(2)]
                oT = sb(st, "oT", [128, 8, 128], BF16)
                z = sb(st, "z", [128, D], F32)
                h1 = [sb(st, "h1", [128, D], F32) for _ in range(2)]
                small = sb(st, "small", [128, 32], F32)
                for t in range(NT):
                    b = t % 2
                    hn = "hs%d" % b
                    dma("sp", hs[b][:], hbuf.ap()[t * 128:(t + 1) * 128, :], [("hbuf", t)], [hn], hn)
                    dma("sp", orow[b][:], O_d.ap()[t * 128:(t + 1) * 128, :], [], ["orow%d" % b], "orow%d" % b)
                    transpose_blocks(orow[b], "orow%d" % b, 8, oT, "oT")
                    for nh in range(2):
                        linear(oT, "oT", 8, wo, "wo", nh * 512, (nh + 1) * 512, nh)
                        P.add("dve", lambda e, nh=nh, b=b: e.scalar_tensor_tensor(out=z[:, nh * 512:(nh + 1) * 512], in0=hs[b][:, nh * 512:(nh + 1) * 512],
                                                                                  scalar=ALPHA, in1=psf[nh][:, :], op0=ALU.mult, op1=ALU.add), [hn, PSF[nh]], ["z"])
                    layer_norm(z, "z", h1[b], "h1%d" % b, ln_g, ln_b, ["ln_g", "ln_b"], small, "small")
                    dma("pool", hbuf.ap()[t * 128:(t + 1) * 128, :], h1[b][:], ["h1%d" % b], [("hbuf", t)], "h1%d" % b)
            P.barrier()

        fin = []
        for l in range(DEPTH):
            if l < NA:
                retention_layer(l)
            else:
                if l == NA:
                    mla_kv()
                mla_layer(l)
            moe_ple(l, l == DEPTH - 1)
        if LIMIT < 999:
            fin.append(dma('sp', y_d.ap(), hbuf.ap(), [], [], 'ydbg'))
            if dbg:
                fin.append(dma('sp', dq_d.ap(), qkvg_d.ap(), [], [], 'ydbg2'))
        P.emit(final_ops=fin)
    return nc, consts_np


_CACHE = {}


def run(inputs, S, E, DEPTH, NA, CAP, cores, dbg=False):
    key = (S, E, DEPTH, NA, CAP, dbg)
    if key not in _CACHE:
        _CACHE[key] = build(S, E, DEPTH, NA, CAP, dbg)
    nc, consts_np = _CACHE[key]
    NT = S // 128
    f32 = lambda a: np.ascontiguousarray(np.asarray(a), dtype=np.float32)
    shared = {}
    for k in ("ret_w_in", "ret_gn_g", "ret_gn_b", "ret_w_out", "mla_w_kv_a", "mla_w_kv_b", "mla_w_q_a", "mla_q_norm_g",
              "mla_w_q_b", "mla_w_o", "ln_mix_g", "ln_mix_b", "ln_ffn_g", "ln_ffn_b", "moe_w_router", "moe_b_router",
              "moe_w_gate_up", "moe_b_gate_up", "moe_w_down", "moe_b_down", "ple_w_gate", "ple_w_proj"):
        shared[k] = f32(inputs[k])
    shared["mla_kv_norm_g"] = f32(inputs["mla_kv_norm_g"]).reshape(1, 256)
    bgu_ = shared["moe_b_gate_up"]
    shared["moe_b_gate_up"] = np.ascontiguousarray(bgu_.reshape(bgu_.shape[0], bgu_.shape[1], 16, 128).transpose(0, 1, 3, 2))
    for k, v in consts_np.items():
        shared["c_" + k] = f32(v)
    x = f32(inputs["x"])
    p = f32(inputs["p"])
    pos = np.asarray(inputs["positions"]).astype(np.int32)
    in_maps = []
    for c in range(cores):
        m = dict(shared)
        m["x"] = x[c]
        m["p"] = np.ascontiguousarray(p[:, c])
        m["pos_tm"] = np.ascontiguousarray(pos[c].reshape(NT, 128).T)
        in_maps.append(m)
    res = run_bass_kernel_spmd(nc, in_maps, core_ids=list(range(cores)))
    return res.results


def kernel(**inputs):
    S, E, DEPTH, NA = 8192, 32, 4, 2
    CAP = 1280
    res = run(inputs, S, E, DEPTH, NA, CAP, cores=4)
    return np.stack([r["y"] for r in res], axis=0).astype(np.float32)
```

```python
import contextlib
import numpy as np
import concourse.bass as bass
import concourse.mybir as mybir
from concourse.bass_utils import run_bass_kernel_spmd

F32 = mybir.dt.float32
BF16 = mybir.dt.bfloat16
I32 = mybir.dt.int32
U32 = mybir.dt.uint32
AF = mybir.ActivationFunctionType
ALU = mybir.AluOpType

D = 1024
SEM_ROT = 30000
DN_EPS = 1e-5


import types


def _snap(fn):
    if fn.__closure__ is None:
        return fn
    cells = []
    for c in fn.__closure__:
        try:
            cells.append(types.CellType(c.cell_contents))
        except ValueError:
            cells.append(c)
    g = types.FunctionType(fn.__code__, fn.__globals__, fn.__name__, fn.__defaults__, tuple(cells))
    g.__kwdefaults__ = fn.__kwdefaults__
    return g


class Op:
    __slots__ = ("eng", "fn", "deps", "signal", "event", "dma_key")

    def __init__(self, eng, fn, deps, dma_key):
        self.eng = eng
        self.fn = fn
        self.deps = deps
        self.signal = dma_key is not None
        self.event = None
        self.dma_key = dma_key


class Prog:
    ENGS = ("pe", "act", "dve", "pool", "sp")

    def __init__(self, nc):
        self.nc = nc
        self.ops = {e: [] for e in self.ENGS}
        self.last_w = {}
        self.readers = {}
        self.all_ops = []
        self.pending = {}
        self.last_dma = {}

    def add(self, eng, fn, reads=(), writes=(), dma_key=None):
        pr = [r for r in reads if isinstance(r, str) and r.startswith(("psf", "psb"))]
        if pr:
            reads = [r for r in reads if r not in pr]
            writes = list(writes) + [r for r in pr if r not in writes]
        deps = []
        for r in reads:
            w = self.last_w.get(r)
            if w is not None:
                deps.append(w)
        for w_ in writes:
            w = self.last_w.get(w_)
            if w is not None:
                deps.append(w)
            deps.extend(self.readers.get(w_, ()))
        if eng in self.pending:
            deps.extend(self.pending.pop(eng))
        op = Op(eng, _snap(fn), deps, dma_key)
        for r in reads:
            self.readers.setdefault(r, []).append(op)
        for w_ in writes:
            self.last_w[w_] = op
            self.readers[w_] = []
        self.ops[eng].append(op)
        self.all_ops.append(op)
        if dma_key is not None:
            self.last_dma[dma_key] = op
        return op

    def barrier(self):
        deps = [self.ops[e][-1] for e in self.ENGS if self.ops[e]]
        deps += list(self.last_dma.values())
        for e in self.ENGS:
            self.pending[e] = list(deps) + self.pending.get(e, [])
        self.last_w = {}
        self.readers = {}

    def emit(self, final_ops=()):
        nc = self.nc
        for op in self.all_ops:
            for d in op.deps:
                if d is op:
                    continue
                if d.eng == "pe" and op.eng == "pe" and d.dma_key is None and op.dma_key is None:
                    continue
                d.signal = True
        for op in final_ops:
            op.signal = True
        eng_cnt = {e: 0 for e in self.ENGS}
        key_cnt = {}
        names = []
        for op in self.all_ops:
            if op.dma_key is not None:
                k = ("dma", op.dma_key)
                key_cnt[k] = key_cnt.get(k, 0) + 16
                op.event = (k, key_cnt[k])
                if k not in names:
                    names.append(k)
        for e in self.ENGS:
            for op in self.ops[e]:
                if op.dma_key is not None:
                    continue
                elif op.signal:
                    eng_cnt[e] += 1
                    c = eng_cnt[e]
                    k = ("eng", e, (c - 1) // SEM_ROT)
                    op.event = (k, (c - 1) % SEM_ROT + 1)
                else:
                    continue
                if op.event[0] not in names:
                    names.append(op.event[0])
        self.n_sems = len(names)
        with contextlib.ExitStack() as st:
            sems = {}
            for i, k in enumerate(names):
                sems[k] = st.enter_context(nc.semaphore("s%d" % i))
            block = st.enter_context(nc.Block())
            engmap = {"pe": block.tensor, "act": block.scalar, "dve": block.vector,
                      "pool": block.gpsimd, "sp": block.sync}
            for e in self.ENGS:
                ops = self.ops[e]
                if not ops and e != "sp":
                    continue

                def body(eng, ops=ops, e=e):
                    known = {}
                    for op in ops:
                        need = {}
                        for d in op.deps:
                            if d is op or d.event is None:
                                continue
                            if d.eng == "pe" and e == "pe" and d.dma_key is None and op.dma_key is None:
                                continue
                            k, v = d.event
                            if need.get(k, 0) < v:
                                need[k] = v
                        for k, v in need.items():
                            if known.get(k, 0) < v:
                                eng.wait_ge(sems[k], v)
                                known[k] = v
                        ins = op.fn(eng)
                        if op.event is not None:
                            ins.then_inc(sems[op.event[0]], 16 if op.dma_key is not None else 1)
                    if e == "sp":
                        for op in final_ops:
                            k, v = op.event
                            if known.get(k, 0) < v:
                                eng.wait_ge(sems[k], v)
                                known[k] = v
                engmap[e](body)


def model_consts(E):
    H, dk, C = 4, 256, 128
    log_g = np.log(1.0 - 2.0 ** (-5.0 - np.arange(H, dtype=np.float64)))
    idx = np.arange(C, dtype=np.float64)
    diff = idx[:, None] - idx[None, :]
    intra = np.where(diff >= 0, np.exp(log_g[:, None, None] * np.maximum(diff, 0.0)), 0.0)
    maskT = np.transpose(intra, (0, 2, 1)) * dk ** -0.5
    decay_q = np.exp(log_g[None, :] * (idx[:, None] + 1.0))
    decay_k = np.exp(log_g[None, :] * (C - 1.0 - idx[:, None])) * dk ** -0.5
    decay_chunk = np.exp(log_g * C)
    inv_r = (10000.0 ** (-np.arange(0, 256, 2, dtype=np.float32) / np.float32(256))).astype(np.float32)
    inv_m = (10000.0 ** (-np.arange(0, 64, 2, dtype=np.float32) / np.float32(64))).astype(np.float32)
    c = {}
    c["ident"] = np.eye(128, dtype=np.float32)
    c["maskT"] = np.ascontiguousarray(np.transpose(maskT, (1, 0, 2))).astype(np.float32)
    c["dq"] = decay_q.astype(np.float32)
    c["dk"] = decay_k.astype(np.float32)
    c["inv_r"] = np.tile(inv_r[None, :], (128, 1)).astype(np.float32)
    c["inv_m"] = np.tile(inv_m[None, :], (128, 1)).astype(np.float32)
    c["causal"] = (np.arange(128)[:, None] <= np.arange(128)[None, :]).astype(np.float32)
    c["tri"] = (np.arange(128)[:, None] < np.arange(128)[None, :]).astype(np.float32)
    c["iota_e"] = np.tile(np.arange(E, dtype=np.float32)[None, :], (128, 1))
    return c, [float(v) for v in decay_chunk]


CONST_SHAPES = lambda E: {"ident": [128, 128], "maskT": [128, 4, 128], "dq": [128, 4], "dk": [128, 4],
                          "inv_r": [128, 128], "inv_m": [128, 32], "causal": [128, 128], "tri": [128, 128],
                          "iota_e": [128, E]}


def build(S, E, DEPTH, NA, CAP, dbg=False):
    import os
    LIMIT = int(os.environ.get('KLIMIT', '999'))
    PH = [0]

    def skip():
        PH[0] += 1
        return PH[0] > LIMIT
    NT = S // 128
    NB = DEPTH - NA
    TOPK = 4
    ALPHA = float((2 * DEPTH) ** 0.25)
    SCALE = float(192 ** -0.5)
    NSLOT = E * CAP
    CT = CAP // 128
    consts_np, dchunk = model_consts(E)

    nc = bass.Bass("TRN2", target_bir_lowering=False)

    def din(name, shape, dt=F32):
        return nc.dram_tensor(name, list(shape), dt, kind="ExternalInput")

    x_d = din("x", [S, D])
    p_d = din("p", [DEPTH, S, 256])
    pos_d = din("pos_tm", [128, NT], I32)
    W = {}
    W["ret_w_in"] = din("ret_w_in", [NA, D, 6144])
    W["ret_gn_g"] = din("ret_gn_g", [NA, 2048])
    W["ret_gn_b"] = din("ret_gn_b", [NA, 2048])
    W["ret_w_out"] = din("ret_w_out", [NA, 2048, D])
    W["mla_w_kv_a"] = din("mla_w_kv_a", [D, 320])
    W["mla_kv_norm_g"] = din("mla_kv_norm_g", [1, 256])
    W["mla_w_kv_b"] = din("mla_w_kv_b", [256, 2048])
    W["mla_w_q_a"] = din("mla_w_q_a", [NB, D, 256])
    W["mla_q_norm_g"] = din("mla_q_norm_g", [NB, 256])
    W["mla_w_q_b"] = din("mla_w_q_b", [NB, 256, 1536])
    W["mla_w_o"] = din("mla_w_o", [NB, D, D])
    for n in ("ln_mix_g", "ln_mix_b", "ln_ffn_g", "ln_ffn_b"):
        W[n] = din(n, [DEPTH, D])
    W["moe_w_router"] = din("moe_w_router", [DEPTH, D, E])
    W["moe_b_router"] = din("moe_b_router", [DEPTH, E])
    W["moe_w_gate_up"] = din("moe_w_gate_up", [DEPTH, E, D, 2048])
    W["moe_b_gate_up"] = din("moe_b_gate_up", [DEPTH, E, 128, 16])
    W["moe_w_down"] = din("moe_w_down", [DEPTH, E, D, D])
    W["moe_b_down"] = din("moe_b_down", [DEPTH, E, D])
    W["ple_w_gate"] = din("ple_w_gate", [DEPTH, D, D])
    W["ple_w_proj"] = din("ple_w_proj", [DEPTH, 256, D])
    CD = {k: din("c_" + k, shp) for k, shp in CONST_SHAPES(E).items()}
    y_d = nc.dram_tensor("y", [S, D], F32, kind="ExternalOutput")
    dbg_d = nc.dram_tensor("dbg", [DEPTH, S, D], F32, kind="ExternalOutput") if dbg else None
    dq_d = nc.dram_tensor("dq", [S, 6144], BF16, kind="ExternalOutput") if dbg else None
    dq2_d = nc.dram_tensor("dq2", [S, 6144], BF16, kind="ExternalOutput") if dbg else None

    hbuf = nc.dram_tensor("hbuf", [S, D], F32)
    qkvg_d = nc.dram_tensor("qkvg", [S, 6144], BF16)
    xs_d = nc.dram_tensor("xs", [NSLOT, D], BF16)
    ys_d = nc.dram_tensor("ys", [NSLOT, D], F32)
    KT_d = nc.dram_tensor("KT", [8, 128, S], BF16)
    KR_d = nc.dram_tensor("KR", [65, S], BF16)
    V_d = nc.dram_tensor("Vx", [S, 8, 130], BF16)
    QT_d = nc.dram_tensor("QT", [8, 128, S], BF16)
    QR_d = nc.dram_tensor("QR", [8, 65, S], BF16)
    O_d = nc.dram_tensor("Oa", [S, D], BF16)

    P = Prog(nc)
    uid = [0]

    def bc_row(handle, row_off, n):
        return bass.AP(handle, row_off, [[0, 128], [1, n]])

    with contextlib.ExitStack() as root:
        sbstate = {"cur": 0, "persist": 0, "st": None}
        DTB = {F32: 4, BF16: 2, I32: 4, U32: 4}

        def sb(st, name, shape, dt):
            uid[0] += 1
            nb = DTB[dt]
            for d_ in shape[1:]:
                nb *= d_
            nb = (nb + 63) // 64 * 64
            if True:
                return nc.alloc_sbuf_tensor("%s_%d" % (name, uid[0]), list(shape), dt) if st is root else st.enter_context(nc.sbuf_tensor("%s_%d" % (name, uid[0]), list(shape), dt))
            if st is root:
                assert sbstate["st"] is None
                off = sbstate["persist"]
                sbstate["persist"] += nb
            else:
                if sbstate["st"] is not st:
                    sbstate["st"] = st
                    sbstate["cur"] = sbstate["persist"]
                off = sbstate["cur"]
                sbstate["cur"] += nb
                assert sbstate["cur"] <= 190 * 1024, ("SBUF overflow", name, sbstate["cur"])
            return nc.alloc_sbuf_tensor_at("%s_%d" % (name, uid[0]), list(shape), dt, offset=off)

        psf = [root.enter_context(nc.psum_tensor("psf%d" % i, [128, 512], F32)) for i in range(6)]
        psb = [root.enter_context(nc.psum_tensor("psb%d" % i, [128, 1024], BF16)) for i in range(2)]
        PSF = ["psf%d" % i for i in range(6)]
        PSB = ["psb%d" % i for i in range(2)]

        ident_f = sb(root, "ident_f", [128, 128], F32)
        ident_b = sb(root, "ident_b", [128, 128], BF16)
        ones_b = sb(root, "ones_b", [128, 128], BF16)
        ones_f = sb(root, "ones_f", [128, 128], F32)
        eps_ln = sb(root, "eps_ln", [128, 1], F32)
        eps_6 = sb(root, "eps_6", [128, 1], F32)
        posf = sb(root, "posf", [128, NT], F32)
        posi = sb(root, "posi", [128, NT], I32)
        inv_r = sb(root, "inv_r", [128, 128], F32)
        inv_m = sb(root, "inv_m", [128, 32], F32)
        slots_i = sb(root, "slots_i", [128, NT, TOPK], I32)
        gates_s = sb(root, "gates_s", [128, NT, TOPK], F32)

        def dma(eng, out, in_, reads, writes, key):
            return P.add(eng, lambda e: e.dma_start(out=out, in_=in_), reads, writes, dma_key=key)

        dma("sp", ident_f[:], CD["ident"].ap(), [], ["ident_f"], "ident_f")
        dma("sp", posi[:], pos_d.ap(), [], ["posi"], "posi")
        dma("sp", inv_r[:], CD["inv_r"].ap(), [], ["inv_r"], "inv_r")
        dma("sp", inv_m[:], CD["inv_m"].ap(), [], ["inv_m"], "inv_m")
        P.add("dve", lambda e: e.tensor_copy(out=ident_b[:], in_=ident_f[:]), ["ident_f"], ["ident_b"])
        P.add("dve", lambda e: e.tensor_copy(out=posf[:], in_=posi[:]), ["posi"], ["posf"])
        zeros_f = sb(root, "zeros_f", [128, 1024], F32)
        P.add("dve", lambda e: e.memset(zeros_f[:], 0.0), [], ["zeros_f"])
        P.add("pool", lambda e: e.memset(ones_f[:], 1.0), [], ["ones_f"])
        P.add("pool", lambda e: e.tensor_copy(out=ones_b[:], in_=ones_f[:]), ["ones_f"], ["ones_b"])
        P.add("pool", lambda e: e.memset(eps_ln[:], DN_EPS), [], ["eps_ln"])
        P.add("pool", lambda e: e.memset(eps_6[:], 1e-6), [], ["eps_6"])
        dma("sp", hbuf.ap(), x_d.ap(), [], ["hbuf"], "x2h")
        P.barrier()

        rr = {"cast": 0, "ps": 0}

        def load_w_bf16(st, dst, dst_name, src2d, KC, N, stage, nstage, engs=("act", "pool")):
            CH = stage[0].shape[1]
            i = 0
            for kc in range(KC):
                for n0 in range(0, N, CH):
                    n1 = min(N, n0 + CH)
                    b = rr["cast"] % nstage
                    rr["cast"] += 1
                    sname = "wst%d" % b
                    dma("sp", stage[b][:, 0:n1 - n0], src2d[kc * 128:(kc + 1) * 128, n0:n1], [], [sname], sname)
                    eng = engs[rr["cast"] % len(engs)]
                    if eng == "act":
                        P.add("act", lambda e, b=b, kc=kc, n0=n0, n1=n1: e.copy(out=dst[:, kc, n0:n1], in_=stage[b][:, 0:n1 - n0]),
                              [sname], [dst_name])
                    else:
                        P.add(eng, lambda e, b=b, kc=kc, n0=n0, n1=n1: e.tensor_copy(out=dst[:, kc, n0:n1], in_=stage[b][:, 0:n1 - n0]),
                              [sname], [dst_name])
                    i += 1

        def transpose_blocks(src, src_name, nblk, dst, dst_name, rows=128, blkw=128, src_off=0):
            for g0 in range(0, nblk, 8):
                g1 = min(nblk, g0 + 8)
                pb = rr["ps"] % 2
                rr["ps"] += 1
                for j in range(g0, g1):
                    P.add("pe", lambda e, j=j, pb=pb, g0=g0: e.transpose(
                        out=psb[pb][0:blkw, (j - g0) * 128:(j - g0 + 1) * 128],
                        in_=src[:, src_off + j * blkw: src_off + (j + 1) * blkw], identity=ident_b[:]),
                        [src_name, "ident_b"], [PSB[pb]])
                P.add("act", lambda e, pb=pb, g0=g0, g1=g1: e.copy(
                    out=dst[0:blkw, g0:g1, :], in_=psb[pb][0:blkw, 0:(g1 - g0) * 128].rearrange("p (j t) -> p j t", t=128)),
                    [PSB[pb]], [dst_name])

        def rstd_from(var_ap, eps_tile, out_ap, tmp_ap, names_r, name_w, scale=1.0):
            epsv = DN_EPS if eps_tile is eps_ln else 1e-6
            P.add("dve", lambda e: e.tensor_scalar(out=tmp_ap, in0=var_ap, scalar1=float(scale), scalar2=float(epsv), op0=ALU.mult, op1=ALU.add),
                  list(names_r), [name_w + "_t"])
            P.add("act", lambda e: e.activation(out=tmp_ap, in_=tmp_ap, func=AF.Ln), [name_w + "_t"], [name_w + "_t"])
            P.add("act", lambda e: e.activation(out=out_ap, in_=tmp_ap, func=AF.Exp, scale=-0.5),
                  [name_w + "_t"], [name_w])

        def layer_norm(z, zname, out, oname, g_t, b_t, gb_names, small, sname):
            for c in range(2):
                P.add("dve", lambda e, c=c: e.bn_stats(out=small[:, c * 6:(c + 1) * 6], in_=z[:, c * 512:(c + 1) * 512]),
                      [zname], [sname])
            P.add("dve", lambda e: e.bn_aggr(out=small[:, 12:14], in_=small[:, 0:12].rearrange("p (c s) -> p c s", s=6)),
                  [sname], [sname])
            rstd_from(small[:, 13:14], eps_ln, small[:, 15:16], small[:, 14:15], [sname], sname + "r")
            P.add("dve", lambda e: e.tensor_scalar(out=out[:], in0=z[:], scalar1=small[:, 12:13], scalar2=small[:, 15:16],
                                                   op0=ALU.subtract, op1=ALU.mult), [zname, sname, sname + "r"], [oname])
            P.add("dve", lambda e: e.tensor_tensor(out=out[:], in0=out[:], in1=g_t[:], op=ALU.mult), [oname, gb_names[0]], [oname])
            P.add("dve", lambda e: e.tensor_tensor(out=out[:], in0=out[:], in1=b_t[:], op=ALU.add), [oname, gb_names[1]], [oname])

        def rope_tables(t, inv_t, nf, cos_t, sin_t, tmpf, tmpi, tag):
            for which, dst, shift in (("s", sin_t, 0.0), ("c", cos_t, float(np.pi / 2))):
                nm = tag + which
                P.add("dve", lambda e, dst=dst, shift=shift: e.tensor_scalar(
                    out=dst[:, 0:nf], in0=inv_t[:, 0:nf], scalar1=posf[:, t:t + 1], scalar2=shift, op0=ALU.mult, op1=ALU.add),
                    ["posf", "inv"], [nm])
                P.add("dve", lambda e, dst=dst: e.tensor_scalar(out=tmpf[:, 0:nf], in0=dst[:, 0:nf], scalar1=float(1 / (2 * np.pi)),
                                                                scalar2=None, op0=ALU.mult), [nm], [tag + "tf"])
                P.add("dve", lambda e: e.tensor_copy(out=tmpi[:, 0:nf], in_=tmpf[:, 0:nf]), [tag + "tf"], [tag + "ti"])
                P.add("dve", lambda e: e.tensor_copy(out=tmpf[:, 0:nf], in_=tmpi[:, 0:nf]), [tag + "ti"], [tag + "tf"])
                P.add("dve", lambda e, dst=dst: e.scalar_tensor_tensor(out=dst[:, 0:nf], in0=tmpf[:, 0:nf], scalar=float(-2 * np.pi),
                                                                       in1=dst[:, 0:nf], op0=ALU.mult, op1=ALU.add), [tag + "tf", nm], [nm])
                P.add("dve", lambda e, dst=dst: e.tensor_scalar(out=tmpf[:, 0:nf], in0=dst[:, 0:nf], scalar1=float(np.pi), scalar2=float(-2 * np.pi),
                                                                op0=ALU.is_gt, op1=ALU.mult), [nm], [tag + "tf"])
                P.add("dve", lambda e, dst=dst: e.tensor_tensor(out=dst[:, 0:nf], in0=dst[:, 0:nf], in1=tmpf[:, 0:nf], op=ALU.add), [nm, tag + "tf"], [nm])
                P.add("dve", lambda e, dst=dst: e.tensor_scalar(out=tmpf[:, 0:nf], in0=dst[:, 0:nf], scalar1=float(-np.pi), scalar2=float(2 * np.pi),
                                                                op0=ALU.is_lt, op1=ALU.mult), [nm], [tag + "tf"])
                P.add("dve", lambda e, dst=dst: e.tensor_tensor(out=dst[:, 0:nf], in0=dst[:, 0:nf], in1=tmpf[:, 0:nf], op=ALU.add), [nm, tag + "tf"], [nm])
                P.add("act", lambda e, dst=dst: e.activation(out=dst[:, 0:nf], in_=dst[:, 0:nf], func=AF.Sin), [nm], [nm])

        def rope_apply(x1, x2, o1, o2, cos_t, sin_t, nf, t1, t2, rnames, wname, tag):
            P.add("dve", lambda e: e.tensor_tensor(out=t1[:, 0:nf], in0=x1, in1=cos_t[:, 0:nf], op=ALU.mult), rnames + [tag + "c"], [tag + "t1"])
            P.add("dve", lambda e: e.tensor_tensor(out=t2[:, 0:nf], in0=x2, in1=sin_t[:, 0:nf], op=ALU.mult), rnames + [tag + "s"], [tag + "t2"])
            P.add("pool", lambda e: e.tensor_tensor(out=o1, in0=t1[:, 0:nf], in1=t2[:, 0:nf], op=ALU.subtract), [tag + "t1", tag + "t2"], [wname])
            P.add("dve", lambda e: e.tensor_tensor(out=t1[:, 0:nf], in0=x1, in1=sin_t[:, 0:nf], op=ALU.mult), rnames + [tag + "s"], [tag + "t1"])
            P.add("dve", lambda e: e.tensor_tensor(out=t2[:, 0:nf], in0=x2, in1=cos_t[:, 0:nf], op=ALU.mult), rnames + [tag + "c"], [tag + "t2"])
            P.add("pool", lambda e: e.tensor_tensor(out=o2, in0=t1[:, 0:nf], in1=t2[:, 0:nf], op=ALU.add), [tag + "t1", tag + "t2"], [wname])

        def linear(xT, xT_name, KC, w, w_name, n0, n1, bank, extra=None):
            for kc in range(KC):
                P.add("pe", lambda e, kc=kc: e.matmul(psf[bank][:, 0:n1 - n0], lhsT=xT[:, kc, :], rhs=w[:, kc, n0:n1],
                                                     start=(kc == 0), stop=(kc == KC - 1 and extra is None)),
                      [xT_name, w_name], [PSF[bank]])

        def retention_layer(l):
            if skip():
                return
            with contextlib.ExitStack() as st:
                win = sb(st, "win", [128, 8, 6144], BF16)
                stage = [sb(st, "wst", [128, 2048], F32) for _ in range(2)]
                load_w_bf16(st, win, "win", W["ret_w_in"].ap()[l], 8, 6144, stage, 2)
                hs = [sb(st, "hs", [128, D], F32) for _ in range(2)]
                hb = sb(st, "hb", [128, D], BF16)
                hT = sb(st, "hT", [128, 8, 128], BF16)
                row = [sb(st, "row", [128, 6144], BF16) for _ in range(2)]
                cos_t = sb(st, "cos", [128, 128], F32)
                sin_t = sb(st, "sin", [128, 128], F32)
                tf = sb(st, "tf", [128, 128], F32)
                ti = sb(st, "ti", [128, 128], I32)
                t1 = sb(st, "t1", [128, 128], F32)
                t2 = sb(st, "t2", [128, 128], F32)
                P.last_w["inv"] = P.last_w.get("inv_r")
                for t in range(NT):
                    b = t % 2
                    dma("sp", hs[b][:], hbuf.ap()[t * 128:(t + 1) * 128, :], [("hbuf", t)], ["hs%d" % b], "hs%d" % b)
                    P.add("pool", lambda e, b=b: e.tensor_copy(out=hb[:], in_=hs[b][:]), ["hs%d" % b], ["hb"])
                    transpose_blocks(hb, "hb", 8, hT, "hT")
                    rope_tables(t, inv_r, 128, cos_t, sin_t, tf, ti, "rp")
                    rw = "row%d" % b
                    for n in range(12):
                        bank = n % 4
                        linear(hT, "hT", 8, win, "win", n * 512, (n + 1) * 512, bank)
                        if n < 4:
                            for hh in range(2):
                                c0 = hh * 256
                                rope_apply(psf[bank][:, c0:c0 + 128], psf[bank][:, c0 + 128:c0 + 256],
                                           row[b][:, n * 512 + c0: n * 512 + c0 + 128], row[b][:, n * 512 + c0 + 128: n * 512 + c0 + 256],
                                           cos_t, sin_t, 128, t1, t2, [PSF[bank]], rw, "rp")
                        elif n < 8:
                            P.add("act", lambda e, n=n, b=b, bank=bank: e.copy(out=row[b][:, n * 512:(n + 1) * 512], in_=psf[bank][:, :]),
                                  [PSF[bank]], [rw])
                        else:
                            P.add("act", lambda e, n=n, b=b, bank=bank: e.activation(out=row[b][:, n * 512:(n + 1) * 512], in_=psf[bank][:, :], func=AF.Silu),
                                  [PSF[bank]], [rw])
                    dma("act", qkvg_d.ap()[t * 128:(t + 1) * 128, :], row[b][:], [rw], [("qkvg", t)], rw)
            P.barrier()
            if dbg and l == 0:
                dma("sp", dq2_d.ap(), qkvg_d.ap(), [], [], "dq2dbg")
                P.barrier()
            if skip():
                return
            with contextlib.ExitStack() as st:
                wout = sb(st, "wout", [128, 16, D], BF16)
                stage = [sb(st, "wst", [128, 1024], F32) for _ in range(2)]
                load_w_bf16(st, wout, "wout", W["ret_w_out"].ap()[l], 16, D, stage, 2)
                gn_g = sb(st, "gn_g", [128, 2048], F32)
                gn_b = sb(st, "gn_b", [128, 2048], F32)
                ln_g = sb(st, "ln_g", [128, D], F32)
                ln_b = sb(st, "ln_b", [128, D], F32)
                dma("sp", gn_g[:], bc_row(W["ret_gn_g"], l * 2048, 2048), [], ["gn_g"], "gn_g")
                dma("sp", gn_b[:], bc_row(W["ret_gn_b"], l * 2048, 2048), [], ["gn_b"], "gn_b")
                dma("sp", ln_g[:], bc_row(W["ln_mix_g"], l * D, D), [], ["ln_g"], "ln_g")
                dma("sp", ln_b[:], bc_row(W["ln_mix_b"], l * D, D), [], ["ln_b"], "ln_b")
                maskT = sb(st, "maskT", [128, 4, 128], F32)
                dq = sb(st, "dq", [128, 4], F32)
                dkc = sb(st, "dkc", [128, 4], F32)
                dma("sp", maskT[:], CD["maskT"].ap(), [], ["maskT"], "maskT")
                dma("sp", dq[:], CD["dq"].ap(), [], ["dq"], "dq")
                dma("sp", dkc[:], CD["dk"].ap(), [], ["dkc"], "dkc")
                Sf = sb(st, "Sf", [128, 8, 512], F32)
                Sb = sb(st, "Sb", [128, 8, 512], BF16)
                P.add("dve", lambda e: e.memset(Sf[:], 0.0), [], ["Sf"])
                for i_ in range(8):
                    P.add("dve", lambda e, i_=i_: e.tensor_copy(out=Sb[:, i_, :], in_=zeros_f[:, 0:512]), ["zeros_f"], ["Sb"])
                row = [sb(st, "row", [128, 6144], BF16) for _ in range(2)]
                hs = [sb(st, "hs", [128, D], F32) for _ in range(2)]
                qd = sb(st, "qd", [128, D], BF16)
                kd = sb(st, "kd", [128, D], BF16)
                qT = sb(st, "qT", [128, 8, 128], BF16)
                qdT = sb(st, "qdT", [128, 8, 128], BF16)
                kT = sb(st, "kT", [128, 8, 128], BF16)
                innerT = sb(st, "innerT", [128, 128], BF16)
                yn = sb(st, "yn", [128, 512], F32)
                yg = sb(st, "yg", [128, 2048], BF16)
                ygT = sb(st, "ygT", [128, 16, 128], BF16)
                z = sb(st, "z", [128, D], F32)
                h1 = [sb(st, "h1", [128, D], F32) for _ in range(2)]
                small = sb(st, "small", [128, 32], F32)
                gsm = sb(st, "gsm", [128, 32], F32)
                ydb = sb(st, "ydb", [128, 512], F32)
                for t in range(NT):
                    b = t % 2
                    rw = "row%d" % b
                    R = row[b]
                    dma("sp", R[:], qkvg_d.ap()[t * 128:(t + 1) * 128, :], [("qkvg", t)], [rw], rw)
                    dma("sp", hs[b][:], hbuf.ap()[t * 128:(t + 1) * 128, :], [("hbuf", t)], ["hs%d" % b], "hs%d" % b)
                    for h in range(4):
                        P.add("dve", lambda e, h=h, R=R: e.tensor_scalar(out=qd[:, h * 256:(h + 1) * 256], in0=R[:, h * 256:(h + 1) * 256],
                                                                          scalar1=dq[:, h:h + 1], scalar2=None, op0=ALU.mult), [rw, "dq"], ["qd"])
                        P.add("pool", lambda e, h=h, R=R: e.tensor_scalar(out=kd[:, h * 256:(h + 1) * 256], in0=R[:, 1024 + h * 256:1024 + (h + 1) * 256],
                                                                           scalar1=dkc[:, h:h + 1], scalar2=None, op0=ALU.mult), [rw, "dkc"], ["kd"])
                    transpose_blocks(R, rw, 8, qT, "qT", src_off=0)
                    transpose_blocks(R, rw, 8, kT, "kT", src_off=1024)
                    transpose_blocks(qd, "qd", 8, qdT, "qdT")
                    for h in range(4):
                        vs = R[:, 2048 + h * 512: 2048 + (h + 1) * 512]
                        for j in range(2):
                            P.add("pe", lambda e, h=h, j=j: e.matmul(psf[4][:, 0:128], lhsT=kT[:, 2 * h + j, :], rhs=qT[:, 2 * h + j, :],
                                                                    start=(j == 0), stop=(j == 1)), ["kT", "qT"], [PSF[4]])
                        P.add("dve", lambda e, h=h: e.tensor_tensor(out=innerT[:], in0=psf[4][:, 0:128], in1=maskT[:, h, :], op=ALU.mult),
                              [PSF[4], "maskT"], ["innerT"])
                        yb = h % 2
                        NOACC = os.environ.get("KNOACC", "") == "1"
                        P.add("pe", lambda e, vs=vs, yb=yb: e.matmul(psf[yb][:, :], lhsT=innerT[:], rhs=vs, start=True, stop=NOACC),
                              ["innerT", rw], [PSF[yb]])
                        for j in range(0 if NOACC else 2):
                            P.add("pe", lambda e, h=h, j=j, yb=yb: e.matmul(psf[yb][:, :], lhsT=qdT[:, 2 * h + j, :], rhs=Sb[:, 2 * h + j, :],
                                                                           start=False, stop=(j == 1)), ["qdT", "Sb"], [PSF[yb]])
                        if os.environ.get("KDBG", "") in ("y", "rv") and h == 3:
                            P.add("dve", lambda e, yb=yb, b=b: e.tensor_copy(out=ydb[:], in_=psf[yb][:, :]), [PSF[yb]], ["ydb"])
                        P.add("dve", lambda e, yb=yb: e.bn_stats(out=gsm[:, 0:6], in_=psf[yb][:, :]), [PSF[yb]], ["gsm"])
                        P.add("dve", lambda e: e.bn_aggr(out=gsm[:, 6:8], in_=gsm[:, 0:6]), ["gsm"], ["gsm"])
                        rstd_from(gsm[:, 7:8], eps_6, gsm[:, 9:10], gsm[:, 8:9], ["gsm"], "gsmr")
                        P.add("dve", lambda e, yb=yb: e.tensor_scalar(out=yn[:], in0=psf[yb][:, :], scalar1=gsm[:, 6:7], scalar2=gsm[:, 9:10],
                                                                      op0=ALU.subtract, op1=ALU.mult), [PSF[yb], "gsm", "gsmr"], ["yn"])
                        P.add("dve", lambda e, h=h: e.tensor_tensor(out=yn[:], in0=yn[:], in1=gn_g[:, h * 512:(h + 1) * 512], op=ALU.mult), ["yn", "gn_g"], ["yn"])
                        P.add("dve", lambda e, h=h: e.tensor_tensor(out=yn[:], in0=yn[:], in1=gn_b[:, h * 512:(h + 1) * 512], op=ALU.add), ["yn", "gn_b"], ["yn"])
                        P.add("pool", lambda e, h=h, R=R: e.tensor_tensor(out=yg[:, h * 512:(h + 1) * 512], in0=yn[:], in1=R[:, 4096 + h * 512:4096 + (h + 1) * 512],
                                                                           op=ALU.mult), ["yn", rw], ["yg"])
                        for j in range(2):
                            sbk = 2 + j
                            P.add("pe", lambda e, h=h, j=j, vs=vs, sbk=sbk: e.matmul(psf[sbk][:, :], lhsT=kd[:, h * 256 + j * 128: h * 256 + (j + 1) * 128],
                                                                                    rhs=vs, start=True, stop=True), ["kd", rw], [PSF[sbk]])
                            P.add("dve", lambda e, h=h, j=j, sbk=sbk: e.scalar_tensor_tensor(out=Sf[:, 2 * h + j, :], in0=Sf[:, 2 * h + j, :], scalar=dchunk[h],
                                                                                             in1=psf[sbk][:, :], op0=ALU.mult, op1=ALU.add),
                                  ["Sf", PSF[sbk]], ["Sf"])
                            P.add("dve", lambda e, h=h, j=j: e.tensor_copy(out=Sb[:, 2 * h + j, :], in_=Sf[:, 2 * h + j, :]), ["Sf"], ["Sb"])
                    transpose_blocks(yg, "yg", 16, ygT, "ygT")
                    for nh in range(2):
                        for fc in range(16):
                            P.add("pe", lambda e, nh=nh, fc=fc: e.matmul(psf[nh][:, :], lhsT=ygT[:, fc, :], rhs=wout[:, fc, nh * 512:(nh + 1) * 512],
                                                                        start=(fc == 0), stop=(fc == 15)), ["ygT", "wout"], [PSF[nh]])
                        P.add("dve", lambda e, nh=nh, b=b: e.scalar_tensor_tensor(out=z[:, nh * 512:(nh + 1) * 512], in0=hs[b][:, nh * 512:(nh + 1) * 512],
                                                                                  scalar=ALPHA, in1=psf[nh][:, :], op0=ALU.mult, op1=ALU.add),
                              ["hs%d" % b, PSF[nh]], ["z"])
                    layer_norm(z, "z", h1[b], "h1%d" % b, ln_g, ln_b, ["ln_g", "ln_b"], small, "small")
                    kd_ = os.environ.get("KDBG", "")
                    if kd_ == "z":
                        P.add("dve", lambda e, b=b: e.tensor_copy(out=h1[b][:], in_=z[:]), ["z", "h1%d" % b], ["h1%d" % b])
                    elif kd_ == "yg":
                        P.add("dve", lambda e, b=b: e.tensor_copy(out=h1[b][:], in_=yg[:, 0:1024]), ["yg", "h1%d" % b], ["h1%d" % b])
                    elif kd_ == "y":
                        P.add("dve", lambda e, b=b: e.tensor_copy(out=h1[b][:, 0:512], in_=yg[:, 1536:2048]), ["yg", "h1%d" % b], ["h1%d" % b])
                        P.add("dve", lambda e, b=b: e.tensor_copy(out=h1[b][:, 512:1024], in_=ydb[:]), ["ydb", "h1%d" % b], ["h1%d" % b])
                    elif kd_ == "rv":
                        P.add("dve", lambda e, b=b, R=R: e.tensor_copy(out=h1[b][:, 0:512], in_=R[:, 3584:4096]), [rw, "h1%d" % b], ["h1%d" % b])
                        P.add("dve", lambda e, b=b: e.tensor_copy(out=h1[b][:, 512:1024], in_=ydb[:]), ["ydb", "h1%d" % b], ["h1%d" % b])
                    elif kd_ == "yn":
                        P.add("dve", lambda e, b=b: e.tensor_copy(out=h1[b][:, 0:512], in_=yn[:]), ["yn", "h1%d" % b], ["h1%d" % b])
                        P.add("dve", lambda e, b=b: e.tensor_copy(out=h1[b][:, 512:544], in_=gsm[:]), ["gsm", "gsmr", "h1%d" % b], ["h1%d" % b])
                        P.add("dve", lambda e, b=b: e.tensor_copy(out=h1[b][:, 640:768], in_=innerT[:]), ["innerT", "h1%d" % b], ["h1%d" % b])
                    dma("pool", hbuf.ap()[t * 128:(t + 1) * 128, :], h1[b][:], ["h1%d" % b], [("hbuf", t)], "h1%d" % b)
            P.barrier()

        def moe_ple(l, last):
            if skip():
                return
            with contextlib.ExitStack() as st:
                wr = sb(st, "wr", [128, 8, E], F32)
                dma("sp", wr[:], W["moe_w_router"].ap()[l].rearrange("(kc p) e -> p kc e", p=128), [], ["wr"], "wr")
                br = sb(st, "br", [128, E], F32)
                dma("sp", br[:], bc_row(W["moe_b_router"], l * E, E), [], ["br"], "br")
                iota_e = sb(st, "iota_e", [128, E], F32)
                dma("sp", iota_e[:], CD["iota_e"].ap(), [], ["iota_e"], "iota_e")
                ecap = sb(st, "ecap", [128, E], F32)
                P.add("dve", lambda e: e.tensor_scalar(out=ecap[:], in0=iota_e[:], scalar1=float(CAP), scalar2=None, op0=ALU.mult), ["iota_e"], ["ecap"])
                tri_f = sb(st, "tri_f", [128, 128], F32)
                tri_b = sb(st, "tri_b", [128, 128], BF16)
                dma("sp", tri_f[:], CD["tri"].ap(), [], ["tri_f"], "tri_f")
                P.add("dve", lambda e: e.tensor_copy(out=tri_b[:], in_=tri_f[:]), ["tri_f"], ["tri_b"])
                base = sb(st, "base", [128, E], F32)
                P.add("dve", lambda e: e.memset(base[:], 0.0), [], ["base"])
                hs = [sb(st, "hs", [128, D], F32) for _ in range(2)]
                hb = [sb(st, "hb", [128, D], BF16) for _ in range(2)]
                hT32 = sb(st, "hT32", [128, 8, 128], F32)
                lg = sb(st, "lg", [128, E], F32)
                mx = sb(st, "mx", [128, 8], F32)
                mi = sb(st, "mi", [128, 8], U32)
                mif = sb(st, "mif", [128, 8], F32)
                negm = sb(st, "negm", [128, 1], F32)
                ex = sb(st, "ex", [128, 4], F32)
                rs = sb(st, "rs", [128, 2], F32)
                maskf = sb(st, "maskf", [128, E], F32)
                maskb = sb(st, "maskb", [128, E], BF16)
                slotf = sb(st, "slotf", [128, E], F32)
                eq = sb(st, "eq", [128, E], F32)
                s4f = sb(st, "s4f", [128, 4], F32)
                sk = [[sb(st, "sk", [128, 1], I32) for _ in range(2)] for _ in range(TOPK)]
                for t in range(NT):
                    b = t % 2
                    hn = "hs%d" % b
                    dma("sp", hs[b][:], hbuf.ap()[t * 128:(t + 1) * 128, :], [("hbuf", t)], [hn], hn)
                    P.add("pool", lambda e, b=b: e.tensor_copy(out=hb[b][:], in_=hs[b][:]), [hn], ["hb%d" % b])
                    for half in range(2):
                        bank = 2 + half
                        for j in range(4):
                            kc = half * 4 + j
                            P.add("pe", lambda e, kc=kc, j=j, b=b, bank=bank: e.transpose(out=psf[bank][:, j * 128:(j + 1) * 128],
                                                                                         in_=hs[b][:, kc * 128:(kc + 1) * 128], identity=ident_f[:]),
                                  [hn, "ident_f"], [PSF[bank]])
                        P.add("act", lambda e, half=half, bank=bank: e.copy(out=hT32[:, half * 4:(half + 1) * 4, :],
                                                                             in_=psf[bank][:, :].rearrange("p (j t) -> p j t", t=128)),
                              [PSF[bank]], ["hT32"])
                    for kc in range(8):
                        P.add("pe", lambda e, kc=kc: e.matmul(psf[0][:, 0:E], lhsT=hT32[:, kc, :], rhs=wr[:, kc, :], start=(kc == 0), stop=(kc == 7)),
                              ["hT32", "wr"], [PSF[0]])
                    P.add("dve", lambda e: e.tensor_tensor(out=lg[:], in0=psf[0][:, 0:E], in1=br[:], op=ALU.add), [PSF[0], "br"], ["lg"])
                    P.add("dve", lambda e: e.max(out=mx[:], in_=lg[:]), ["lg"], ["mx"])
                    P.add("dve", lambda e: e.max_index(out=mi[:], in_max=mx[:], in_values=lg[:]), ["lg", "mx"], ["mi"])
                    P.add("dve", lambda e: e.tensor_copy(out=mif[:], in_=mi[:]), ["mi"], ["mif"])
                    P.add("dve", lambda e: e.tensor_scalar(out=negm[:], in0=mx[:, 0:1], scalar1=-1.0, scalar2=None, op0=ALU.mult), ["mx"], ["negm"])
                    P.add("dve", lambda e: e.tensor_scalar(out=ex[:], in0=mx[:, 0:4], scalar1=negm[:, 0:1], scalar2=None, op0=ALU.add), ["mx", "negm"], ["ex"])
                    P.add("act", lambda e: e.activation(out=ex[:], in_=ex[:], func=AF.Exp), ["ex"], ["ex"])
                    P.add("dve", lambda e: e.reduce_sum(out=rs[:, 0:1], in_=ex[:], axis=mybir.AxisListType.X), ["ex"], ["rs"])
                    P.add("dve", lambda e: e.reciprocal(out=rs[:, 1:2], in_=rs[:, 0:1]), ["rs"], ["rs2"])
                    P.add("dve", lambda e, t=t: e.tensor_scalar(out=gates_s[:, t, :], in0=ex[:], scalar1=rs[:, 1:2], scalar2=None, op0=ALU.mult),
                          ["ex", "rs2"], ["gates_s"])
                    P.add("dve", lambda e: e.tensor_scalar(out=maskf[:], in0=lg[:], scalar1=mx[:, 3:4], scalar2=None, op0=ALU.is_ge), ["lg", "mx"], ["maskf"])
                    P.add("dve", lambda e: e.tensor_copy(out=maskb[:], in_=maskf[:]), ["maskf"], ["maskb"])
                    P.add("pe", lambda e: e.matmul(psf[1][:, 0:E], lhsT=tri_b[:], rhs=maskb[:], start=True, stop=True), ["tri_b", "maskb"], [PSF[1]])
                    P.add("pe", lambda e: e.matmul(psf[1][:, 64:64 + E], lhsT=ones_b[:], rhs=maskb[:], start=True, stop=True), ["ones_b", "maskb"], [PSF[1]])
                    P.add("dve", lambda e: e.tensor_tensor(out=slotf[:], in0=psf[1][:, 0:E], in1=base[:], op=ALU.add), [PSF[1], "base"], ["slotf"])
                    P.add("dve", lambda e: e.tensor_tensor(out=slotf[:], in0=slotf[:], in1=ecap[:], op=ALU.add), ["slotf", "ecap"], ["slotf"])
                    P.add("dve", lambda e: e.tensor_tensor(out=base[:], in0=base[:], in1=psf[1][:, 64:64 + E], op=ALU.add), [PSF[1], "base", "slotf"], ["base"])
                    for k in range(TOPK):
                        P.add("dve", lambda e, k=k: e.tensor_scalar(out=eq[:], in0=iota_e[:], scalar1=mif[:, k:k + 1], scalar2=None, op0=ALU.is_equal),
                              ["iota_e", "mif"], ["eq"])
                        P.add("dve", lambda e: e.tensor_tensor(out=eq[:], in0=eq[:], in1=slotf[:], op=ALU.mult), ["eq", "slotf"], ["eq"])
                        P.add("dve", lambda e, k=k: e.reduce_sum(out=s4f[:, k:k + 1], in_=eq[:], axis=mybir.AxisListType.X), ["eq"], ["s4f"])
                    P.add("dve", lambda e, t=t: e.tensor_copy(out=slots_i[:, t, :], in_=s4f[:]), ["s4f"], ["slots_i"])
                    for k in range(TOPK):
                        P.add("dve", lambda e, t=t, k=k, b=b: e.tensor_copy(out=sk[k][b][:, :], in_=s4f[:, k:k + 1]), ["s4f"], ["sk%d_%d" % (k, b)])
                        P.add("pool", lambda e, t=t, k=k, b=b: e.indirect_dma_start(
                            out=xs_d.ap(), out_offset=bass.IndirectOffsetOnAxis(ap=sk[k][b][:, :], axis=0),
                            in_=hb[b][:, :], in_offset=None),
                            ["sk%d_%d" % (k, b), "hb%d" % b], ["xs"], dma_key="hb%d" % b)
            P.barrier()
            if skip():
                return
            with contextlib.ExitStack() as st:
                wgu = [sb(st, "wgu", [128, 8, 2048], BF16) for _ in range(2)]
                wdn = [sb(st, "wdn", [128, 8, D], BF16) for _ in range(2)]
                stage = [sb(st, "wst", [128, 2048], F32) for _ in range(3)]
                bgu = [sb(st, "bgu", [128, 16], F32) for _ in range(2)]
                bdf = [sb(st, "bdf", [1, D], F32) for _ in range(2)]
                bdb = [sb(st, "bdb", [1, D], BF16) for _ in range(2)]
                xr = [sb(st, "xr", [128, D], BF16) for _ in range(2)]
                xT = sb(st, "xT", [128, 8, CAP], BF16)
                actT = sb(st, "actT", [128, 8, CAP], BF16)
                gc = [sb(st, "gc", [128, 512], F32) for _ in range(2)]
                sg = [sb(st, "sg", [128, 512], F32) for _ in range(2)]
                uc = [sb(st, "uc", [128, 512], F32) for _ in range(2)]
                yo = [sb(st, "yo", [128, D], F32) for _ in range(2)]
                cnt = 0
                for ex_ in range(E):
                    eb = ex_ % 2
                    load_w_bf16(st, wgu[eb], "wgu%d" % eb, W["moe_w_gate_up"].ap()[l, ex_], 8, 2048, stage, 3, engs=("act", "dve"))
                    load_w_bf16(st, wdn[eb], "wdn%d" % eb, W["moe_w_down"].ap()[l, ex_], 8, D, stage, 3, engs=("act", "dve"))
                    dma("sp", bgu[eb][:], W["moe_b_gate_up"].ap()[l, ex_], [], ["bgu%d" % eb], "bgu%d" % eb)
                    dma("sp", bdf[eb][:], W["moe_b_down"].ap()[l, ex_:ex_ + 1, :], [], ["bdf%d" % eb], "bdf%d" % eb)
                    P.add("dve", lambda e, eb=eb: e.tensor_copy(out=bdb[eb][:], in_=bdf[eb][:]), ["bdf%d" % eb], ["bdb%d" % eb])
                    for s_ in range(CT):
                        xb = cnt % 2
                        cnt += 1
                        r0 = ex_ * CAP + s_ * 128
                        dma("sp", xr[xb][:], xs_d.ap()[r0:r0 + 128, :], ["xs"], ["xr%d" % xb], "xr%d" % xb)
                        pb = rr["ps"] % 2
                        rr["ps"] += 1
                        for j in range(8):
                            P.add("pe", lambda e, j=j, pb=pb, xb=xb: e.transpose(out=psb[pb][:, j * 128:(j + 1) * 128], in_=xr[xb][:, j * 128:(j + 1) * 128],
                                                                                 identity=ident_b[:]), ["xr%d" % xb, "ident_b"], [PSB[pb]])
                        P.add("act", lambda e, pb=pb, s_=s_: e.copy(out=xT[:, :, s_ * 128:(s_ + 1) * 128], in_=psb[pb][:, :].rearrange("p (j t) -> p j t", t=128)),
                              [PSB[pb]], ["xT"])
                    for fc in range(8):
                        for n0 in range(0, CAP, 512):
                            n1 = min(CAP, n0 + 512)
                            nn = n1 - n0
                            w_ = (fc * 8 + n0 // 512) % 2
                            bg_, bu_ = (0, 1) if w_ == 0 else (2, 3)
                            for kc in range(8):
                                P.add("pe", lambda e, kc=kc, fc=fc, n0=n0, n1=n1, bg_=bg_, eb=eb, nn=nn: e.matmul(
                                    psf[bg_][:, 0:nn], lhsT=wgu[eb][:, kc, fc * 128:(fc + 1) * 128], rhs=xT[:, kc, n0:n1], start=(kc == 0), stop=(kc == 7)),
                                    ["wgu%d" % eb, "xT"], [PSF[bg_]])
                            for kc in range(8):
                                P.add("pe", lambda e, kc=kc, fc=fc, n0=n0, n1=n1, bu_=bu_, eb=eb, nn=nn: e.matmul(
                                    psf[bu_][:, 0:nn], lhsT=wgu[eb][:, kc, 1024 + fc * 128:1024 + (fc + 1) * 128], rhs=xT[:, kc, n0:n1], start=(kc == 0), stop=(kc == 7)),
                                    ["wgu%d" % eb, "xT"], [PSF[bu_]])
                            P.add("dve", lambda e, fc=fc, bg_=bg_, eb=eb, nn=nn, w_=w_: e.tensor_scalar(out=gc[w_][:, 0:nn], in0=psf[bg_][:, 0:nn], scalar1=bgu[eb][:, fc:fc + 1],
                                                                                                      scalar2=7.0, op0=ALU.add, op1=ALU.min), [PSF[bg_], "bgu%d" % eb], ["gc%d" % w_])
                            P.add("act", lambda e, nn=nn, w_=w_: e.activation(out=sg[w_][:, 0:nn], in_=gc[w_][:, 0:nn], func=AF.Sigmoid, scale=1.702), ["gc%d" % w_], ["sg%d" % w_])
                            P.add("dve", lambda e, fc=fc, bu_=bu_, eb=eb, nn=nn, w_=w_: e.tensor_scalar(out=uc[w_][:, 0:nn], in0=psf[bu_][:, 0:nn], scalar1=bgu[eb][:, 8 + fc:9 + fc],
                                                                                                      scalar2=-7.0, op0=ALU.add, op1=ALU.max), [PSF[bu_], "bgu%d" % eb], ["uc%d" % w_])
                            P.add("dve", lambda e, nn=nn, w_=w_: e.tensor_scalar(out=uc[w_][:, 0:nn], in0=uc[w_][:, 0:nn], scalar1=7.0, scalar2=1.0, op0=ALU.min, op1=ALU.add),
                                  ["uc%d" % w_], ["uc%d" % w_])
                            P.add("dve", lambda e, nn=nn, w_=w_: e.tensor_tensor(out=gc[w_][:, 0:nn], in0=gc[w_][:, 0:nn], in1=sg[w_][:, 0:nn], op=ALU.mult),
                                  ["gc%d" % w_, "sg%d" % w_], ["gc%d" % w_])
                            P.add("dve", lambda e, fc=fc, n0=n0, n1=n1, nn=nn, w_=w_: e.tensor_tensor(out=actT[:, fc, n0:n1], in0=gc[w_][:, 0:nn], in1=uc[w_][:, 0:nn], op=ALU.mult),
                                  ["gc%d" % w_, "uc%d" % w_], ["actT"])
                    for s_ in range(CT):
                        yb = s_ % 2
                        for nh in range(2):
                            bank = 4 + nh
                            for fc in range(8):
                                P.add("pe", lambda e, fc=fc, s_=s_, nh=nh, bank=bank, eb=eb: e.matmul(
                                    psf[bank][:, :], lhsT=actT[:, fc, s_ * 128:(s_ + 1) * 128], rhs=wdn[eb][:, fc, nh * 512:(nh + 1) * 512], start=(fc == 0), stop=False),
                                    ["actT", "wdn%d" % eb], [PSF[bank]])
                            P.add("pe", lambda e, nh=nh, bank=bank, eb=eb: e.matmul(psf[bank][:, :], lhsT=ones_b[0:1, :], rhs=bdb[eb][0:1, nh * 512:(nh + 1) * 512],
                                                                                  start=False, stop=True), ["ones_b", "bdb%d" % eb], [PSF[bank]])
                            P.add("act", lambda e, nh=nh, bank=bank, yb=yb: e.copy(out=yo[yb][:, nh * 512:(nh + 1) * 512], in_=psf[bank][:, :]), [PSF[bank]], ["yo%d" % yb])
                        r0 = ex_ * CAP + s_ * 128
                        dma("act", ys_d.ap()[r0:r0 + 128, :], yo[yb][:], ["yo%d" % yb], ["ys"], "yo%d" % yb)
            P.barrier()
            if skip():
                return
            with contextlib.ExitStack() as st:
                wg = sb(st, "wg", [128, 8, D], BF16)
                wp = sb(st, "wp", [128, 2, D], BF16)
                stage = [sb(st, "wst", [128, 1024], F32) for _ in range(2)]
                load_w_bf16(st, wg, "wg", W["ple_w_gate"].ap()[l], 8, D, stage, 2)
                load_w_bf16(st, wp, "wp", W["ple_w_proj"].ap()[l], 2, D, stage, 2)
                ln_g = sb(st, "ln_g", [128, D], F32)
                ln_b = sb(st, "ln_b", [128, D], F32)
                dma("sp", ln_g[:], bc_row(W["ln_ffn_g"], l * D, D), [], ["ln_g"], "ln_g")
                dma("sp", ln_b[:], bc_row(W["ln_ffn_b"], l * D, D), [], ["ln_b"], "ln_b")
                yk = [sb(st, "yk", [128, D], F32) for _ in range(4)]
                hs = [sb(st, "hs", [128, D], F32) for _ in range(2)]
                ps_ = [sb(st, "ps_", [128, 256], F32) for _ in range(2)]
                pb_ = sb(st, "pb_", [128, 256], BF16)
                pT = sb(st, "pT", [128, 2, 128], BF16)
                acc = sb(st, "acc", [128, D], F32)
                z = sb(st, "z", [128, D], F32)
                h2 = sb(st, "h2", [128, D], F32)
                h2b = sb(st, "h2b", [128, D], BF16)
                h2T = sb(st, "h2T", [128, 8, 128], BF16)
                sgm = sb(st, "sgm", [128, 512], F32)
                h3 = [sb(st, "h3", [128, D], F32) for _ in range(2)]
                small = sb(st, "small", [128, 32], F32)
                ck = [sb(st, "ck", [128, 1], I32) for _ in range(TOPK)]
                for t in range(NT):
                    b = t % 2
                    hn = "hs%d" % b
                    dma("sp", hs[b][:], hbuf.ap()[t * 128:(t + 1) * 128, :], [("hbuf", t)], [hn], hn)
                    dma("sp", ps_[b][:], p_d.ap()[l, t * 128:(t + 1) * 128, :], [], ["ps_%d" % b], "ps_%d" % b)
                    for k in range(TOPK):
                        P.add("dve", lambda e, t=t, k=k: e.tensor_copy(out=ck[k][:, :], in_=slots_i[:, t, k:k + 1]), ["slots_i"], ["ck%d" % k])
                        P.add("pool", lambda e, t=t, k=k: e.indirect_dma_start(
                            out=yk[k][:, :], out_offset=None, in_=ys_d.ap(), in_offset=bass.IndirectOffsetOnAxis(ap=ck[k][:, :], axis=0)),
                            ["ck%d" % k, "ys"], ["yk%d" % k], dma_key="yk%d" % k)
                    P.add("dve", lambda e, t=t: e.tensor_scalar(out=acc[:], in0=yk[0][:], scalar1=gates_s[:, t, 0:1], scalar2=None, op0=ALU.mult),
                          ["yk0", "gates_s"], ["acc"])
                    for k in range(1, TOPK):
                        P.add("dve", lambda e, t=t, k=k: e.scalar_tensor_tensor(out=acc[:], in0=yk[k][:], scalar=gates_s[:, t, k:k + 1], in1=acc[:],
                                                                                op0=ALU.mult, op1=ALU.add), ["yk%d" % k, "gates_s", "acc"], ["acc"])
                    P.add("dve", lambda e, b=b: e.scalar_tensor_tensor(out=z[:], in0=hs[b][:], scalar=ALPHA, in1=acc[:], op0=ALU.mult, op1=ALU.add),
                          [hn, "acc"], ["z"])
                    layer_norm(z, "z", h2, "h2", ln_g, ln_b, ["ln_g", "ln_b"], small, "small")
                    P.add("act", lambda e: e.copy(out=h2b[:], in_=h2[:]), ["h2"], ["h2b"])
                    transpose_blocks(h2b, "h2b", 8, h2T, "h2T")
                    P.add("act", lambda e, b=b: e.copy(out=pb_[:], in_=ps_[b][:]), ["ps_%d" % b], ["pb_"])
                    transpose_blocks(pb_, "pb_", 2, pT, "pT")
                    for nh in range(2):
                        linear(h2T, "h2T", 8, wg, "wg", nh * 512, (nh + 1) * 512, nh)
                        linear(pT, "pT", 2, wp, "wp", nh * 512, (nh + 1) * 512, 2 + nh)
                        P.add("act", lambda e, nh=nh: e.activation(out=sgm[:], in_=psf[nh][:, :], func=AF.Sigmoid), [PSF[nh]], ["sgm"])
                        P.add("dve", lambda e, nh=nh: e.tensor_tensor(out=sgm[:], in0=sgm[:], in1=psf[2 + nh][:, :], op=ALU.mult), ["sgm", PSF[2 + nh]], ["sgm"])
                        P.add("dve", lambda e, nh=nh, b=b: e.tensor_tensor(out=h3[b][:, nh * 512:(nh + 1) * 512], in0=sgm[:], in1=h2[:, nh * 512:(nh + 1) * 512], op=ALU.add),
                              ["sgm", "h2"], ["h3%d" % b])
                    if last:
                        fin.append(dma("pool", y_d.ap()[t * 128:(t + 1) * 128, :], h3[b][:], ["h3%d" % b], [("y", t)], "h3%d" % b))
                    else:
                        dma("pool", hbuf.ap()[t * 128:(t + 1) * 128, :], h3[b][:], ["h3%d" % b], [("hbuf", t)], "h3%d" % b)
            P.barrier()
            if dbg:
                dma("sp", dbg_d.ap()[l], (y_d if last else hbuf).ap(), [], [], "dbg")
                P.barrier()

        kmax_bc = sb(root, "kmax_bc", [128, 1], F32)

        def sumsq(ps_ap, n, out_col, junk, psname, wname):
            P.add("act", lambda e: e.activation(out=junk[:, 0:n], in_=ps_ap, func=AF.Square), [psname], ["junk"])
            P.add("dve", lambda e: e.reduce_sum(out=out_col, in_=junk[:, 0:n], axis=mybir.AxisListType.X), ["junk"], [wname])

        def rms_norm_ps(ps_ap, psname, n, g_t, gname, out_b, oname, junk, small, sname, col):
            sumsq(ps_ap, n, small[:, col:col + 1], junk, psname, sname)
            rstd_from(small[:, col:col + 1], eps_6, small[:, col + 2:col + 3], small[:, col + 1:col + 2], [sname], sname + "r", scale=1.0 / n)
            P.add("dve", lambda e: e.tensor_scalar(out=junk[:, 0:n], in0=ps_ap, scalar1=small[:, col + 2:col + 3], scalar2=None, op0=ALU.mult),
                  [psname, sname + "r"], ["junk"])
            P.add("pool", lambda e: e.tensor_tensor(out=out_b, in0=junk[:, 0:n], in1=g_t[:, 0:n], op=ALU.mult), ["junk", gname], [oname])

        def mla_kv():
            if skip():
                return
            with contextlib.ExitStack() as st:
                wa = sb(st, "wa", [128, 8, 320], BF16)
                wb = sb(st, "wb", [128, 2, 2048], BF16)
                stage = [sb(st, "wst", [128, 2048], F32) for _ in range(2)]
                load_w_bf16(st, wa, "wa", W["mla_w_kv_a"].ap(), 8, 320, stage, 2)
                load_w_bf16(st, wb, "wb", W["mla_w_kv_b"].ap(), 2, 2048, stage, 2)
                g_t = sb(st, "kvg", [128, 256], F32)
                dma("sp", g_t[:], bc_row(W["mla_kv_norm_g"], 0, 256), [], ["kvg"], "kvg")
                hs = [sb(st, "hs", [128, D], F32) for _ in range(2)]
                hb = sb(st, "hb", [128, D], BF16)
                hT = sb(st, "hT", [128, 8, 128], BF16)
                junk = sb(st, "junk", [128, 512], F32)
                small = sb(st, "small", [128, 32], F32)
                cb = sb(st, "cb", [128, 256], BF16)
                cT = sb(st, "cT", [128, 2, 128], BF16)
                cos_t = sb(st, "cos", [128, 32], F32)
                sin_t = sb(st, "sin", [128, 32], F32)
                tf = sb(st, "tf", [128, 32], F32)
                ti = sb(st, "ti", [128, 32], I32)
                t1 = sb(st, "t1", [128, 32], F32)
                t2 = sb(st, "t2", [128, 32], F32)
                kn = [sb(st, "kn", [128, D], BF16) for _ in range(2)]
                knT = [sb(st, "knT", [128, 8, 128], BF16) for _ in range(2)]
                vx = [sb(st, "vx", [128, 8, 130], BF16) for _ in range(2)]
                kr = sb(st, "kr", [128, 128], BF16)
                krT = [sb(st, "krT", [128, 1, 128], BF16) for _ in range(2)]
                ksq = sb(st, "ksq", [128, 16], F32)
                kmx = sb(st, "kmx", [128, 8], F32)
                P.add("dve", lambda e: e.memset(kmx[:], 0.0), [], ["kmx"])
                P.add("pool", lambda e: e.tensor_copy(out=kr[:], in_=zeros_f[:, 0:128]), ["zeros_f"], ["kr"])
                P.add("pool", lambda e: e.tensor_copy(out=kr[:, 64:65], in_=ones_f[:, 0:1]), ["kr", "ones_f"], ["kr"])
                for b in range(2):
                    for h_ in range(8):
                        P.add("pool", lambda e, b=b, h_=h_: e.tensor_copy(out=vx[b][:, h_, 128:130], in_=ones_f[:, 0:2]), ["ones_f"], ["vx%d" % b])
                P.last_w["inv"] = P.last_w.get("inv_m")
                for t in range(NT):
                    b = t % 2
                    hn = "hs%d" % b
                    dma("sp", hs[b][:], hbuf.ap()[t * 128:(t + 1) * 128, :], [("hbuf", t)], [hn], hn)
                    P.add("pool", lambda e, b=b: e.tensor_copy(out=hb[:], in_=hs[b][:]), [hn], ["hb"])
                    transpose_blocks(hb, "hb", 8, hT, "hT")
                    rope_tables(t, inv_m, 32, cos_t, sin_t, tf, ti, "kv")
                    linear(hT, "hT", 8, wa, "wa", 0, 320, 4)
                    rms_norm_ps(psf[4][:, 0:256], PSF[4], 256, g_t, "kvg", cb[:], "cb", junk, small, "small", 0)
                    rope_apply(psf[4][:, 256:288], psf[4][:, 288:320], kr[:, 0:32], kr[:, 32:64], cos_t, sin_t, 32, t1, t2, [PSF[4]], "kr", "kv")
                    sumsq(psf[4][:, 256:320], 64, ksq[:, 8:9], junk, PSF[4], "ksq8")
                    P.add("pe", lambda e: e.transpose(out=psb[0][:, 0:128], in_=kr[:, 0:128], identity=ident_b[:]), ["kr", "ident_b"], [PSB[0]])
                    P.add("act", lambda e, b=b: e.copy(out=krT[b][:, 0, :], in_=psb[0][:, 0:128]), [PSB[0]], ["krT%d" % b])
                    dma("act", KR_d.ap()[:, t * 128:(t + 1) * 128], krT[b][0:65, 0, :], ["krT%d" % b], [("KR", t)], "krT%d" % b)
                    transpose_blocks(cb, "cb", 2, cT, "cT")
                    for n in range(4):
                        bank = n % 4
                        linear(cT, "cT", 2, wb, "wb", n * 512, (n + 1) * 512, bank)
                        for hh in range(2):
                            h = n * 2 + hh
                            P.add("act", lambda e, h=h, hh=hh, bank=bank, b=b: e.copy(out=kn[b][:, h * 128:(h + 1) * 128], in_=psf[bank][:, hh * 256: hh * 256 + 128]),
                                  [PSF[bank]], ["kn%d" % b])
                            sumsq(psf[bank][:, hh * 256: hh * 256 + 128], 128, ksq[:, h:h + 1], junk, PSF[bank], "ksq")
                            P.add("dve", lambda e, h=h, hh=hh, bank=bank, b=b: e.tensor_copy(out=vx[b][:, h, 0:128], in_=psf[bank][:, hh * 256 + 128: hh * 256 + 256]),
                                  [PSF[bank]], ["vx%d" % b])
                    P.add("dve", lambda e: e.tensor_scalar(out=ksq[:, 0:8], in0=ksq[:, 0:8], scalar1=ksq[:, 8:9], scalar2=None, op0=ALU.add), ["ksq", "ksq8"], ["ksq"])
                    P.add("dve", lambda e: e.tensor_tensor(out=kmx[:], in0=kmx[:], in1=ksq[:, 0:8], op=ALU.max), ["ksq", "kmx"], ["kmx"])
                    transpose_blocks(kn[b], "kn%d" % b, 8, knT[b], "knT%d" % b)
                    dma("act", KT_d.ap()[:, :, t * 128:(t + 1) * 128].rearrange("h d t -> d h t"), knT[b][:], ["knT%d" % b], [("KT", t)], "knT%d" % b)
                    dma("act", V_d.ap()[t * 128:(t + 1) * 128], vx[b][:], ["vx%d" % b], [("V", t)], "vx%d" % b)
                P.add("dve", lambda e: e.reduce_max(out=ksq[:, 9:10], in_=kmx[:], axis=mybir.AxisListType.X), ["kmx"], ["kmr"])
                P.add("pe", lambda e: e.transpose(out=psf[5][0:1, 0:128], in_=ksq[:, 9:10], identity=ident_f[:]), ["kmr", "ident_f"], [PSF[5]])
                P.add("dve", lambda e: e.reduce_max(out=small[0:1, 20:21], in_=psf[5][0:1, 0:128], axis=mybir.AxisListType.X), [PSF[5]], ["km1"])
                P.add("pe", lambda e: e.matmul(psf[5][:, 256:257], lhsT=ones_f[0:1, :], rhs=small[0:1, 20:21], start=True, stop=True), ["km1", "ones_f"], [PSF[5]])
                P.add("dve", lambda e: e.tensor_copy(out=kmax_bc[:], in_=psf[5][:, 256:257]), [PSF[5]], ["kmax_bc"])
            P.barrier()

        def mla_layer(l):
            j = l - NA
            if skip():
                return
            with contextlib.ExitStack() as st:
                wa = sb(st, "wqa", [128, 8, 256], BF16)
                wb = sb(st, "wqb", [128, 2, 1536], BF16)
                stage = [sb(st, "wst", [128, 1536], F32) for _ in range(2)]
                load_w_bf16(st, wa, "wqa", W["mla_w_q_a"].ap()[j], 8, 256, stage, 2)
                load_w_bf16(st, wb, "wqb", W["mla_w_q_b"].ap()[j], 2, 1536, stage, 2)
                g_t = sb(st, "qg", [128, 256], F32)
                dma("sp", g_t[:], bc_row(W["mla_q_norm_g"], j * 256, 256), [], ["qg"], "qg")
                hs = [sb(st, "hs", [128, D], F32) for _ in range(2)]
                hb = sb(st, "hb", [128, D], BF16)
                hT = sb(st, "hT", [128, 8, 128], BF16)
                junk = sb(st, "junk", [128, 512], F32)
                small = sb(st, "small", [128, 32], F32)
                cb = sb(st, "cb", [128, 256], BF16)
                cT = sb(st, "cT", [128, 2, 128], BF16)
                cos_t = sb(st, "cos", [128, 32], F32)
                sin_t = sb(st, "sin", [128, 32], F32)
                tf = sb(st, "tf", [128, 32], F32)
                ti = sb(st, "ti", [128, 32], I32)
                t1 = sb(st, "t1", [128, 32], F32)
                t2 = sb(st, "t2", [128, 32], F32)
                qn = sb(st, "qn", [128, D], BF16)
                qr = sb(st, "qr", [128, 1024], BF16)
                qsq = sb(st, "qsq", [128, 8], F32)
                qsh = sb(st, "qsh", [128, 8], F32)
                qnT = [sb(st, "qnT", [128, 8, 128], BF16) for _ in range(2)]
                qrT = [sb(st, "qrT", [128, 8, 128], BF16) for _ in range(2)]
                for h_ in range(8):
                    P.add("pool", lambda e, h_=h_: e.tensor_copy(out=qr[:, h_ * 128:(h_ + 1) * 128], in_=zeros_f[:, 0:128]), ["zeros_f"], ["qr"])
                P.last_w["inv"] = P.last_w.get("inv_m")
                for t in range(NT):
                    b = t % 2
                    hn = "hs%d" % b
                    dma("sp", hs[b][:], hbuf.ap()[t * 128:(t + 1) * 128, :], [("hbuf", t)], [hn], hn)
                    P.add("pool", lambda e, b=b: e.tensor_copy(out=hb[:], in_=hs[b][:]), [hn], ["hb"])
                    transpose_blocks(hb, "hb", 8, hT, "hT")
                    rope_tables(t, inv_m, 32, cos_t, sin_t, tf, ti, "q")
                    linear(hT, "hT", 8, wa, "wqa", 0, 256, 4)
                    rms_norm_ps(psf[4][:, 0:256], PSF[4], 256, g_t, "qg", cb[:], "cb", junk, small, "small", 0)
                    transpose_blocks(cb, "cb", 2, cT, "cT")
                    for n in range(3):
                        linear(cT, "cT", 2, wb, "wqb", n * 512, (n + 1) * 512, n)
                    for h in range(8):
                        c0 = h * 192
                        def seg(a, bnd):
                            bk = a // 512
                            assert (bnd - 1) // 512 == bk
                            return psf[bk][:, a - bk * 512: bnd - bk * 512], PSF[bk]
                        a = c0
                        while a < c0 + 128:
                            bnd = min(c0 + 128, (a // 512 + 1) * 512)
                            ap_, nm = seg(a, bnd)
                            P.add("act", lambda e, ap_=ap_, a=a, bnd=bnd, h=h, c0=c0: e.copy(out=qn[:, h * 128 + (a - c0): h * 128 + (bnd - c0)], in_=ap_), [nm], ["qn"])
                            a = bnd
                        a = c0
                        pieces = []
                        while a < c0 + 192:
                            bnd = min(c0 + 192, (a // 512 + 1) * 512)
                            pieces.append((a, bnd))
                            a = bnd
                        for pi, (a, bnd) in enumerate(pieces):
                            ap_, nm = seg(a, bnd)
                            sumsq(ap_, bnd - a, small[:, 8 + pi: 9 + pi], junk, nm, "sq%d" % pi)
                        if len(pieces) == 2:
                            P.add("dve", lambda e, h=h: e.tensor_tensor(out=qsq[:, h:h + 1], in0=small[:, 8:9], in1=small[:, 9:10], op=ALU.add), ["sq0", "sq1"], ["qsq"])
                        else:
                            P.add("dve", lambda e, h=h: e.tensor_copy(out=qsq[:, h:h + 1], in_=small[:, 8:9]), ["sq0"], ["qsq"])
                        r1, n1_ = seg(c0 + 128, c0 + 160)
                        r2, n2_ = seg(c0 + 160, c0 + 192)
                        rope_apply(r1, r2, qr[:, h * 128:h * 128 + 32], qr[:, h * 128 + 32:h * 128 + 64], cos_t, sin_t, 32, t1, t2, list({n1_, n2_}), "qr", "q")
                    P.add("dve", lambda e: e.tensor_scalar(out=qsh[:], in0=qsq[:], scalar1=kmax_bc[:, 0:1], scalar2=1e-30, op0=ALU.mult, op1=ALU.add), ["qsq", "kmax_bc"], ["qsh"])
                    P.add("act", lambda e: e.activation(out=qsh[:], in_=qsh[:], func=AF.Ln), ["qsh"], ["qsh"])
                    P.add("act", lambda e: e.activation(out=qsh[:], in_=qsh[:], func=AF.Exp, scale=0.5), ["qsh"], ["qsh"])
                    for h in range(8):
                        P.add("dve", lambda e, h=h: e.tensor_scalar(out=qr[:, h * 128 + 64:h * 128 + 65], in0=qsh[:, h:h + 1], scalar1=-1.0, scalar2=None, op0=ALU.mult), ["qsh", "qr"], ["qr"])
                    transpose_blocks(qn, "qn", 8, qnT[b], "qnT%d" % b)
                    transpose_blocks(qr, "qr", 8, qrT[b], "qrT%d" % b, src_off=0)
                    dma("act", QT_d.ap()[:, :, t * 128:(t + 1) * 128].rearrange("h d t -> d h t"), qnT[b][:], ["qnT%d" % b], [("QT", t)], "qnT%d" % b)
                    dma("act", QR_d.ap()[:, :, t * 128:(t + 1) * 128].rearrange("h d t -> d h t"), qrT[b][0:65, :, :], ["qrT%d" % b], [("QR", t)], "qrT%d" % b)
            P.barrier()
            if skip():
                return
            with contextlib.ExitStack() as st:
                krT = sb(st, "krTa", [65, S], BF16)
                dma("sp", krT[:], KR_d.ap(), [], ["krTa"], "krTa")
                causal_f = sb(st, "causal_f", [128, 128], F32)
                causal = sb(st, "causal", [128, 128], BF16)
                dma("sp", causal_f[:], CD["causal"].ap(), [], ["causal_f"], "causal_f")
                P.add("dve", lambda e: e.tensor_copy(out=causal[:], in_=causal_f[:]), ["causal_f"], ["causal"])
                KT = [sb(st, "KTh", [128, S], BF16) for _ in range(2)]
                QT = [sb(st, "QTh", [128, S], BF16) for _ in range(2)]
                QR = [sb(st, "QRh", [65, S], BF16) for _ in range(2)]
                Vh = [sb(st, "Vh", [128, NT, 130], BF16) for _ in range(2)]
                pT = [sb(st, "pTa", [128, 512], BF16) for _ in range(2)]
                ob = [sb(st, "ob", [128, 128], BF16) for _ in range(2)]
                rsum = sb(st, "rsum", [128, 2], F32)
                QG = 512 if S >= 512 else S
                NQB = QG // 128
                it = 0
                oc = 0
                for h in range(8):
                    hb_ = h % 2
                    dma("sp", KT[hb_][:], KT_d.ap()[h], [], ["KT%d" % hb_], "KT%d" % hb_)
                    dma("sp", QT[hb_][:], QT_d.ap()[h], [], ["QT%d" % hb_], "QT%d" % hb_)
                    dma("sp", QR[hb_][:], QR_d.ap()[h], [], ["QR%d" % hb_], "QR%d" % hb_)
                    dma("sp", Vh[hb_][:], V_d.ap()[:, h, :].rearrange("(t p) c -> p t c", p=128), [], ["Vh%d" % hb_], "Vh%d" % hb_)
                    for qg in range(S // QG):
                        nkb = (qg + 1) * NQB
                        for kb in range(nkb):
                            sbk = 4 + it % 2
                            pb = it % 2
                            it += 1
                            P.add("pe", lambda e, kb=kb, qg=qg, sbk=sbk, hb_=hb_: e.matmul(psf[sbk][:, 0:QG], lhsT=KT[hb_][:, kb * 128:(kb + 1) * 128],
                                                                                        rhs=QT[hb_][:, qg * QG:(qg + 1) * QG], start=True, stop=False),
                                  ["KT%d" % hb_, "QT%d" % hb_], [PSF[sbk]])
                            P.add("pe", lambda e, kb=kb, qg=qg, sbk=sbk, hb_=hb_: e.matmul(psf[sbk][:, 0:QG], lhsT=krT[:, kb * 128:(kb + 1) * 128],
                                                                                        rhs=QR[hb_][:, qg * QG:(qg + 1) * QG], start=False, stop=True),
                                  ["krTa", "QR%d" % hb_], [PSF[sbk]])
                            P.add("act", lambda e, sbk=sbk, pb=pb: e.activation(out=pT[pb][:, 0:QG], in_=psf[sbk][:, 0:QG], func=AF.Exp, scale=SCALE),
                                  [PSF[sbk]], ["pTa%d" % pb])
                            for qb in range(NQB):
                                gq = qg * NQB + qb
                                if kb > gq:
                                    continue
                                if kb == gq:
                                    P.add("dve", lambda e, pb=pb, qb=qb: e.tensor_tensor(out=pT[pb][:, qb * 128:(qb + 1) * 128], in0=pT[pb][:, qb * 128:(qb + 1) * 128],
                                                                                         in1=causal[:], op=ALU.mult), ["pTa%d" % pb, "causal"], ["pTa%d" % pb])
                                P.add("pe", lambda e, pb=pb, qb=qb, kb=kb, gq=gq, hb_=hb_: e.matmul(psf[qb][:, 0:129], lhsT=pT[pb][:, qb * 128:(qb + 1) * 128],
                                                                                                  rhs=Vh[hb_][:, kb, 0:129], start=(kb == 0), stop=(kb == gq)),
                                      ["pTa%d" % pb, "Vh%d" % hb_], [PSF[qb]])
                                if kb == gq:
                                    o_ = oc % 2
                                    oc += 1
                                    P.add("dve", lambda e, qb=qb: e.reciprocal(out=rsum[:, 0:1], in_=psf[qb][:, 128:129]), [PSF[qb]], ["rsum"])
                                    P.add("dve", lambda e, qb=qb, o_=o_: e.tensor_scalar(out=ob[o_][:], in0=psf[qb][:, 0:128], scalar1=rsum[:, 0:1], scalar2=None, op0=ALU.mult),
                                          [PSF[qb], "rsum"], ["ob%d" % o_])
                                    dma("pool", O_d.ap()[gq * 128:(gq + 1) * 128, h * 128:(h + 1) * 128], ob[o_][:], ["ob%d" % o_], [("O", gq)], "ob%d" % o_)
            P.barrier()
            if skip():
                return
            with contextlib.ExitStack() as st:
                wo = sb(st, "wo", [128, 8, D], BF16)
                stage = [sb(st, "wst", [128, 1024], F32) for _ in range(2)]
                load_w_bf16(st, wo, "wo", W["mla_w_o"].ap()[j], 8, D, stage, 2)
                ln_g = sb(st, "ln_g", [128, D], F32)
                ln_b = sb(st, "ln_b", [128, D], F32)
                dma("sp", ln_g[:], bc_row(W["ln_mix_g"], l * D, D), [], ["ln_g"], "ln_g")
                dma("sp", ln_b[:], bc_row(W["ln_mix_b"], l * D, D), [], ["ln_b"], "ln_b")
                hs = [sb(st, "hs", [128, D], F32) for _ in range(2)]
                orow = [sb(st, "orow", [128, D], BF16) for _ in range(2)]
                oT = sb(st, "oT", [128, 8, 128], BF16)
                z = sb(st, "z", [128, D], F32)
                h1 = [sb(st, "h1", [128, D], F32) for _ in range(2)]
                small = sb(st, "small", [128, 32], F32)
                for t in range(NT):
                    b = t % 2
                    hn = "hs%d" % b
                    dma("sp", hs[b][:], hbuf.ap()[t * 128:(t + 1) * 128, :], [("hbuf", t)], [hn], hn)
                    dma("sp", orow[b][:], O_d.ap()[t * 128:(t + 1) * 128, :], [], ["orow%d" % b], "orow%d" % b)
                    transpose_blocks(orow[b], "orow%d" % b, 8, oT, "oT")
                    for nh in range(2):
                        linear(oT, "oT", 8, wo, "wo", nh * 512, (nh + 1) * 512, nh)
                        P.add("dve", lambda e, nh=nh, b=b: e.scalar_tensor_tensor(out=z[:, nh * 512:(nh + 1) * 512], in0=hs[b][:, nh * 512:(nh + 1) * 512],
                                                                                  scalar=ALPHA, in1=psf[nh][:, :], op0=ALU.mult, op1=ALU.add), [hn, PSF[nh]], ["z"])
                    layer_norm(z, "z", h1[b], "h1%d" % b, ln_g, ln_b, ["ln_g", "ln_b"], small, "small")
                    dma("pool", hbuf.ap()[t * 128:(t + 1) * 128, :], h1[b][:], ["h1%d" % b], [("hbuf", t)], "h1%d" % b)
            P.barrier()

        fin = []
        for l in range(DEPTH):
            if l < NA:
                retention_layer(l)
            else:
                if l == NA:
                    mla_kv()
                mla_layer(l)
            moe_ple(l, l == DEPTH - 1)
        if LIMIT < 999:
            fin.append(dma('sp', y_d.ap(), hbuf.ap(), [], [], 'ydbg'))
            if dbg:
                fin.append(dma('sp', dq_d.ap(), qkvg_d.ap(), [], [], 'ydbg2'))
        P.emit(final_ops=fin)
    return nc, consts_np


_CACHE = {}


def run(inputs, S, E, DEPTH, NA, CAP, cores, dbg=False):
    key = (S, E, DEPTH, NA, CAP, dbg)
    if key not in _CACHE:
        _CACHE[key] = build(S, E, DEPTH, NA, CAP, dbg)
    nc, consts_np = _CACHE[key]
    NT = S // 128
    f32 = lambda a: np.ascontiguousarray(np.asarray(a), dtype=np.float32)
    shared = {}
    for k in ("ret_w_in", "ret_gn_g", "ret_gn_b", "ret_w_out", "mla_w_kv_a", "mla_w_kv_b", "mla_w_q_a", "mla_q_norm_g",
              "mla_w_q_b", "mla_w_o", "ln_mix_g", "ln_mix_b", "ln_ffn_g", "ln_ffn_b", "moe_w_router", "moe_b_router",
              "moe_w_gate_up", "moe_b_gate_up", "moe_w_down", "moe_b_down", "ple_w_gate", "ple_w_proj"):
        shared[k] = f32(inputs[k])
    shared["mla_kv_norm_g"] = f32(inputs["mla_kv_norm_g"]).reshape(1, 256)
    bgu_ = shared["moe_b_gate_up"]
    shared["moe_b_gate_up"] = np.ascontiguousarray(bgu_.reshape(bgu_.shape[0], bgu_.shape[1], 16, 128).transpose(0, 1, 3, 2))
    for k, v in consts_np.items():
        shared["c_" + k] = f32(v)
    x = f32(inputs["x"])
    p = f32(inputs["p"])
    pos = np.asarray(inputs["positions"]).astype(np.int32)
    in_maps = []
    for c in range(cores):
        m = dict(shared)
        m["x"] = x[c]
        m["p"] = np.ascontiguousarray(p[:, c])
        m["pos_tm"] = np.ascontiguousarray(pos[c].reshape(NT, 128).T)
        in_maps.append(m)
    res = run_bass_kernel_spmd(nc, in_maps, core_ids=list(range(cores)))
    return res.results


def kernel(**inputs):
    S, E, DEPTH, NA = 8192, 32, 4, 2
    CAP = 1280
    res = run(inputs, S, E, DEPTH, NA, CAP, cores=4)
    return np.stack([r["y"] for r in res], axis=0).astype(np.float32)
```
